# Optimizing a Trainium2 kernel written in Bass

```python
import math
import jax, jax.numpy as jnp
from jax import lax
import numpy as np

D_MODEL = 1024
BATCH = 8
SEQ = 2048
DEPTH = 2

D_MIX = D_MODEL
D_SSM = D_MIX // 2
SSM_GROUP = 16
N_SSM_HEADS = D_SSM // SSM_GROUP
SSM_STATE = 64
DT_MIN = 0.001
DT_MAX = 0.1
D_POOL = D_MIX - D_SSM
POOL_WINDOWS = (2, 4, 8, 16)
N_POOL_GROUPS = len(POOL_WINDOWS)
POOL_GROUP_DIM = D_POOL // N_POOL_GROUPS
D_FF = 2816
N_EXPERTS = 8
TOP_K = 2
D_FF_EXPERT = 2816
N_DENSE = (DEPTH + 1) // 2
N_MOE = DEPTH // 2
RMS_EPS = 1e-6

kernel_name = "hybrid_s5_pool_moe_trunk"


def rmsnorm(x, g):
    xf = x.astype(jnp.float32)
    inv = lax.rsqrt(jnp.mean(xf * xf, axis=-1, keepdims=True) + RMS_EPS)
    return (xf * inv * g.astype(jnp.float32)).astype(x.dtype)


def _complex_affine_combine(left, right):
    a1r, a1i, b1r, b1i = left
    a2r, a2i, b2r, b2i = right
    ar = a2r * a1r - a2i * a1i
    ai = a2r * a1i + a2i * a1r
    br = a2r * b1r - a2i * b1i + b2r
    bi = a2r * b1i + a2i * b1r + b2i
    return (ar, ai, br, bi)


def s5_mixer(u, log_dt, a_re, a_im, b_re, b_im, c_re, c_im, d, w_glu, b_glu):
    bsz, L, _ = u.shape
    uf = u.astype(jnp.float32)
    ug = uf.reshape(bsz, L, N_SSM_HEADS, SSM_GROUP)
    dt = jnp.exp(log_dt.astype(jnp.float32))[:, None]
    ar = jnp.minimum(a_re.astype(jnp.float32), -1e-4)
    ai = a_im.astype(jnp.float32)
    mag = jnp.exp(ar * dt)
    lam_re = mag * jnp.cos(ai * dt)
    lam_im = mag * jnp.sin(ai * dt)
    den = ar * ar + ai * ai
    nr = lam_re - 1.0
    ni = lam_im
    coef_re = (nr * ar + ni * ai) / den
    coef_im = (ni * ar - nr * ai) / den
    br = b_re.astype(jnp.float32)
    bi = b_im.astype(jnp.float32)
    bb_re = coef_re[..., None] * br - coef_im[..., None] * bi
    bb_im = coef_re[..., None] * bi + coef_im[..., None] * br
    bu_re = jnp.einsum('blhg,hpg->blhp', ug, bb_re)
    bu_im = jnp.einsum('blhg,hpg->blhp', ug, bb_im)
    shape = (1, L, N_SSM_HEADS, SSM_STATE)
    a_seq_re = jnp.broadcast_to(lam_re[None, None], shape)
    a_seq_im = jnp.broadcast_to(lam_im[None, None], shape)
    _, _, s_re, s_im = lax.associative_scan(
        _complex_affine_combine, (a_seq_re, a_seq_im, bu_re, bu_im), axis=1)
    y = (jnp.einsum('blhp,hgp->blhg', s_re, c_re.astype(jnp.float32))
         - jnp.einsum('blhp,hgp->blhg', s_im, c_im.astype(jnp.float32)))
    y = y.reshape(bsz, L, D_SSM) + d.astype(jnp.float32) * uf
    h = jax.nn.gelu(y)
    out = h * jax.nn.sigmoid(h @ w_glu.astype(jnp.float32) + b_glu.astype(jnp.float32))
    return out.astype(u.dtype)


def pool_mixer(xp, pool_w, pool_scale):
    bsz, L, _ = xp.shape
    xg = xp.astype(jnp.float32).reshape(bsz, L, N_POOL_GROUPS, POOL_GROUP_DIM)
    pos = jnp.arange(L)
    outs = []
    for g, w in enumerate(POOL_WINDOWS):
        xc = xg[:, :, g]
        cs0 = jnp.pad(jnp.cumsum(xc, axis=1), ((0, 0), (1, 0), (0, 0)))
        lag = jnp.pad(cs0, ((0, 0), (w - 1, 0), (0, 0)))[:, :L]
        count = jnp.minimum(pos + 1, w).astype(jnp.float32)[None, :, None]
        outs.append((cs0[:, 1:] - lag) / count - xc)
    pooled = jnp.stack(outs, axis=2)
    mixed = jnp.einsum('blgc,gcd->blgd', pooled, pool_w.astype(jnp.float32))
    out = mixed.reshape(bsz, L, D_POOL) * pool_scale.astype(jnp.float32)
    return out.astype(xp.dtype)


def swiglu(h, w_gate, w_up, w_down):
    return (jax.nn.silu(h @ w_gate) * (h @ w_up)) @ w_down


def moe_swiglu(h, router_w, w_gate, w_up, w_down):
    logits = (h @ router_w).astype(jnp.float32)
    top_v, top_i = lax.top_k(logits, TOP_K)
    top_g = jax.nn.softmax(top_v, axis=-1)
    gates = jnp.sum(jax.nn.one_hot(top_i, N_EXPERTS, dtype=jnp.float32) * top_g[..., None], axis=-2)
    out = jnp.zeros(h.shape, jnp.float32)
    for e in range(N_EXPERTS):
        y_e = swiglu(h, w_gate[e], w_up[e], w_down[e]).astype(jnp.float32)
        out = out + gates[..., e:e + 1] * y_e
    return out.astype(h.dtype)


def setup_inputs(seed: int = 0) -> dict:
    key = jax.random.key(seed)
    ks = jax.random.split(key, 24)
    f32 = jnp.float32

    def nrm(k, shape, scale):
        return jax.random.normal(k, shape, f32) * scale

    H, P, G = N_SSM_HEADS, SSM_STATE, SSM_GROUP
    x = jax.random.normal(ks[0], (BATCH, SEQ, D_MODEL), f32)
    norm_mix_g = 1.0 + nrm(ks[1], (DEPTH, D_MODEL), 0.02)
    w_in = nrm(ks[2], (DEPTH, D_MODEL, D_MIX), D_MODEL ** -0.5)
    ssm_log_dt = jax.random.uniform(ks[3], (DEPTH, H), f32, math.log(DT_MIN), math.log(DT_MAX))
    ssm_a_re = -0.5 + nrm(ks[4], (DEPTH, H, P), 0.01)
    ssm_a_im = jnp.pi * jnp.arange(P, dtype=f32)[None, None, :] + nrm(ks[5], (DEPTH, H, P), 0.01)
    ssm_b_re = nrm(ks[6], (DEPTH, H, P, G), (2.0 * G) ** -0.5)
    ssm_b_im = nrm(ks[7], (DEPTH, H, P, G), (2.0 * G) ** -0.5)
    ssm_c_re = nrm(ks[8], (DEPTH, H, G, P), (2.0 * P) ** -0.5 * 4.0)
    ssm_c_im = nrm(ks[9], (DEPTH, H, G, P), (2.0 * P) ** -0.5 * 4.0)
    ssm_d = nrm(ks[10], (DEPTH, D_SSM), 1.0)
    ssm_w_glu = nrm(ks[11], (DEPTH, D_SSM, D_SSM), D_SSM ** -0.5)
    ssm_b_glu = nrm(ks[12], (DEPTH, D_SSM), 0.02)
    pool_w = nrm(ks[13], (DEPTH, N_POOL_GROUPS, POOL_GROUP_DIM, POOL_GROUP_DIM), POOL_GROUP_DIM ** -0.5)
    pool_scale = 1.0 + nrm(ks[14], (DEPTH, D_POOL), 0.02)
    w_out = nrm(ks[15], (DEPTH, D_MIX, D_MODEL), D_MIX ** -0.5)
    norm_ffn_g = 1.0 + nrm(ks[16], (DEPTH, D_MODEL), 0.02)
    ffn_w_gate = nrm(ks[17], (N_DENSE, D_MODEL, D_FF), D_MODEL ** -0.5)
    ffn_w_up = nrm(ks[18], (N_DENSE, D_MODEL, D_FF), D_MODEL ** -0.5)
    ffn_w_down = nrm(ks[19], (N_DENSE, D_FF, D_MODEL), D_FF ** -0.5)
    router_w = nrm(ks[20], (N_MOE, D_MODEL, N_EXPERTS), D_MODEL ** -0.5)
    moe_w_gate = nrm(ks[21], (N_MOE, N_EXPERTS, D_MODEL, D_FF_EXPERT), D_MODEL ** -0.5)
    moe_w_up = nrm(ks[22], (N_MOE, N_EXPERTS, D_MODEL, D_FF_EXPERT), D_MODEL ** -0.5)
    k_down, k_fin = jax.random.split(ks[23])
    moe_w_down = nrm(k_down, (N_MOE, N_EXPERTS, D_FF_EXPERT, D_MODEL), D_FF_EXPERT ** -0.5)
    final_norm_g = 1.0 + nrm(k_fin, (D_MODEL,), 0.02)
    return {
        "x": x, "norm_mix_g": norm_mix_g, "w_in": w_in,
        "ssm_log_dt": ssm_log_dt, "ssm_a_re": ssm_a_re, "ssm_a_im": ssm_a_im,
        "ssm_b_re": ssm_b_re, "ssm_b_im": ssm_b_im, "ssm_c_re": ssm_c_re, "ssm_c_im": ssm_c_im,
        "ssm_d": ssm_d, "ssm_w_glu": ssm_w_glu, "ssm_b_glu": ssm_b_glu,
        "pool_w": pool_w, "pool_scale": pool_scale, "w_out": w_out,
        "norm_ffn_g": norm_ffn_g, "ffn_w_gate": ffn_w_gate, "ffn_w_up": ffn_w_up, "ffn_w_down": ffn_w_down,
        "router_w": router_w, "moe_w_gate": moe_w_gate, "moe_w_up": moe_w_up, "moe_w_down": moe_w_down,
        "final_norm_g": final_norm_g,
    }


def reference(x, norm_mix_g, w_in, ssm_log_dt, ssm_a_re, ssm_a_im, ssm_b_re, ssm_b_im,
              ssm_c_re, ssm_c_im, ssm_d, ssm_w_glu, ssm_b_glu, pool_w, pool_scale, w_out,
              norm_ffn_g, ffn_w_gate, ffn_w_up, ffn_w_down, router_w, moe_w_gate, moe_w_up,
              moe_w_down, final_norm_g):
    h_res = x
    for i in range(DEPTH):
        h = rmsnorm(h_res, norm_mix_g[i])
        proj = h @ w_in[i]
        u_ssm = proj[..., :D_SSM]
        u_pool = proj[..., D_SSM:]
        y_ssm = s5_mixer(u_ssm, ssm_log_dt[i], ssm_a_re[i], ssm_a_im[i], ssm_b_re[i], ssm_b_im[i],
                         ssm_c_re[i], ssm_c_im[i], ssm_d[i], ssm_w_glu[i], ssm_b_glu[i])
        y_pool = pool_mixer(u_pool, pool_w[i], pool_scale[i])
        mixed = jnp.concatenate([y_ssm, y_pool], axis=-1)
        h_res = h_res + mixed @ w_out[i]
        h = rmsnorm(h_res, norm_ffn_g[i])
        j = i // 2
        if i % 2 == 0:
            y = swiglu(h, ffn_w_gate[j], ffn_w_up[j], ffn_w_down[j])
        else:
            y = moe_swiglu(h, router_w[j], moe_w_gate[j], moe_w_up[j], moe_w_down[j])
        h_res = h_res + y
    return rmsnorm(h_res, final_norm_g)
```

```python
import math
import os
import numpy as np
from contextlib import ExitStack
import concourse.bass as bass
import concourse.mybir as mybir
from concourse.bass_utils import run_bass_kernel_spmd

F32 = mybir.dt.float32
BF16 = mybir.dt.bfloat16
I32 = mybir.dt.int32
ALU = mybir.AluOpType
AF = mybir.ActivationFunctionType
AX = mybir.AxisListType
ENGS = ["tensor", "vector", "scalar", "gpsimd", "sync"]

L = 2048
NTC = 4
TC = 512
J = 8
MB = L // J
NF = 22
NS = 8
GROUPS = [4, 4, 4, 4, 3, 3]
TWO_PI = 2.0 * math.pi
CARVE_INFO = {}


class Tok:
    __slots__ = ("sem", "val")

    def __init__(self, sem, val):
        self.sem = sem
        self.val = val


class T:
    def __init__(self, ap, name=""):
        self.ap = ap
        self.name = name
        self.last_w = None
        self.readers = []

    def __getitem__(self, k):
        return self.ap[k]


class DmaGroup:
    def __init__(self, sem):
        self.sem = sem
        self.count = 0


class Prog:
    def __init__(self, nc, ctx):
        self.nc = nc
        self.ctx = ctx
        self.streams = {e: [] for e in ENGS}
        self.sem = {e: ctx.enter_context(nc.semaphore("s_" + e)) for e in ENGS}
        self.cnt = {e: 0 for e in ENGS}
        self.waited = {e: {} for e in ENGS}
        self.pend_r = {e: [] for e in ENGS}
        self.pend_w = {e: [] for e in ENGS}
        self.groups = []
        self.ps_i = 0

    def sbuf(self, name, shape, dtype):
        return self.ctx.enter_context(self.nc.sbuf_tensor("sb_" + name, shape, dtype))

    def dma_group(self, name):
        g = DmaGroup(self.ctx.enter_context(self.nc.semaphore(name)))
        self.groups.append(g)
        return g

    def grp(self, key):
        if not hasattr(self, "_grps"):
            self._grps = {}
        if key not in self._grps:
            self._grps[key] = self.dma_group("dg_" + key)
        return self._grps[key]

    def _waits(self, eng, reads, writes, extra=()):
        need = {}

        def add(tok):
            if tok is None:
                return
            k = id(tok.sem)
            if k not in need or need[k].val < tok.val:
                need[k] = tok

        for t in reads:
            add(t.last_w)
            for e2 in ENGS:
                if e2 != eng:
                    assert not any(t is w for w in self.pend_w[e2]), ("RAW on unsignaled write", t.name, eng, e2)
        for t in writes:
            add(t.last_w)
            for r in t.readers:
                add(r)
            for e2 in ENGS:
                if e2 != eng:
                    assert not any(t is w for w in self.pend_r[e2]), ("WAR on unsignaled read", t.name, eng, e2)
                    assert not any(t is w for w in self.pend_w[e2]), ("WAW on unsignaled write", t.name, eng, e2)
        for tok in extra:
            add(tok)
        own = self.sem[eng]
        for k, tok in need.items():
            if eng == "tensor" and tok.sem is own:
                continue
            if self.waited[eng].get(k, 0) >= tok.val:
                continue
            self.waited[eng][k] = tok.val
            self.streams[eng].append(("wait", tok.sem, tok.val))

    def _commit(self, tok, reads, writes):
        for t in writes:
            t.last_w = tok
            t.readers = []
        for t in reads:
            rs = [r for r in t.readers if r.sem is not tok.sem]
            rs.append(tok)
            t.readers = rs

    def op(self, eng, fn, reads=(), writes=(), signal=True, extra=()):
        reads = list(reads)
        writes = list(writes)
        self._waits(eng, reads, writes, extra)
        if not signal:
            self.pend_r[eng] += reads
            self.pend_w[eng] += writes
            self.streams[eng].append(("op", fn, None))
            return None
        self.cnt[eng] += 1
        tok = Tok(self.sem[eng], self.cnt[eng])
        self.streams[eng].append(("op", fn, (tok.sem, 1)))
        self._commit(tok, reads + self.pend_r[eng], writes + self.pend_w[eng])
        self.pend_r[eng] = []
        self.pend_w[eng] = []
        return tok

    def dma(self, eng, fn, grp, reads=(), writes=(), extra=()):
        reads = list(reads)
        writes = list(writes)
        self._waits(eng, reads, writes, extra)
        if grp.count > 0:
            self.wait_tok(eng, Tok(grp.sem, 16 * grp.count))
        grp.count += 1
        tok = Tok(grp.sem, 16 * grp.count)
        self.streams[eng].append(("op", fn, (grp.sem, 16)))
        self._commit(tok, reads, writes)
        return tok

    def cond_begin(self, flag_ap, flag_T):
        for e in ENGS:
            assert not self.pend_r[e] and not self.pend_w[e]
        for e in ("tensor", "vector", "scalar", "gpsimd"):
            self._waits(e, [flag_T], [])
        self._cond = (flag_ap, self.streams, {e: dict(self.waited[e]) for e in ENGS})
        self.streams = {e: [] for e in ENGS}

    def cond_end(self):
        flag_ap, outer, saved_waited = self._cond
        inner = self.streams
        self.streams = outer
        for e in ENGS:
            assert not self.pend_r[e] and not self.pend_w[e]
            items = inner[e]
            if not items:
                continue
            incs = {}
            for it in items:
                if it[0] == "op" and it[2] is not None:
                    k = id(it[2][0])
                    if k not in incs:
                        incs[k] = [it[2][0], 0]
                    incs[k][1] += it[2][1]
            self.streams[e].append(("cond", flag_ap, items, list(incs.values())))
            self.waited[e] = saved_waited[e]
        self._cond = None

    def wait_tok(self, eng, tok):
        k = id(tok.sem)
        if self.waited[eng].get(k, 0) < tok.val:
            self.waited[eng][k] = tok.val
            self.streams[eng].append(("wait", tok.sem, tok.val))

    def barrier(self):
        toks = [Tok(self.sem[e], self.cnt[e]) for e in ENGS if self.cnt[e] > 0]
        toks += [Tok(g.sem, 16 * g.count) for g in self.groups if g.count > 0]
        for e in ENGS:
            for t in toks:
                if t.sem is self.sem[e]:
                    continue
                self.wait_tok(e, t)

    def finish(self):
        nc = self.nc
        streams = self.streams

        def run_items(e, items, regbox):
            for item in items:
                if item[0] == "wait":
                    e.wait_ge(item[1], item[2])
                elif item[0] == "cond":
                    if regbox[0] is None:
                        regbox[0] = e.alloc_register("flagreg")
                    r = regbox[0]
                    e.reg_load(r, item[1])
                    with e.If_eq(r, 1):
                        run_items(e, item[2], regbox)
                    with e.Else():
                        for sem, n in item[3]:
                            e.sem_inc(sem, n)
                else:
                    ins = item[1](e)
                    if item[2] is not None:
                        ins.then_inc(item[2][0], item[2][1])

        def replay(name):
            def f(e):
                run_items(e, streams[name], [None])
            return f

        with nc.Block() as block:
            block.tensor(replay("tensor"))
            block.vector(replay("vector"))
            block.scalar(replay("scalar"))
            block.gpsimd(replay("gpsimd"))
            block.sync(replay("sync"))


def build_program(stages=("mix0", "ffn0", "mix1", "ffn1", "final"), dbg=False):
    nc = bass.Bass("TRN2", target_bir_lowering=False)

    def din(name, shape, dt=F32):
        return nc.dram_tensor(name, shape, dt, kind="ExternalInput").ap()

    d_x = din("xT", [128, 8, L])
    d_gains = din("gains", [128, 5, 8])
    d_win = din("w_in", [2, 128, 8, 1024])
    d_wout = din("w_out", [2, 128, 8, 1024])
    d_wglu = din("w_glu", [2, 128, 4, 512])
    d_vec = din("vec512", [128, 2, 3, 4])
    d_poolw = din("pool_w", [2, 128, 4, 128])
    d_ssma = din("ssm_a", [2, 128, 3, 16])
    d_ssmbc = din("ssm_bc", [2, 128, 4, 16, 16])
    d_fgu = din("ffn_gu", [NF, 128, 2, 8, 128])
    d_fdn = din("ffn_dn", [NF, 128, 1024])
    d_mgu = din("moe_gu", [8 * NF, 128, 2, 8, 128])
    d_mdn = din("moe_dn", [8 * NF, 128, 1024])
    d_router = din("router", [128, 8, 8])
    d_out = nc.dram_tensor("yT", [128, 8, L], F32, kind="ExternalOutput").ap()
    d_dbg = None
    if dbg:
        d_dbg = nc.dram_tensor("dbg", [4, 128, 8, L], F32, kind="ExternalOutput").ap()
        d_dbgh = nc.dram_tensor("dbgh", [2, 128, 8, L], BF16, kind="ExternalOutput").ap()
        d_dbgs = nc.dram_tensor("dbgs", [128, 41456], BF16, kind="ExternalOutput").ap()
        d_dbgw = nc.dram_tensor("dbgw", [128, 8192], BF16, kind="ExternalOutput").ap()

    with ExitStack() as ctx:
        P = Prog(nc, ctx)
        op = P.op

        xres_t = P.sbuf("xres", [128, 8, L], F32)
        h_t = P.sbuf("hbf", [128, 8, L], BF16)
        wbuf_t = P.sbuf("wbuf", [128, 8 * 1024], BF16)
        wglu_t = P.sbuf("wglu", [128, 4, 512], BF16)
        poolw_t = P.sbuf("poolw", [128, 4, 128], BF16)
        gains_t = P.sbuf("gains", [128, 5, 8], F32)
        vec_t = P.sbuf("vec", [128, 2, 3, 4], F32)
        router_t = P.sbuf("router", [128, 8, 8], F32)
        ssma_t = P.sbuf("ssma", [128, 3, 16], F32)
        ssmbc_t = P.sbuf("ssmbc", [128, 4, 16, 16], F32)
        ident_t = P.sbuf("ident", [128, 128], F32)
        onesb_t = P.sbuf("onesb", [128, 128], BF16)
        onesf_t = P.sbuf("onesf", [128, 128], F32)
        iota1_t = P.sbuf("iota1", [128, 256], F32)
        iotaf_t = P.sbuf("iotaf", [128, 128], F32)
        iotap_t = P.sbuf("iotap", [128, 1], F32)
        maskq_t = P.sbuf("maskq", [128, 4], F32)
        maskb_t = P.sbuf("maskb", [128, 4, 128], F32)
        invc_t = P.sbuf("invc", [128, 4, 16], F32)
        SCR_ELEMS = 41456
        scr_t = P.sbuf("scr", [128, SCR_ELEMS], BF16)
        psum_t = [ctx.enter_context(nc.psum_tensor(f"ps{i}", [128, 512], F32)) for i in range(8)]

        XR = [[T(xres_t[:, c, tc * TC:(tc + 1) * TC], f"xr{c}_{tc}") for tc in range(NTC)] for c in range(8)]
        H = [[T(h_t[:, c, tc * TC:(tc + 1) * TC], f"h{c}_{tc}") for tc in range(NTC)] for c in range(8)]
        WBUF = T(wbuf_t[:, :], "wbuf")
        WGLU = T(wglu_t[:], "wglu")
        POOLW = T(poolw_t[:], "poolw")
        GAINS = T(gains_t[:], "gains")
        VEC = T(vec_t[:], "vec")
        ROUTER = T(router_t[:], "router")
        SSMA = T(ssma_t[:], "ssma")
        SSMBC = T(ssmbc_t[:], "ssmbc")
        IDENT = T(ident_t[:], "ident")
        ONESB = T(onesb_t[:], "onesb")
        ONESF = T(onesf_t[:], "onesf")
        IOTA1 = T(iota1_t[:], "iota1")
        IOTAF = T(iotaf_t[:], "iotaf")
        IOTAP = T(iotap_t[:], "iotap")
        MASKQ = T(maskq_t[:], "maskq")
        MASKB = T(maskb_t[:], "maskb")
        INVC = T(invc_t[:], "invc")
        PS = [T(psum_t[i][:], f"ps{i}") for i in range(8)]

        def next_ps():
            p = PS[P.ps_i % 8]
            P.ps_i += 1
            return p

        class Carver:
            def __init__(self):
                self.off = 0

            def take(self, shape, dtype, name):
                n = int(np.prod(shape))
                el = n * (2 if dtype in (F32, I32) else 1)
                if self.off % 2:
                    self.off += 1
                a = scr_t[:, self.off:self.off + el]
                CARVE_INFO[name] = (self.off, tuple(shape), str(dtype))
                self.off += el
                assert self.off <= SCR_ELEMS, (name, self.off)
                if dtype != BF16:
                    a = a.bitcast(dtype)
                if len(shape) == 2:
                    a = a.rearrange("p (a b) -> p a b", a=shape[0])
                elif len(shape) == 3:
                    a = a.rearrange("p (a b c) -> p a b c", a=shape[0], b=shape[1])
                elif len(shape) == 4:
                    a = a.rearrange("p (a b c d) -> p a b c d", a=shape[0], b=shape[1], c=shape[2])
                return T(a, name)

        g_out = P.dma_group("g_dbg")

        op("gpsimd", lambda e: e.iota(iotaf_t[:], [[1, 128]], base=0, channel_multiplier=0, allow_small_or_imprecise_dtypes=True), writes=[IOTAF])
        op("gpsimd", lambda e: e.iota(iotap_t[:], [[1, 1]], base=0, channel_multiplier=1, allow_small_or_imprecise_dtypes=True), writes=[IOTAP])
        op("gpsimd", lambda e: e.iota(iota1_t[:], [[1, 256]], base=1, channel_multiplier=0, allow_small_or_imprecise_dtypes=True), writes=[IOTA1])
        op("vector", lambda e: e.tensor_scalar(out=ident_t[:], in0=iotaf_t[:], scalar1=iotap_t[:, 0:1], scalar2=None, op0=ALU.is_equal), reads=[IOTAF, IOTAP], writes=[IDENT])
        op("vector", lambda e: e.memset(onesb_t[:], 1.0), writes=[ONESB])
        op("vector", lambda e: e.memset(onesf_t[:], 1.0), writes=[ONESF])
        tmpm_t = P.sbuf("tmpm", [128, 4], F32)
        TMPM = T(tmpm_t[:], "tmpm")
        for q4 in range(4):
            op("vector", lambda e, q4=q4: e.tensor_single_scalar(out=maskq_t[:, q4:q4 + 1], in_=iotap_t[:, 0:1], scalar=float(32 * q4), op=ALU.is_ge), reads=[IOTAP], writes=[MASKQ])
            op("vector", lambda e, q4=q4: e.tensor_single_scalar(out=tmpm_t[:, q4:q4 + 1], in_=iotap_t[:, 0:1], scalar=float(32 * q4 + 32), op=ALU.is_lt), reads=[IOTAP], writes=[TMPM])
        op("vector", lambda e: e.tensor_tensor(out=maskq_t[:], in0=maskq_t[:], in1=tmpm_t[:], op=ALU.mult), reads=[MASKQ, TMPM], writes=[MASKQ])
        for q4 in range(4):
            op("vector", lambda e, q4=q4: e.tensor_copy(out=maskb_t[:, :, 32 * q4:32 * q4 + 32], in_=maskq_t[:, q4:q4 + 1].unsqueeze(2).to_broadcast([128, 4, 32])),
               reads=[MASKQ], writes=[MASKB])
        for g in range(4):
            op("vector", lambda e, g=g: e.tensor_single_scalar(out=invc_t[:, g, :], in_=iota1_t[:, 0:16], scalar=float(2 ** (g + 1)), op=ALU.min), reads=[IOTA1], writes=[INVC])
        op("vector", lambda e: e.reciprocal(out=invc_t[:], in_=invc_t[:]), reads=[INVC], writes=[INVC])

        for c in range(8):
            P.dma("sync", lambda e, c=c: e.dma_start(out=xres_t[:, c, :], in_=d_x[:, c, :]), P.grp(f"x{c}"), writes=XR[c])
        P.dma("sync", lambda e: e.dma_start(out=gains_t[:], in_=d_gains), P.grp("gains"), writes=[GAINS])
        P.dma("sync", lambda e: e.dma_start(out=vec_t[:], in_=d_vec), P.grp("vec"), writes=[VEC])
        P.dma("sync", lambda e: e.dma_start(out=router_t[:], in_=d_router), P.grp("router"), writes=[ROUTER])

        def load_wbuf(src):
            P.dma("gpsimd", lambda e: e.dma_start(out=wbuf_t[:, :].rearrange("p (a b) -> p a b", a=8), in_=src), P.grp("wbuf"), writes=[WBUF])

        def load_mixer_small(l):
            P.dma("gpsimd", lambda e: e.dma_start(out=wglu_t[:], in_=d_wglu[l]), P.grp("wglu"), writes=[WGLU])
            P.dma("gpsimd", lambda e: e.dma_start(out=poolw_t[:], in_=d_poolw[l]), P.grp("poolw"), writes=[POOLW])
            P.dma("sync", lambda e: e.dma_start(out=ssma_t[:], in_=d_ssma[l]), P.grp("ssma"), writes=[SSMA])
            P.dma("sync", lambda e: e.dma_start(out=ssmbc_t[:], in_=d_ssmbc[l]), P.grp("ssmbc"), writes=[SSMBC])

        def rmsnorm(gi, sqb, rst, tmpl, sink, rstd_keep=None):
            for tc in range(NTC):
                ps = next_ps()
                for c in range(8):
                    sq = sqb[(tc * 8 + c) % len(sqb)]
                    op("scalar", lambda e, sq=sq, c=c, tc=tc: e.activation(out=sq[:], in_=XR[c][tc][:], func=AF.Square), reads=[XR[c][tc]], writes=[sq])
                    op("tensor", lambda e, sq=sq, ps=ps, c=c: e.matmul(ps[:], lhsT=onesb_t[:], rhs=sq[:], start=(c == 0), stop=(c == 7)),
                       reads=[sq, ONESB], writes=[ps], signal=True)
                r = rst[tc % len(rst)] if rstd_keep is None else rstd_keep[tc]
                op("scalar", lambda e, ps=ps: e.activation(out=tmpl[:], in_=ps[:], func=AF.Ln, scale=1.0 / 1024.0, bias=1e-6), reads=[ps], writes=[tmpl])
                op("scalar", lambda e, r=r: e.activation(out=r[:], in_=tmpl[:], func=AF.Exp, scale=-0.5), reads=[tmpl], writes=[r])
                for c in range(8):
                    sink(c, tc, r)

        def norm_to_h(gi):
            def sink(c, tc, r):
                op("vector", lambda e: e.scalar_tensor_tensor(out=H[c][tc][:], in0=XR[c][tc][:], scalar=gains_t[:, gi, c:c + 1], in1=r[:], op0=ALU.mult, op1=ALU.mult),
                   reads=[XR[c][tc], GAINS, r], writes=[H[c][tc]])
            return sink

        def mixer(l):
            cv = Carver()
            USSM = [[cv.take([8, 64], BF16, f"u{mc}_{tc}") for tc in range(NTC)] for mc in range(4)]
            SBF = [[cv.take([2, 260], BF16, f"sbf{s}_{q4}") for q4 in range(4)] for s in range(2)]
            NTMP = 12
            off_tmps = cv.off
            TMPS = [[cv.take([256], F32, f"st{s}_{i}") for i in range(NTMP)] for s in range(1)]
            off_pb = cv.off
            PBRE = cv.take([8, 4, 32], F32, "pbre")
            PBIM = cv.take([8, 4, 32], F32, "pbim")
            off_wx = cv.off
            WX = cv.take([16, 4, 128], BF16, "wx")
            KD = cv.take([8, 128], BF16, "kd")
            POOLED = [cv.take([512], BF16, f"pooled{i}") for i in range(4)]
            TAIL = [cv.take([16], F32, f"tail{g}") for g in range(4)]
            cva = Carver(); cva.off = off_tmps
            XH = [cva.take([528], F32, f"xh{i}") for i in range(2)]
            SA = cva.take([528], F32, "sa")
            SB_ = cva.take([528], F32, "sb")
            cvb = Carver(); cvb.off = off_wx
            SQB = [cvb.take([512], BF16, f"sqb{i}") for i in range(4)]
            RST = [cvb.take([512], F32, f"rst{i}") for i in range(2)]
            TMPL = cvb.take([512], F32, "tmpl")
            assert cvb.off <= off_wx + 8192
            cvc = Carver(); cvc.off = off_pb
            SGT = [cvc.take([512], F32, f"sgt{i}") for i in range(2)]
            def p16(name):
                return cv.take([16], F32, name)
            DT, AR, ZR, ZI, MAG, Q1, QF, PH, SN, PA, CS, LR, LI, DEN, RDEN, NR, CR, CI, TA, TB, RHO8, F8 = [p16(f"pp{i}") for i in range(22)]
            QI = cv.take([16], I32, "qi")
            PWR = cv.take([9, 16], F32, "pwr")
            PWI = cv.take([9, 16], F32, "pwi")
            BBRE = cv.take([16, 16], F32, "bbre")
            BBIM = cv.take([16, 16], F32, "bbim")
            BT1 = cv.take([16, 16], F32, "bt1")
            CRET = cv.take([16, 32], F32, "cret")
            NCIMT = cv.take([16, 32], F32, "ncimt")
            PBT = cv.take([8, 4, 16], F32, "pbt")
            WCT1 = cv.take([8, 16], F32, "wct1")
            WCT2 = cv.take([8, 16], F32, "wct2")
            DIAGD = cv.take([128], F32, "diagd")
            KTMP = cv.take([128], F32, "ktmp")
            TI = [cv.take([256], I32, f"ti{s}") for s in range(1)]

            V = lambda e: e

            def tt(eng, out, in0, in1, o, reads, writes):
                return op(eng, lambda e: e.tensor_tensor(out=out, in0=in0, in1=in1, op=o), reads=reads, writes=writes)

            load_wbuf(d_win[l])
            load_mixer_small(l)

            rmsnorm(2 * l, SQB, RST, TMPL, norm_to_h(2 * l))

            A_ldt = ssma_t[:, 0, :]
            A_are = ssma_t[:, 1, :]
            A_aim = ssma_t[:, 2, :]
            op("scalar", lambda e: e.activation(out=DT[:], in_=A_ldt, func=AF.Exp), reads=[SSMA], writes=[DT])
            op("vector", lambda e: e.tensor_single_scalar(out=AR[:], in_=A_are, scalar=-1e-4, op=ALU.min), reads=[SSMA], writes=[AR])
            tt("vector", ZR[:], AR[:], DT[:], ALU.mult, [AR, DT], [ZR])
            tt("vector", ZI[:], A_aim, DT[:], ALU.mult, [SSMA, DT], [ZI])
            op("scalar", lambda e: e.activation(out=MAG[:], in_=ZR[:], func=AF.Exp), reads=[ZR], writes=[MAG])

            def sincos(src, scale, SNo, CSo):
                op("vector", lambda e: e.tensor_single_scalar(out=Q1[:], in_=src[:], scalar=scale / TWO_PI, op=ALU.mult), reads=[src], writes=[Q1])
                op("vector", lambda e: e.tensor_copy(out=QI[:], in_=Q1[:]), reads=[Q1], writes=[QI])
                op("vector", lambda e: e.tensor_copy(out=QF[:], in_=QI[:]), reads=[QI], writes=[QF])
                tt("vector", PH[:], Q1[:], QF[:], ALU.subtract, [Q1, QF], [PH])
                if SNo is not None:
                    op("scalar", lambda e: e.activation(out=SNo[:], in_=PH[:], func=AF.Sin, scale=TWO_PI), reads=[PH], writes=[SNo])
                if CSo is not None:
                    op("vector", lambda e: e.scalar_tensor_tensor(out=PA[:], in0=PH[:], scalar=-1.0, in1=PH[:], op0=ALU.mult, op1=ALU.max), reads=[PH], writes=[PA])
                    op("scalar", lambda e: e.activation(out=CSo[:], in_=PA[:], func=AF.Sin, scale=-TWO_PI, bias=HALFPI[:, 0:1]), reads=[PA, HALFPI_T], writes=[CSo])

            sincos(ZI, 1.0, SN, CS)
            tt("vector", LR[:], MAG[:], CS[:], ALU.mult, [MAG, CS], [LR])
            tt("vector", LI[:], MAG[:], SN[:], ALU.mult, [MAG, SN], [LI])
            tt("vector", TA[:], AR[:], AR[:], ALU.mult, [AR], [TA])
            tt("vector", TB[:], A_aim, A_aim, ALU.mult, [SSMA], [TB])
            tt("vector", DEN[:], TA[:], TB[:], ALU.add, [TA, TB], [DEN])
            op("vector", lambda e: e.reciprocal(out=RDEN[:], in_=DEN[:]), reads=[DEN], writes=[RDEN])
            op("vector", lambda e: e.tensor_single_scalar(out=NR[:], in_=LR[:], scalar=-1.0, op=ALU.add), reads=[LR], writes=[NR])
            tt("vector", TA[:], NR[:], AR[:], ALU.mult, [NR, AR], [TA])
            tt("vector", TB[:], LI[:], A_aim, ALU.mult, [LI, SSMA], [TB])
            tt("vector", TA[:], TA[:], TB[:], ALU.add, [TA, TB], [TA])
            tt("vector", CR[:], TA[:], RDEN[:], ALU.mult, [TA, RDEN], [CR])
            tt("vector", TA[:], LI[:], AR[:], ALU.mult, [LI, AR], [TA])
            tt("vector", TB[:], NR[:], A_aim, ALU.mult, [NR, SSMA], [TB])
            tt("vector", TA[:], TA[:], TB[:], ALU.subtract, [TA, TB], [TA])
            tt("vector", CI[:], TA[:], RDEN[:], ALU.mult, [TA, RDEN], [CI])
            b_re = ssmbc_t[:, 0]
            b_im = ssmbc_t[:, 1]
            c_re = ssmbc_t[:, 2]
            c_im = ssmbc_t[:, 3]
            crb = CR[:].unsqueeze(2).to_broadcast([128, 16, 16])
            cib = CI[:].unsqueeze(2).to_broadcast([128, 16, 16])
            tt("vector", BBRE[:], b_re, crb, ALU.mult, [SSMBC, CR], [BBRE])
            tt("vector", BT1[:], b_im, cib, ALU.mult, [SSMBC, CI], [BT1])
            tt("vector", BBRE[:], BBRE[:], BT1[:], ALU.subtract, [BBRE, BT1], [BBRE])
            tt("vector", BBIM[:], b_im, crb, ALU.mult, [SSMBC, CR], [BBIM])
            tt("vector", BT1[:], b_re, cib, ALU.mult, [SSMBC, CI], [BT1])
            tt("vector", BBIM[:], BBIM[:], BT1[:], ALU.add, [BBIM, BT1], [BBIM])
            op("vector", lambda e: e.memset(PWR[:, 0, :], 1.0), writes=[PWR])
            op("vector", lambda e: e.memset(PWI[:, 0, :], 0.0), writes=[PWI])
            for k in range(8):
                tt("vector", TA[:], PWR[:, k, :], LR[:], ALU.mult, [PWR, LR], [TA])
                tt("vector", TB[:], PWI[:, k, :], LI[:], ALU.mult, [PWI, LI], [TB])
                tt("vector", PWR[:, k + 1, :], TA[:], TB[:], ALU.subtract, [TA, TB], [PWR])
                tt("vector", TA[:], PWR[:, k, :], LI[:], ALU.mult, [PWR, LI], [TA])
                tt("vector", TB[:], PWI[:, k, :], LR[:], ALU.mult, [PWI, LR], [TB])
                tt("vector", PWI[:, k + 1, :], TA[:], TB[:], ALU.add, [TA, TB], [PWI])
            op("scalar", lambda e: e.activation(out=RHO8[:], in_=ZR[:], func=AF.Exp, scale=float(J)), reads=[ZR], writes=[RHO8])
            op("vector", lambda e: e.tensor_single_scalar(out=Q1[:], in_=ZI[:], scalar=float(J) / TWO_PI, op=ALU.mult), reads=[ZI], writes=[Q1])
            op("vector", lambda e: e.tensor_copy(out=QI[:], in_=Q1[:]), reads=[Q1], writes=[QI])
            op("vector", lambda e: e.tensor_copy(out=QF[:], in_=QI[:]), reads=[QI], writes=[QF])
            tt("vector", F8[:], Q1[:], QF[:], ALU.subtract, [Q1, QF], [F8])
            op("vector", lambda e: e.memset(CRET[:], 0.0), writes=[CRET])
            op("vector", lambda e: e.memset(NCIMT[:], 0.0), writes=[NCIMT])
            for h2 in range(2):
                ps_ = slice(64 * h2, 64 * h2 + 64)
                cs_ = slice(16 * h2, 16 * h2 + 16)
                op("vector", lambda e, ps_=ps_, cs_=cs_: e.tensor_copy(out=CRET[ps_, :, cs_], in_=c_re[ps_]), reads=[SSMBC], writes=[CRET])
                op("vector", lambda e, ps_=ps_, cs_=cs_: e.tensor_single_scalar(out=NCIMT[ps_, :, cs_], in_=c_im[ps_], scalar=-1.0, op=ALU.mult), reads=[SSMBC], writes=[NCIMT])
            op("vector", lambda e: e.memset(PBRE[:], 0.0), writes=[PBRE])
            op("vector", lambda e: e.memset(PBIM[:], 0.0), writes=[PBIM])
            for s in range(2):
                for q4 in range(4):
                    op("vector", lambda e, s=s, q4=q4: e.memset(SBF[s][q4][:], 0.0), writes=[SBF[s][q4]])

            for tc in range(NTC):
                for mc in [4, 5, 6, 7, 0, 1, 2, 3]:
                    ps = next_ps()
                    for k in range(8):
                        op("tensor", lambda e, ps=ps, k=k, mc=mc, tc=tc: e.matmul(ps[:], lhsT=wbuf_t[:, k * 1024 + mc * 128:k * 1024 + mc * 128 + 128], rhs=H[k][tc][:],
                                                                                   start=(k == 0), stop=(k == 7)),
                           reads=[WBUF, H[k][tc]], writes=[ps], signal=(k == 7))
                    if mc < 4:
                        u = USSM[mc][tc]
                        op("scalar", lambda e, ps=ps, u=u: e.activation(out=u[:], in_=ps[:].rearrange("p (m i) -> p i m", i=8), func=AF.Copy), reads=[ps], writes=[u])
                    else:
                        g = mc - 4
                        w = 2 ** (g + 1)
                        xh = XH[(tc * 4 + g) % 2]
                        op("scalar", lambda e, ps=ps, xh=xh: e.activation(out=xh[:, 16:528], in_=ps[:], func=AF.Copy), reads=[ps], writes=[xh])
                        if tc == 0:
                            op("vector", lambda e, xh=xh: e.memset(xh[:, 0:16], 0.0), writes=[xh])
                        else:
                            op("vector", lambda e, xh=xh, g=g: e.tensor_copy(out=xh[:, 0:16], in_=TAIL[g][:]), reads=[TAIL[g]], writes=[xh])
                        op("vector", lambda e, xh=xh, g=g: e.tensor_copy(out=TAIL[g][:], in_=xh[:, 512:528]), reads=[xh], writes=[TAIL[g]])
                        src = xh
                        bufs = [SA, SB_]
                        sh = 1
                        lo = 1
                        for lev in range(g + 1):
                            dst = bufs[lev % 2]
                            op("vector", lambda e, src=src, dst=dst, sh=sh, lo=lo: e.tensor_tensor(out=dst[:, lo:528], in0=src[:, lo:528], in1=src[:, lo - sh:528 - sh], op=ALU.add),
                               reads=[src], writes=[dst])
                            src = dst
                            sh *= 2
                            lo += sh
                        pl = POOLED[g]
                        op("vector", lambda e, src=src, xh=xh, pl=pl, w=w: e.scalar_tensor_tensor(out=pl[:], in0=src[:, 16:528], scalar=1.0 / w, in1=xh[:, 16:528], op0=ALU.mult, op1=ALU.subtract),
                           reads=[src, xh], writes=[pl])
                        if tc == 0:
                            tmpc = TMPL
                            op("vector", lambda e, src=src, g=g: e.tensor_tensor(out=TMPL[:, 0:16], in0=src[:, 16:32], in1=invc_t[:, g, :], op=ALU.mult), reads=[src, INVC], writes=[TMPL])
                            op("vector", lambda e, xh=xh, pl=pl: e.tensor_tensor(out=pl[:, 0:16], in0=TMPL[:, 0:16], in1=xh[:, 16:32], op=ALU.subtract), reads=[TMPL, xh], writes=[pl])
                for g in range(4):
                    ps = next_ps()
                    op("tensor", lambda e, ps=ps, g=g: e.matmul(ps[:], lhsT=poolw_t[:, g, :], rhs=POOLED[g][:], start=True, stop=True), reads=[POOLW, POOLED[g]], writes=[ps])
                    op("scalar", lambda e, ps=ps, g=g, tc=tc: e.activation(out=H[4 + g][tc][:], in_=ps[:], func=AF.Identity, scale=vec_t[:, l, 1, g:g + 1]), reads=[ps, VEC], writes=[H[4 + g][tc]])

            op("gpsimd", lambda e: e.memset(wbuf_t[:, :], 0.0), writes=[WBUF])
            WC = wbuf_t[:, :].rearrange("p (j q r n) -> p j q r n", j=8, q=4, r=2)

            first_tab_extra = [Tok(P.sem[e_], P.cnt[e_]) for e_ in ("vector", "scalar", "tensor") if P.cnt[e_] > 0]
            for qt in range(4):
                for h2 in range(2):
                    pp = slice(64 * h2, 64 * h2 + 64)
                    cc = slice(16 * h2, 16 * h2 + 16)
                    pr = PWR[pp, 0:8, 4 * qt:4 * qt + 4].unsqueeze(3).to_broadcast([64, 8, 4, 16])
                    pi = PWI[pp, 0:8, 4 * qt:4 * qt + 4].unsqueeze(3).to_broadcast([64, 8, 4, 16])
                    br = BBRE[pp, 4 * qt:4 * qt + 4, :].unsqueeze(1).to_broadcast([64, 8, 4, 16])
                    bi = BBIM[pp, 4 * qt:4 * qt + 4, :].unsqueeze(1).to_broadcast([64, 8, 4, 16])
                    tt("vector", PBRE[pp, :, :, cc], pr, br, ALU.mult, [PWR, BBRE], [PBRE])
                    tt("vector", PBT[pp], pi, bi, ALU.mult, [PWI, BBIM], [PBT])
                    tt("vector", PBRE[pp, :, :, cc], PBRE[pp, :, :, cc], PBT[pp], ALU.subtract, [PBRE, PBT], [PBRE])
                    tt("vector", PBIM[pp, :, :, cc], pr, bi, ALU.mult, [PWR, BBIM], [PBIM])
                    tt("vector", PBT[pp], pi, br, ALU.mult, [PWI, BBRE], [PBT])
                    tt("vector", PBIM[pp, :, :, cc], PBIM[pp, :, :, cc], PBT[pp], ALU.add, [PBIM, PBT], [PBIM])
                for b in range(4):
                    ps = next_ps()
                    for i4 in range(4):
                        idx = 4 * b + i4
                        j = idx // 2
                        reim = idx % 2
                        src = PBRE if reim == 0 else PBIM
                        op("tensor", lambda e, ps=ps, i4=i4, src=src, j=j: e.transpose(ps[:, i4 * 128:(i4 + 1) * 128], src[:, 7 - j, :, :].rearrange("p a b -> p (a b)"), ident_t[:]),
                           reads=[src, IDENT], writes=[ps], signal=(i4 == 3))
                    for q4 in range(4):
                        op("scalar", lambda e, ps=ps, b=b, q4=q4: e.activation(out=WX[:, 4 * b:4 * b + 4, q4, :], in_=ps[:].rearrange("p (a n) -> p a n", a=4), func=AF.Identity,
                                                                                scale=maskq_t[:, q4:q4 + 1]), reads=[ps, MASKQ], writes=[WX],
                           extra=(first_tab_extra if (qt == 0 and b == 0 and q4 == 0) else ()))
                psk = [next_ps(), next_ps()]
                for d in range(8):
                    pk = psk[d // 4]
                    oc = slice((d % 4) * 128, (d % 4) * 128 + 128)
                    op("tensor", lambda e, pk=pk, oc=oc, d=d, qt=qt: e.matmul(pk[:, oc], lhsT=PBRE[:, d, :, :].rearrange("p a b -> p (a b)"),
                                                                       rhs=CRET[:, 4 * qt:4 * qt + 4, :].rearrange("p a b -> p (a b)"), start=True, stop=False),
                       reads=[PBRE, CRET], writes=[pk], signal=False)
                    op("tensor", lambda e, pk=pk, oc=oc, d=d, qt=qt: e.matmul(pk[:, oc], lhsT=PBIM[:, d, :, :].rearrange("p a b -> p (a b)"),
                                                                       rhs=NCIMT[:, 4 * qt:4 * qt + 4, :].rearrange("p a b -> p (a b)"), start=False, stop=True),
                       reads=[PBIM, NCIMT], writes=[pk], signal=(d % 4 == 3))
                op("vector", lambda e, qt=qt: e.tensor_scalar(out=DIAGD[:], in0=ident_t[:], scalar1=vec_t[:, l, 2, qt:qt + 1], scalar2=None, op0=ALU.mult), reads=[IDENT, VEC], writes=[DIAGD])
                tt("vector", KTMP[:], psk[0][:, 0:128], maskb_t[:, 0, :], ALU.mult, [psk[0], MASKB], [KTMP])
                tt("vector", KD[:, 0, :], KTMP[:], DIAGD[:], ALU.add, [KTMP, DIAGD], [KD])
                tt("vector", KD[:, 1:4, :], psk[0][:, 128:512].rearrange("p (a n) -> p a n", a=3), maskb_t[:, 0:3, :], ALU.mult, [psk[0], MASKB], [KD])
                tt("vector", KD[:, 4:8, :], psk[1][:].rearrange("p (a n) -> p a n", a=4), maskb_t[:], ALU.mult, [psk[1], MASKB], [KD])
                for q4 in range(4):
                    q = 4 * qt + q4
                    for h2 in range(2):
                        pp = slice(64 * h2, 64 * h2 + 64)
                        cc = slice(32 * q4 + 16 * h2, 32 * q4 + 16 * h2 + 16)
                        crb_ = c_re[pp, q, :].unsqueeze(1).to_broadcast([64, 8, 16])
                        cib_ = c_im[pp, q, :].unsqueeze(1).to_broadcast([64, 8, 16])
                        prb = PWR[pp, 1:9, q].unsqueeze(2).to_broadcast([64, 8, 16])
                        pib = PWI[pp, 1:9, q].unsqueeze(2).to_broadcast([64, 8, 16])
                        tt("vector", WCT1[pp], crb_, prb, ALU.mult, [SSMBC, PWR], [WCT1])
                        tt("vector", WCT2[pp], cib_, pib, ALU.mult, [SSMBC, PWI], [WCT2])
                        tt("vector", WC[pp, :, q4, 0, cc], WCT1[pp], WCT2[pp], ALU.subtract, [WCT1, WCT2], [WBUF])
                        tt("vector", WCT1[pp], crb_, pib, ALU.mult, [SSMBC, PWI], [WCT1])
                        tt("vector", WCT2[pp], cib_, prb, ALU.mult, [SSMBC, PWR], [WCT2])
                        op("vector", lambda e, pp=pp, q4=q4, cc=cc: e.scalar_tensor_tensor(out=WC[pp, :, q4, 1, cc], in0=WCT1[pp], scalar=-1.0, in1=WCT2[pp], op0=ALU.mult, op1=ALU.subtract),
                           reads=[WCT1, WCT2], writes=[WBUF])
                sset = qt % 2
                for q4 in range(4):
                    q = 4 * qt + q4
                    psx = next_ps()
                    for reim in range(2):
                        for j in range(8):
                            op("tensor", lambda e, psx=psx, reim=reim, j=j, q4=q4, qt=qt: e.matmul(
                                psx[:, reim * 256:(reim + 1) * 256].rearrange("p (a m) -> p a m", a=4),
                                lhsT=WX[:, 2 * j + reim, q4, :],
                                rhs=scr_u[qt][:, :, j, :], start=(j == 0), stop=(j == 7)),
                               reads=[WX] + USSM[qt], writes=[psx], signal=(reim == 1 and j == 7))
                    tm = TMPS[0]
                    PHr, TFp, SN0, SN1, CS0, CS1, T1, T2, VRE, VIM, RRE, RIM = tm
                    SNt = SN0 if q % 2 == 0 else SN1
                    CSt = CS0 if q % 2 == 0 else CS1
                    tix = TI[0]
                    ex_ = first_tab_extra if (qt == 0 and q4 == 0) else ()
                    op("vector", lambda e, q=q: e.tensor_scalar(out=PHr[:], in0=iota1_t[:], scalar1=F8[:, q:q + 1], scalar2=None, op0=ALU.mult), reads=[IOTA1, F8], writes=[PHr], extra=ex_)
                    op("vector", lambda e: e.tensor_copy(out=tix[:], in_=PHr[:]), reads=[PHr], writes=[tix])
                    op("vector", lambda e: e.tensor_copy(out=TFp[:], in_=tix[:]), reads=[tix], writes=[TFp])
                    op("vector", lambda e: e.tensor_tensor(out=PHr[:], in0=PHr[:], in1=TFp[:], op=ALU.subtract), reads=[PHr, TFp], writes=[PHr])
                    op("vector", lambda e: e.scalar_tensor_tensor(out=TFp[:], in0=PHr[:], scalar=-1.0, in1=PHr[:], op0=ALU.mult, op1=ALU.max), reads=[PHr], writes=[TFp])
                    op("scalar", lambda e, SNt=SNt: e.activation(out=SNt[:], in_=PHr[:], func=AF.Sin, scale=TWO_PI), reads=[PHr], writes=[SNt])
                    op("scalar", lambda e, CSt=CSt: e.activation(out=CSt[:], in_=TFp[:], func=AF.Sin, scale=-TWO_PI, bias=HALFPI[:, 0:1]), reads=[TFp, HALFPI_T], writes=[CSt])
                    xre = psx[:, 0:256]
                    xim = psx[:, 256:512]
                    tt("vector", T1[:], xre, CSt[:], ALU.mult, [psx, CSt], [T1])
                    tt("vector", T2[:], xim, SNt[:], ALU.mult, [psx, SNt], [T2])
                    tt("vector", VRE[:], T1[:], T2[:], ALU.add, [T1, T2], [VRE])
                    tt("vector", T1[:], xim, CSt[:], ALU.mult, [psx, CSt], [T1])
                    tt("vector", T2[:], xre, SNt[:], ALU.mult, [psx, SNt], [T2])
                    tt("vector", VIM[:], T1[:], T2[:], ALU.subtract, [T1, T2], [VIM])
                    rho = RHO8[:, q:q + 1].to_broadcast([128, 256])
                    op("vector", lambda e, VRE=VRE, RRE=RRE, rho=rho: e.tensor_tensor_scan(out=RRE[:], data0=rho, data1=VRE[:], initial=0.0, op0=ALU.mult, op1=ALU.add), reads=[VRE, RHO8], writes=[RRE])
                    op("vector", lambda e, VIM=VIM, RIM=RIM, rho=rho: e.tensor_tensor_scan(out=RIM[:], data0=rho, data1=VIM[:], initial=0.0, op0=ALU.mult, op1=ALU.add), reads=[VIM, RHO8], writes=[RIM])
                    sb = SBF[sset][q4]
                    tt("vector", T1[:], RRE[:], CSt[:], ALU.mult, [RRE, CSt], [T1])
                    tt("vector", T2[:], RIM[:], SNt[:], ALU.mult, [RIM, SNt], [T2])
                    tt("vector", sb[:, 0, 1:257], T1[:], T2[:], ALU.subtract, [T1, T2], [sb])
                    tt("vector", T1[:], RRE[:], SNt[:], ALU.mult, [RRE, SNt], [T1])
                    tt("vector", T2[:], RIM[:], CSt[:], ALU.mult, [RIM, CSt], [T2])
                    tt("vector", sb[:, 1, 1:257], T1[:], T2[:], ALU.add, [T1, T2], [sb])
                for tc in range(NTC):
                    psy = next_ps()
                    for j in range(8):
                        oc = slice(j * 64, j * 64 + 64)
                        first = True
                        for q4 in range(4):
                            for reim in range(2):
                                op("tensor", lambda e, psy=psy, oc=oc, j=j, q4=q4, reim=reim, tc=tc, first=first, sset=sset: e.matmul(
                                    psy[:, oc], lhsT=WC[:, j, q4, reim, :], rhs=SBF[sset][q4][:, reim, 64 * tc:64 * tc + 64], start=first, stop=False),
                                   reads=[WBUF, SBF[sset][q4]], writes=[psy], signal=False)
                                first = False
                        for i in range(j + 1):
                            op("tensor", lambda e, psy=psy, oc=oc, j=j, i=i, tc=tc, qt=qt: e.matmul(psy[:, oc], lhsT=KD[:, j - i, :], rhs=USSM[qt][tc][:, i, :], start=False, stop=(i == j)),
                               reads=[KD, USSM[qt][tc]], writes=[psy], signal=(j == 7 and i == j))
                    op("scalar", lambda e, psy=psy, tc=tc, qt=qt: e.activation(out=H[qt][tc][:].rearrange("p (m j) -> p m j", j=8), in_=psy[:].rearrange("p (j m) -> p m j", j=8),
                                                                        func=AF.Gelu_apprx_tanh), reads=[psy], writes=[H[qt][tc]])

            if dbg and l == 0:
                P.barrier()
                P.dma("sync", lambda e: e.dma_start(out=d_dbgw, in_=wbuf_t[:, :]), g_out)
                for c in range(8):
                    P.dma("sync", lambda e, c=c: e.dma_start(out=d_dbgh[1, :, c, :], in_=h_t[:, c, :]), g_out)
                P.barrier()
            for tc in range(NTC):
                pss = [next_ps() for _ in range(4)]
                for mo in range(4):
                    for k in range(4):
                        op("tensor", lambda e, mo=mo, k=k, tc=tc, pss=pss: e.matmul(pss[mo][:], lhsT=wglu_t[:, k, mo * 128:(mo + 1) * 128], rhs=H[k][tc][:], start=(k == 0), stop=(k == 3)),
                           reads=[WGLU, H[k][tc]], writes=[pss[mo]], signal=(k == 3))
                for mo in range(4):
                    sg = SGT[mo % 2]
                    op("scalar", lambda e, mo=mo, sg=sg, pss=pss: e.activation(out=sg[:], in_=pss[mo][:], func=AF.Sigmoid, bias=vec_t[:, l, 0, mo:mo + 1]), reads=[pss[mo], VEC], writes=[sg])
                    tt("vector", H[mo][tc][:], H[mo][tc][:], sg[:], ALU.mult, [H[mo][tc], sg], [H[mo][tc]])

            dumph(l)
            if dbg and l == 0:
                P.barrier()
                P.dma("sync", lambda e: e.dma_start(out=d_dbgs, in_=scr_t[:, :]), g_out)
                P.barrier()
            load_wbuf(d_wout[l])
            for tc in range(NTC):
                for mc in range(8):
                    ps = next_ps()
                    for k in range(8):
                        op("tensor", lambda e, ps=ps, k=k, mc=mc, tc=tc: e.matmul(ps[:], lhsT=wbuf_t[:, k * 1024 + mc * 128:k * 1024 + mc * 128 + 128], rhs=H[k][tc][:],
                                                                                   start=(k == 0), stop=(k == 7)),
                           reads=[WBUF, H[k][tc]], writes=[ps], signal=(k == 7))
                    if dbg and False:
                        pass
                    tt("vector", XR[mc][tc][:], ps[:], XR[mc][tc][:], ALU.add, [ps, XR[mc][tc]], [XR[mc][tc]])

        scr_u = {}

        halfpi_t = P.sbuf("halfpi", [128, 1], F32)
        HALFPI_T = T(halfpi_t[:], "halfpi")
        HALFPI = halfpi_t
        op("vector", lambda e: e.memset(halfpi_t[:], math.pi / 2.0), writes=[HALFPI_T])
        for mc in range(4):
            base = mc * (4 * 8 * 64)
            scr_u[mc] = scr_t[:, base:base + 4 * 8 * 64].rearrange("p (t i m) -> p t i m", t=4, i=8)

        class FFNState:
            pass

        def ffn_setup():
            cv = Carver()
            st = FFNState()
            st.GU = [cv.take([2, 8, 128], BF16, f"gu{s}") for s in range(NS)]
            st.DN = [cv.take([1024], BF16, f"dn{s}") for s in range(NS)]
            st.A = [[cv.take([512], BF16, f"a{p_}_{i}") for i in range(4)] for p_ in range(2)]
            st.SG = [cv.take([512], BF16, f"sg{i}") for i in range(2)]
            off_a = cv.off - 4096 - 1024
            off_g = cv.off
            st.GATEBC = [[cv.take([512], F32, f"gbc{p_}_{tc}") for tc in range(NTC)] for p_ in range(2)]
            cvg = Carver(); cvg.off = off_g
            st.RSTD = [cvg.take([512], F32, f"rstd{tc}") for tc in range(NTC)]
            st.SQB = [cvg.take([512], BF16, f"fsqb{i}") for i in range(4)]
            st.TMPL = cvg.take([512], F32, "ftmpl")
            assert cvg.off <= cv.off
            cva_ = Carver(); cva_.off = off_a
            st.HN32 = cva_.take([L], F32, "hn32")
            st.LG = cv.take([16, 8], F32, "lg")
            st.LG2 = cv.take([16, 8], F32, "lg2")
            st.EQ = cv.take([16, 8], F32, "eq")
            st.G = cv.take([16, 8], F32, "gates")
            st.M1 = cv.take([16], F32, "m1")
            st.M2 = cv.take([16], F32, "m2")
            st.DG = [cv.take([128], F32, f"dg{i}") for i in range(4)]
            st.issued = 0
            st.chunks = []
            return st

        def ffn_issue_loads(st, upto):
            while st.issued < min(upto, len(st.chunks)):
                ci = st.issued
                gu_src, dn_src = st.chunks[ci]
                s = ci % NS
                P.dma("gpsimd", lambda e, s=s, gu_src=gu_src: e.dma_start(out=st.GU[s][:], in_=gu_src), P.grp(f"gu{s}"), writes=[st.GU[s]])
                P.dma("gpsimd", lambda e, s=s, dn_src=dn_src: e.dma_start(out=st.DN[s][:], in_=dn_src), P.grp(f"dn{s}"), writes=[st.DN[s]])
                st.issued += 1

        def ffn_run(st, base_ci, gate_set=None, mid_hook=None):
            f0 = 0
            for gi_, gsz in enumerate(GROUPS):
                if gi_ == 3 and mid_hook is not None:
                    mid_hook()
                ffn_issue_loads(st, base_ci + f0 + NS)
                for tc in range(NTC):
                    par = tc % 2
                    for fl in range(gsz):
                        ci = base_ci + f0 + fl
                        s = ci % NS
                        psg = next_ps()
                        psu = next_ps()
                        for k in range(8):
                            op("tensor", lambda e, psg=psg, s=s, k=k, tc=tc: e.matmul(psg[:], lhsT=st.GU[s][:, 0, k, :], rhs=H[k][tc][:], start=(k == 0), stop=(k == 7)),
                               reads=[st.GU[s], H[k][tc]], writes=[psg], signal=(k == 7))
                        for k in range(8):
                            op("tensor", lambda e, psu=psu, s=s, k=k, tc=tc: e.matmul(psu[:], lhsT=st.GU[s][:, 1, k, :], rhs=H[k][tc][:], start=(k == 0), stop=(k == 7)),
                               reads=[st.GU[s], H[k][tc]], writes=[psu], signal=(k == 7))
                        sg = st.SG[(tc * 4 + fl) % 2]
                        a = st.A[par][fl]
                        op("scalar", lambda e, psg=psg, sg=sg: e.activation(out=sg[:], in_=psg[:], func=AF.Silu), reads=[psg], writes=[sg])
                        if gate_set is not None:
                            gb = st.GATEBC[gate_set][tc]
                            op("gpsimd", lambda e, sg=sg, gb=gb: e.tensor_tensor(out=sg[:], in0=sg[:], in1=gb[:], op=ALU.mult), reads=[sg, gb], writes=[sg])
                        op("vector", lambda e, psu=psu, sg=sg, a=a: e.tensor_tensor(out=a[:], in0=psu[:], in1=sg[:], op=ALU.mult), reads=[psu, sg], writes=[a])
                    for mo in range(8):
                        ps = next_ps()
                        for fl in range(gsz):
                            ci = base_ci + f0 + fl
                            s = ci % NS
                            op("tensor", lambda e, ps=ps, s=s, mo=mo, fl=fl, par=par, gsz=gsz: e.matmul(ps[:], lhsT=st.DN[s][:, mo * 128:(mo + 1) * 128], rhs=st.A[par][fl][:],
                                                                                                   start=(fl == 0), stop=(fl == gsz - 1)),
                               reads=[st.DN[s], st.A[par][fl]], writes=[ps], signal=(fl == gsz - 1))
                        xr = XR[mo][tc]
                        op("vector", lambda e, ps=ps, xr=xr: e.tensor_tensor(out=xr[:], in0=ps[:], in1=xr[:], op=ALU.add), reads=[ps, xr], writes=[xr])
                f0 += gsz

        def dump(idx):
            if not dbg:
                return
            for c in range(8):
                P.dma("sync", lambda e, c=c: e.dma_start(out=d_dbg[idx, :, c, :], in_=xres_t[:, c, :]), g_out, reads=XR[c])

        def dumph(idx):
            if not dbg:
                return
            for c in range(8):
                P.dma("sync", lambda e, c=c: e.dma_start(out=d_dbgh[idx, :, c, :], in_=h_t[:, c, :]), g_out, reads=H[c])

        if "mix0" in stages:
            mixer(0)
            dump(0)
        P.barrier()
        if "ffn0" in stages:
            st = ffn_setup()
            st.chunks = [(d_fgu[f], d_fdn[f]) for f in range(NF)]
            ffn_issue_loads(st, NS)
            rmsnorm(1, st.SQB, None, st.TMPL, norm_to_h(1), rstd_keep=st.RSTD)
            ffn_run(st, 0, None)
            dump(1)
        P.barrier()
        if "mix1" in stages:
            mixer(1)
            dump(2)
        P.barrier()
        if "ffn1" in stages:
            NSM = 4
            GROUPS_S = [2] * 11
            cv = Carver()
            st = FFNState()
            st.GU = [cv.take([2, 8, 128], BF16, f"mgu{s_}") for s_ in range(NSM)]
            st.DN = [cv.take([1024], BF16, f"mdn{s_}") for s_ in range(NSM)]
            off_a = cv.off
            SW = 640
            st.A = [[cv.take([SW], BF16, f"ma{p_}_{i}") for i in range(2)] for p_ in range(2)]
            st.SG = [T(wglu_t[:, :, :].rearrange("p a b -> p (a b)")[:, i * SW:(i + 1) * SW], f"msg{i}") for i in range(2)]
            off_hc = cv.off
            HC = cv.take([8, SW], BF16, "hc")
            YSM = cv.take([5, 1024], BF16, "ysm")
            off_sel = cv.off
            SEL = [cv.take([SW], BF16, f"sel{i}") for i in range(4)]
            off_selt = cv.off
            SELT = [cv.take([512], BF16, f"selt{i}") for i in range(5)]
            cvs = Carver(); cvs.off = off_sel
            st.STG = [cvs.take([512], BF16, f"stg{i}") for i in range(2)]
            POSBC = [cv.take([512], F32, f"posbc{tc}") for tc in range(NTC)]
            st.GATEBC = [[cv.take([512], F32, f"mgbc{tc}") for tc in range(NTC)]]
            IOTA512 = cv.take([SW], F32, "iota512")
            st.LG = cv.take([16, 8], F32, "lg")
            st.LG2 = cv.take([16, 8], F32, "lg2")
            st.EQ = cv.take([16, 8], F32, "eq")
            st.G = cv.take([16, 8], F32, "gates")
            POSM = T(st.LG2.ap, "posm")
            TOT = T(st.LG.ap, "tot")
            OFFS = cv.take([16, 8], F32, "offs")
            st.M1 = cv.take([16], F32, "m1")
            st.M2 = cv.take([16], F32, "m2")
            NE = cv.take([8], F32, "ne")
            FL = cv.take([8, 4], F32, "fl")
            FLI = cv.take([8, 4], I32, "fli")
            cvd = Carver(); cvd.off = off_selt
            st.DG = [cvd.take([128], F32, f"dg{i}") for i in range(2)]
            UT = T(poolw_t[:, :, :].rearrange("p a b -> p (a b)")[:, 0:256].bitcast(F32), "ut")
            IDENTB = cv.take([128], BF16, "identb")
            cvt = Carver(); cvt.off = off_a
            TMPS_ = cvt.take([512], F32, "tmps")
            cvn = Carver(); cvn.off = off_hc
            st.RSTD = [cvn.take([512], F32, f"mrstd{tc}") for tc in range(NTC)]
            st.SQB = [cvn.take([512], BF16, f"msqb{i}") for i in range(4)]
            st.TMPL = cvn.take([512], F32, "mtmpl")
            assert cvn.off <= off_hc + 8 * SW + 5120
            cvh = Carver(); cvh.off = 0
            st.HN32 = cvh.take([L], F32, "mhn32")
            YACC = [T(wbuf_t[:, :].bitcast(F32).rearrange("p (m n) -> p m n", m=8)[:, m, :], f"yacc{m}") for m in range(8)]
            YACC2 = T(ssmbc_t[:, :, :, :].rearrange("p a b c -> p (a b c)").rearrange("p (m n) -> p m n", m=8), "yacc2")
            hT_view = h_t[:, :, :].rearrange("p a b -> p (a b)").rearrange("p (k c) -> p k c", k=16)
            HTK = [T(hT_view[:, k, :], f"ht{k}") for k in range(16)]

            op("gpsimd", lambda e: e.iota(IOTA512[:], [[1, SW]], base=0, channel_multiplier=0, allow_small_or_imprecise_dtypes=True), writes=[IOTA512])
            op("vector", lambda e: e.tensor_scalar(out=UT[:], in0=iotaf_t[:], scalar1=iotap_t[:, 0:1], scalar2=None, op0=ALU.is_gt), reads=[IOTAF, IOTAP], writes=[UT])
            op("vector", lambda e: e.tensor_copy(out=IDENTB[:], in_=ident_t[:]), reads=[IDENT], writes=[IDENTB])

            cnt_ = [0]

            def sink_t(c, tc, r):
                stg = st.STG[cnt_[0] % 2]
                cnt_[0] += 1
                op("vector", lambda e: e.scalar_tensor_tensor(out=stg[:], in0=XR[c][tc][:], scalar=gains_t[:, 3, c:c + 1], in1=r[:], op0=ALU.mult, op1=ALU.mult),
                   reads=[XR[c][tc], GAINS, r], writes=[stg])
                ps = next_ps()
                psb = ps[:].bitcast(BF16)
                for j in range(4):
                    op("tensor", lambda e, j=j: e.transpose(psb[:, j * 128:(j + 1) * 128], stg[:, j * 128:(j + 1) * 128], IDENTB[:]), reads=[stg, IDENTB], writes=[ps], signal=(j == 3))
                op("scalar", lambda e: e.activation(out=hT_view[:, 4 * tc:4 * tc + 4, c * 128:(c + 1) * 128], in_=psb[:, 0:512].rearrange("p (a n) -> p a n", a=4), func=AF.Copy),
                   reads=[ps], writes=[HTK[4 * tc + j] for j in range(4)])

            rmsnorm(3, st.SQB, None, st.TMPL, sink_t, rstd_keep=st.RSTD)

            psl = next_ps()
            hn_v = st.HN32[:].rearrange("p (b c n) -> p b c n", b=2, c=8)
            g3b = gains_t[:, 3, :].unsqueeze(2).to_broadcast([128, 8, 128])
            for tl in range(16):
                tc_, o_ = tl // 4, (tl % 4) * 128
                hv = hn_v[:, tl % 2]
                op("vector", lambda e, hv=hv, tl=tl: e.tensor_tensor(out=hv, in0=xres_t[:, :, tl * 128:(tl + 1) * 128], in1=g3b, op=ALU.mult),
                   reads=[XR[c][tc_] for c in range(8)] + [GAINS], writes=[st.HN32])
                op("vector", lambda e, hv=hv, tc_=tc_, o_=o_: e.tensor_tensor(out=hv, in0=hv, in1=st.RSTD[tc_][:, o_:o_ + 128].unsqueeze(1).to_broadcast([128, 8, 128]), op=ALU.mult),
                   reads=[st.RSTD[tc_], st.HN32], writes=[st.HN32])
                for c in range(8):
                    op("tensor", lambda e, hv=hv, c=c, tl=tl: e.matmul(psl[:, tl * 8:(tl + 1) * 8], lhsT=hv[:, c, :], rhs=router_t[:, c, :], start=(c == 0), stop=(c == 7)),
                       reads=[st.HN32, ROUTER], writes=[psl], signal=(c == 7))
            lg3 = psl[:, 0:128].rearrange("p (t e) -> p t e", e=8)
            op("vector", lambda e: e.tensor_copy(out=st.LG[:], in_=lg3), reads=[psl], writes=[st.LG])
            op("vector", lambda e: e.tensor_reduce(out=st.M1[:], in_=st.LG[:], axis=AX.X, op=ALU.max), reads=[st.LG], writes=[st.M1])
            m1b = st.M1[:].unsqueeze(2).to_broadcast([128, 16, 8])
            op("vector", lambda e: e.tensor_tensor(out=st.EQ[:], in0=st.LG[:], in1=m1b, op=ALU.is_equal), reads=[st.LG, st.M1], writes=[st.EQ])
            op("vector", lambda e: e.scalar_tensor_tensor(out=st.LG2[:], in0=st.EQ[:], scalar=-1e30, in1=st.LG[:], op0=ALU.mult, op1=ALU.add), reads=[st.EQ, st.LG], writes=[st.LG2])
            op("vector", lambda e: e.tensor_reduce(out=st.M2[:], in_=st.LG2[:], axis=AX.X, op=ALU.max), reads=[st.LG2], writes=[st.M2])
            m2b = st.M2[:].unsqueeze(2).to_broadcast([128, 16, 8])
            op("vector", lambda e: e.tensor_tensor(out=st.EQ[:], in0=st.LG[:], in1=m2b, op=ALU.is_ge), reads=[st.LG, st.M2], writes=[st.EQ])
            op("vector", lambda e: e.tensor_tensor(out=st.LG2[:], in0=st.LG[:], in1=m1b, op=ALU.subtract), reads=[st.LG, st.M1], writes=[st.LG2])
            op("scalar", lambda e: e.activation(out=st.LG2[:], in_=st.LG2[:], func=AF.Exp), reads=[st.LG2], writes=[st.LG2])
            op("vector", lambda e: e.tensor_tensor(out=st.LG2[:], in0=st.LG2[:], in1=st.EQ[:], op=ALU.mult), reads=[st.LG2, st.EQ], writes=[st.LG2])
            op("vector", lambda e: e.tensor_reduce(out=st.M1[:], in_=st.LG2[:], axis=AX.X, op=ALU.add), reads=[st.LG2], writes=[st.M1])
            op("vector", lambda e: e.reciprocal(out=st.M1[:], in_=st.M1[:]), reads=[st.M1], writes=[st.M1])
            op("vector", lambda e: e.tensor_tensor(out=st.G[:], in0=st.LG2[:], in1=m1b, op=ALU.mult), reads=[st.LG2, st.M1], writes=[st.G])

            psw = next_ps()
            eq2 = st.EQ[:].rearrange("p t e -> p (t e)")
            op("tensor", lambda e: e.matmul(psw[:, 0:128], lhsT=UT[:], rhs=eq2, start=True, stop=True), reads=[UT, st.EQ], writes=[psw])
            op("tensor", lambda e: e.matmul(psw[:, 128:256], lhsT=onesf_t[:], rhs=eq2, start=True, stop=True), reads=[ONESF, st.EQ], writes=[psw])
            op("vector", lambda e: e.tensor_copy(out=TOT[:], in_=psw[:, 128:256].rearrange("p (t e) -> p t e", e=8)), reads=[psw], writes=[TOT])
            op("vector", lambda e: e.memset(OFFS[:, 0, :], 0.0), writes=[OFFS])
            for k in range(15):
                op("vector", lambda e, k=k: e.tensor_tensor(out=OFFS[:, k + 1, :], in0=OFFS[:, k, :], in1=TOT[:, k, :], op=ALU.add), reads=[OFFS, TOT], writes=[OFFS])
            op("vector", lambda e: e.tensor_tensor(out=NE[:], in0=OFFS[:, 15, :], in1=TOT[:, 15, :], op=ALU.add), reads=[OFFS, TOT], writes=[NE])
            op("vector", lambda e: e.tensor_tensor(out=POSM[:], in0=psw[:, 0:128].rearrange("p (t e) -> p t e", e=8), in1=OFFS[:], op=ALU.add), reads=[psw, OFFS], writes=[POSM])
            op("vector", lambda e: e.tensor_single_scalar(out=POSM[:], in_=POSM[:], scalar=1.0e6, op=ALU.add), reads=[POSM], writes=[POSM])
            op("vector", lambda e: e.tensor_tensor(out=POSM[:], in0=POSM[:], in1=st.EQ[:], op=ALU.mult), reads=[POSM, st.EQ], writes=[POSM])
            op("vector", lambda e: e.tensor_single_scalar(out=POSM[:], in_=POSM[:], scalar=-1.0e6, op=ALU.add), reads=[POSM], writes=[POSM])
            for cch in range(4):
                op("vector", lambda e, cch=cch: e.tensor_single_scalar(out=FL[:, :, cch], in_=NE[:], scalar=float(0 if cch == 0 else SW + 512 * (cch - 1)) + 0.5, op=ALU.is_gt), reads=[NE], writes=[FL])
            op("vector", lambda e: e.tensor_copy(out=FLI[:], in_=FL[:]), reads=[FL], writes=[FLI])

            def build_bc(src, dst, ex):
                for tc in range(NTC):
                    ps = next_ps()
                    for t4 in range(4):
                        tl = tc * 4 + t4
                        dg = st.DG[t4 % 2]
                        op("vector", lambda e, dg=dg, tl=tl: e.tensor_scalar(out=dg[:], in0=ident_t[:], scalar1=src[:, tl, ex:ex + 1], scalar2=None, op0=ALU.mult), reads=[IDENT, src], writes=[dg])
                        op("tensor", lambda e, ps=ps, dg=dg, t4=t4: e.matmul(ps[:, t4 * 128:(t4 + 1) * 128], lhsT=onesf_t[:], rhs=dg[:], start=True, stop=True),
                           reads=[ONESF, dg], writes=[ps], signal=True)
                    gb = dst[tc]
                    op("scalar", lambda e, ps=ps, gb=gb: e.activation(out=gb[:], in_=ps[:], func=AF.Copy), reads=[ps], writes=[gb])

            gci = [0]

            pre_state = {}

            def prefetch_first(ex):
                slot_of = [(gci[0] + f) % NSM for f in range(NF)]
                pre_state[ex] = (gci[0], slot_of)
                gci[0] += NF
                ex_dep = [FLI.last_w]
                for f in range(NSM):
                    s_ = slot_of[f]
                    gu_src, dn_src = d_mgu[ex * NF + f], d_mdn[ex * NF + f]
                    P.dma("gpsimd", lambda e, s_=s_, gu_src=gu_src: e.dma_start(out=st.GU[s_][:], in_=gu_src), P.grp(f"mgu{s_}"), writes=[st.GU[s_]], extra=ex_dep)
                    P.dma("gpsimd", lambda e, s_=s_, dn_src=dn_src: e.dma_start(out=st.DN[s_][:], in_=dn_src), P.grp(f"mdn{s_}"), writes=[st.DN[s_]], extra=ex_dep)

            def chunk_pass(ex, cch, flag_ap, mid_hook=None):
                base = 0 if cch == 0 else SW + 512 * (cch - 1)
                has_b = (cch == 0)
                nsl = SW if has_b else 512
                nst = 5 if has_b else 4
                loads = [(d_mgu[ex * NF + f], d_mdn[ex * NF + f]) for f in range(NF)]
                if cch == 0 and ex in pre_state:
                    slot_of = pre_state[ex][1]
                    issued = [NSM]
                else:
                    slot_of = [(gci[0] + f) % NSM for f in range(NF)]
                    gci[0] += NF
                    issued = [0]
                P.cond_begin(flag_ap, FLI)

                def issue(upto):
                    while issued[0] < min(upto, NF):
                        f = issued[0]
                        s_ = slot_of[f]
                        gu_src, dn_src = loads[f]
                        P.dma("gpsimd", lambda e, s_=s_, gu_src=gu_src: e.dma_start(out=st.GU[s_][:], in_=gu_src), P.grp(f"mgu{s_}"), writes=[st.GU[s_]])
                        P.dma("gpsimd", lambda e, s_=s_, dn_src=dn_src: e.dma_start(out=st.DN[s_][:], in_=dn_src), P.grp(f"mdn{s_}"), writes=[st.DN[s_]])
                        issued[0] += 1

                issue(NSM)
                for half in range(2):
                    pss = [next_ps() for _ in range(4)]
                    psbl = [next_ps() for _ in range(4)] if has_b else None
                    for k in range(16):
                        sel = SEL[k % 4]
                        op("vector", lambda e, sel=sel, k=k: e.tensor_scalar(out=sel[:, 0:nsl], in0=IOTA512[:, 0:nsl], scalar1=float(base), scalar2=POSM[:, k, ex:ex + 1], op0=ALU.add, op1=ALU.is_equal),
                           reads=[IOTA512, POSM], writes=[sel])
                        for mi in range(4):
                            m = half * 4 + mi
                            op("tensor", lambda e, sel=sel, k=k, mi=mi, m=m, pss=pss: e.matmul(pss[mi][:], lhsT=HTK[k][:, m * 128:(m + 1) * 128], rhs=sel[:, 0:512], start=(k == 0), stop=(k == 15)),
                               reads=[HTK[k], sel], writes=[pss[mi]], signal=(mi == 3 and not has_b))
                        if has_b:
                            for mi in range(4):
                                m = half * 4 + mi
                                op("tensor", lambda e, sel=sel, k=k, mi=mi, m=m, psbl=psbl: e.matmul(psbl[mi][:, 0:128], lhsT=HTK[k][:, m * 128:(m + 1) * 128], rhs=sel[:, 512:SW],
                                                                                                      start=(k == 0), stop=(k == 15)),
                                   reads=[HTK[k], sel], writes=[psbl[mi]], signal=(mi == 3))
                    for mi in range(4):
                        m = half * 4 + mi
                        op("scalar", lambda e, mi=mi, m=m, pss=pss: e.activation(out=HC[:, m, 0:512], in_=pss[mi][:], func=AF.Copy), reads=[pss[mi]], writes=[HC])
                    if has_b:
                        for mi in range(4):
                            m = half * 4 + mi
                            op("scalar", lambda e, mi=mi, m=m, psbl=psbl: e.activation(out=HC[:, m, 512:SW], in_=psbl[mi][:, 0:128], func=AF.Copy), reads=[psbl[mi]], writes=[HC])
                f0 = 0
                for gi_, gsz in enumerate(GROUPS_S):
                    issue(f0 + NSM)
                    par = gi_ % 2
                    for fl in range(gsz):
                        s_ = slot_of[f0 + fl]
                        psg = next_ps()
                        psu = next_ps()
                        psb = next_ps() if has_b else None
                        for k in range(8):
                            op("tensor", lambda e, psg=psg, s_=s_, k=k: e.matmul(psg[:], lhsT=st.GU[s_][:, 0, k, :], rhs=HC[:, k, 0:512], start=(k == 0), stop=(k == 7)),
                               reads=[st.GU[s_], HC], writes=[psg], signal=(k == 7))
                        for k in range(8):
                            op("tensor", lambda e, psu=psu, s_=s_, k=k: e.matmul(psu[:], lhsT=st.GU[s_][:, 1, k, :], rhs=HC[:, k, 0:512], start=(k == 0), stop=(k == 7)),
                               reads=[st.GU[s_], HC], writes=[psu], signal=(k == 7))
                        if has_b:
                            for r_ in range(2):
                                for k in range(8):
                                    op("tensor", lambda e, psb=psb, s_=s_, k=k, r_=r_: e.matmul(psb[:, r_ * 128:(r_ + 1) * 128], lhsT=st.GU[s_][:, r_, k, :], rhs=HC[:, k, 512:SW], start=(k == 0), stop=(k == 7)),
                                       reads=[st.GU[s_], HC], writes=[psb], signal=(k == 7))
                        sg = st.SG[fl % 2]
                        a = st.A[par][fl]
                        op("scalar", lambda e, psg=psg, sg=sg: e.activation(out=sg[:, 0:512], in_=psg[:], func=AF.Silu), reads=[psg], writes=[sg])
                        if has_b:
                            op("scalar", lambda e, psb=psb, sg=sg: e.activation(out=sg[:, 512:SW], in_=psb[:, 0:128], func=AF.Silu), reads=[psb], writes=[sg])
                        op("vector", lambda e, psu=psu, sg=sg, a=a: e.tensor_tensor(out=a[:, 0:512], in0=psu[:], in1=sg[:, 0:512], op=ALU.mult), reads=[psu, sg], writes=[a])
                        if has_b:
                            op("vector", lambda e, psb=psb, sg=sg, a=a: e.tensor_tensor(out=a[:, 512:SW], in0=psb[:, 128:256], in1=sg[:, 512:SW], op=ALU.mult), reads=[psb, sg], writes=[a])
                    psd = [next_ps(), next_ps()] if has_b else None
                    for m in range(8):
                        ps = next_ps()
                        while psd is not None and (ps is psd[0] or ps is psd[1]):
                            ps = next_ps()
                        for fl in range(gsz):
                            s_ = slot_of[f0 + fl]
                            op("tensor", lambda e, ps=ps, s_=s_, m=m, fl=fl, par=par, gsz=gsz: e.matmul(ps[:], lhsT=st.DN[s_][:, m * 128:(m + 1) * 128], rhs=st.A[par][fl][:, 0:512],
                                                                                                     start=(fl == 0), stop=(fl == gsz - 1)),
                               reads=[st.DN[s_], st.A[par][fl]], writes=[ps], signal=(fl == gsz - 1))
                        if has_b:
                            for fl in range(gsz):
                                s_ = slot_of[f0 + fl]
                                op("tensor", lambda e, psd=psd, s_=s_, m=m, fl=fl, par=par, gsz=gsz: e.matmul(psd[m // 4][:, (m % 4) * 128:(m % 4 + 1) * 128], lhsT=st.DN[s_][:, m * 128:(m + 1) * 128],
                                                                                                           rhs=st.A[par][fl][:, 512:SW], start=(fl == 0), stop=(fl == gsz - 1)),
                                   reads=[st.DN[s_], st.A[par][fl]], writes=[psd[m // 4]], signal=(fl == gsz - 1))
                        ya = YACC[m]
                        if gi_ == 0:
                            op("scalar", lambda e, ps=ps, ya=ya: e.activation(out=ya[:], in_=ps[:], func=AF.Copy), reads=[ps], writes=[ya])
                        else:
                            op("vector", lambda e, ps=ps, ya=ya: e.tensor_tensor(out=ya[:], in0=ps[:], in1=ya[:], op=ALU.add), reads=[ps, ya], writes=[ya])
                    if has_b:
                        for hf in range(2):
                            y2 = YACC2[:, hf * 4:hf * 4 + 4, :]
                            pv = psd[hf][:].rearrange("p (a n) -> p a n", a=4)
                            if gi_ == 0:
                                op("scalar", lambda e, pv=pv, y2=y2: e.activation(out=y2, in_=pv, func=AF.Copy), reads=[psd[hf]], writes=[YACC2])
                            else:
                                op("vector", lambda e, pv=pv, y2=y2: e.tensor_tensor(out=y2, in0=pv, in1=y2, op=ALU.add), reads=[psd[hf], YACC2], writes=[YACC2])
                    f0 += gsz
                P.cond_end()
                if mid_hook is not None:
                    mid_hook()
                P.cond_begin(flag_ap, FLI)
                for st4 in range(nst):
                    for hf in range(2):
                        ps = next_ps()
                        for mi in range(4):
                            m = hf * 4 + mi
                            if st4 < 4:
                                op("tensor", lambda e, ps=ps, mi=mi, m=m, st4=st4: e.transpose(ps[:, mi * 128:(mi + 1) * 128], YACC[m][:, st4 * 128:(st4 + 1) * 128], ident_t[:]),
                                   reads=[YACC[m], IDENT], writes=[ps], signal=(mi == 3))
                            else:
                                op("tensor", lambda e, ps=ps, mi=mi, m=m: e.transpose(ps[:, mi * 128:(mi + 1) * 128], YACC2[:, m, :], ident_t[:]),
                                   reads=[YACC2, IDENT], writes=[ps], signal=(mi == 3))
                        op("scalar", lambda e, ps=ps, st4=st4, hf=hf: e.activation(out=YSM[:, st4, hf * 512:(hf + 1) * 512], in_=ps[:], func=AF.Copy), reads=[ps], writes=[YSM])
                for tcx in range(NTC):
                    for st4 in range(nst):
                        op("vector", lambda e, st4=st4, tcx=tcx: e.tensor_scalar(out=SELT[st4][:], in0=POSBC[tcx][:], scalar1=float(base + 128 * st4), scalar2=iotap_t[:, 0:1],
                                                                                  op0=ALU.subtract, op1=ALU.is_equal), reads=[POSBC[tcx], IOTAP], writes=[SELT[st4]])
                    for m in range(8):
                        ps = next_ps()
                        for st4 in range(nst):
                            op("tensor", lambda e, ps=ps, st4=st4, m=m: e.matmul(ps[:], lhsT=YSM[:, st4, m * 128:(m + 1) * 128], rhs=SELT[st4][:], start=(st4 == 0), stop=(st4 == nst - 1)),
                               reads=[YSM, SELT[st4]], writes=[ps], signal=(st4 == nst - 1))
                        gb = st.GATEBC[0][tcx]
                        xr = XR[m][tcx]
                        op("vector", lambda e, ps=ps, gb=gb: e.tensor_tensor(out=TMPS_[:], in0=ps[:], in1=gb[:], op=ALU.mult), reads=[ps, gb], writes=[TMPS_])
                        op("vector", lambda e, xr=xr: e.tensor_tensor(out=xr[:], in0=TMPS_[:], in1=xr[:], op=ALU.add), reads=[TMPS_, xr], writes=[xr])
                P.cond_end()

            prefetch_first(0)
            for ex in range(8):
                build_bc(st.G, st.GATEBC[0], ex)
                build_bc(POSM, POSBC, ex)
                chunk_pass(ex, 0, FLI[0:1, ex, 0:1], mid_hook=(lambda ex=ex: prefetch_first(ex + 1)) if ex < 7 else None)
            for ex in range(8):
                for cch in range(1, 4):
                    fa = FLI[0:1, ex, cch:cch + 1]
                    if cch == 1:
                        P.cond_begin(fa, FLI)
                        build_bc(st.G, st.GATEBC[0], ex)
                        build_bc(POSM, POSBC, ex)
                        P.cond_end()
                    chunk_pass(ex, cch, fa)
            dump(3)
        if "final" in stages:
            cv = Carver()
            cv.off = SCR_ELEMS - 16 * 1024
            P.barrier()
            OST = [cv.take([512], F32, f"ost{i}") for i in range(4)]
            SQB = [cv.take([512], BF16, f"osq{i}") for i in range(4)]
            RST = [cv.take([512], F32, f"orst{i}") for i in range(2)]
            TMPL2 = cv.take([512], F32, "otmpl")
            cnt = [0]
            last = [None]

            def sink(c, tc, r):
                oi = cnt[0] % 4
                o = OST[oi]
                cnt[0] += 1
                op("vector", lambda e: e.scalar_tensor_tensor(out=o[:], in0=XR[c][tc][:], scalar=gains_t[:, 4, c:c + 1], in1=r[:], op0=ALU.mult, op1=ALU.mult),
                   reads=[XR[c][tc], GAINS, r], writes=[o])
                last[0] = P.dma("sync", lambda e: e.dma_start(out=d_out[:, c, tc * TC:(tc + 1) * TC], in_=o[:]), P.grp(f"ost{oi}"), reads=[o])

            rmsnorm(4, SQB, RST, TMPL2, sink)
        else:
            last = [None]
            for c in range(8):
                last[0] = P.dma("sync", lambda e, c=c: e.dma_start(out=d_out[:, c, :], in_=xres_t[:, c, :]), P.grp(f"ost{c % 4}"), reads=XR[c])
        for g_ in P.groups:
            if g_.count > 0:
                P.wait_tok("sync", Tok(g_.sem, 16 * g_.count))
        P.barrier()
        P.finish()
    return nc


def _prep_shared(inp):
    f = np.float32

    def cmaj(v):
        return np.ascontiguousarray(np.asarray(v, f).reshape(8, 128).T)

    def c4(v):
        return np.ascontiguousarray(np.asarray(v, f).reshape(4, 128).T)

    gains = np.stack([cmaj(inp["norm_mix_g"][0]), cmaj(inp["norm_ffn_g"][0]), cmaj(inp["norm_mix_g"][1]), cmaj(inp["norm_ffn_g"][1]),
                      cmaj(inp["final_norm_g"])], axis=1)
    w_in = np.ascontiguousarray(np.asarray(inp["w_in"], f).reshape(2, 8, 128, 1024).transpose(0, 2, 1, 3))
    w_out = np.ascontiguousarray(np.asarray(inp["w_out"], f).reshape(2, 8, 128, 1024).transpose(0, 2, 1, 3))
    w_glu = np.ascontiguousarray(np.asarray(inp["ssm_w_glu"], f).reshape(2, 4, 128, 512).transpose(0, 2, 1, 3))
    vec = np.stack([np.stack([c4(inp["ssm_b_glu"][l]), c4(inp["pool_scale"][l]), c4(inp["ssm_d"][l])], axis=1) for l in range(2)], axis=1)
    pool_w = np.ascontiguousarray(np.asarray(inp["pool_w"], f).transpose(0, 2, 1, 3))

    def hn(v):
        return np.asarray(v, f).reshape(16, 2, 64).transpose(1, 2, 0).reshape(128, 16)

    ssm_a = np.zeros((2, 128, 3, 16), f)
    ssm_bc = np.zeros((2, 128, 4, 16, 16), f)
    for l in range(2):
        ssm_a[l, :, 0] = hn(np.repeat(np.asarray(inp["ssm_log_dt"][l], f)[:, None], 64, axis=1))
        ssm_a[l, :, 1] = hn(inp["ssm_a_re"][l])
        ssm_a[l, :, 2] = hn(inp["ssm_a_im"][l])
        for i, key in enumerate(["ssm_b_re", "ssm_b_im"]):
            b = np.asarray(inp[key][l], f).reshape(16, 2, 64, 16)
            ssm_bc[l, :, i] = b.transpose(1, 2, 0, 3).reshape(128, 16, 16)
        for i, key in enumerate(["ssm_c_re", "ssm_c_im"]):
            c = np.asarray(inp[key][l], f).reshape(16, 2, 16, 64)
            ssm_bc[l, :, 2 + i] = c.transpose(1, 3, 0, 2).reshape(128, 16, 16)

    def gu(wg, wu):
        a = np.stack([np.asarray(wg, f), np.asarray(wu, f)], axis=0)
        a = a.reshape(2, 8, 128, NF, 128)
        return np.ascontiguousarray(a.transpose(3, 2, 0, 1, 4))

    ffn_gu = gu(inp["ffn_w_gate"][0], inp["ffn_w_up"][0])
    ffn_dn = np.ascontiguousarray(np.asarray(inp["ffn_w_down"][0], f).reshape(NF, 128, 1024))
    moe_gu = np.concatenate([gu(inp["moe_w_gate"][0][e], inp["moe_w_up"][0][e]) for e in range(8)], axis=0)
    moe_dn = np.ascontiguousarray(np.asarray(inp["moe_w_down"][0], f).reshape(8 * NF, 128, 1024))
    router = np.ascontiguousarray(np.asarray(inp["router_w"][0], f).reshape(8, 128, 8).transpose(1, 0, 2))
    return dict(gains=np.ascontiguousarray(gains), w_in=w_in, w_out=w_out, w_glu=w_glu, vec512=np.ascontiguousarray(vec), pool_w=pool_w,
                ssm_a=ssm_a, ssm_bc=ssm_bc, ffn_gu=ffn_gu, ffn_dn=ffn_dn, moe_gu=moe_gu, moe_dn=moe_dn, router=router)


def _x_layout(xb):
    return np.ascontiguousarray(np.asarray(xb, np.float32).T.reshape(8, 128, L).transpose(1, 0, 2))


def _out_layout(y):
    return np.ascontiguousarray(y.transpose(1, 0, 2).reshape(1024, L).T)


def kernel(**inputs):
    shared = _prep_shared(inputs)
    x = np.asarray(inputs["x"], np.float32)
    nb = x.shape[0]
    nc = build_program()
    in_maps = []
    for b in range(nb):
        m = dict(shared)
        m["xT"] = _x_layout(x[b])
        in_maps.append(m)
    res = run_bass_kernel_spmd(nc, in_maps, core_ids=list(range(nb)))
    out = np.stack([_out_layout(np.asarray(r["yT"])) for r in res.results], axis=0)
    return out.astype(np.float32)
```

```python
import math
import os
import numpy as np
from contextlib import ExitStack
import concourse.bass as bass
import concourse.mybir as mybir
from concourse.bass_utils import run_bass_kernel_spmd

F32 = mybir.dt.float32
BF16 = mybir.dt.bfloat16
I32 = mybir.dt.int32
ALU = mybir.AluOpType
AF = mybir.ActivationFunctionType
AX = mybir.AxisListType
ENGS = ["tensor", "vector", "scalar", "gpsimd", "sync"]

L = 2048
NTC = 4
TC = 512
J = 8
MB = L // J
NF = 22
NS = 8
GROUPS = [4, 4, 4, 4, 3, 3]
TWO_PI = 2.0 * math.pi
CARVE_INFO = {}


class Tok:
    __slots__ = ("sem", "val")

    def __init__(self, sem, val):
        self.sem = sem
        self.val = val


class T:
    def __init__(self, ap, name=""):
        self.ap = ap
        self.name = name
        self.last_w = None
        self.readers = []

    def __getitem__(self, k):
        return self.ap[k]


class DmaGroup:
    def __init__(self, sem):
        self.sem = sem
        self.count = 0


class Prog:
    def __init__(self, nc, ctx):
        self.nc = nc
        self.ctx = ctx
        self.streams = {e: [] for e in ENGS}
        self.sem = {e: ctx.enter_context(nc.semaphore("s_" + e)) for e in ENGS}
        self.cnt = {e: 0 for e in ENGS}
        self.waited = {e: {} for e in ENGS}
        self.pend_r = {e: [] for e in ENGS}
        self.pend_w = {e: [] for e in ENGS}
        self.groups = []
        self.ps_i = 0

    def sbuf(self, name, shape, dtype):
        return self.ctx.enter_context(self.nc.sbuf_tensor("sb_" + name, shape, dtype))

    def dma_group(self, name):
        g = DmaGroup(self.ctx.enter_context(self.nc.semaphore(name)))
        self.groups.append(g)
        return g

    def grp(self, key):
        if not hasattr(self, "_grps"):
            self._grps = {}
        if key not in self._grps:
            self._grps[key] = self.dma_group("dg_" + key)
        return self._grps[key]

    def _waits(self, eng, reads, writes, extra=()):
        need = {}

        def add(tok):
            if tok is None:
                return
            k = id(tok.sem)
            if k not in need or need[k].val < tok.val:
                need[k] = tok

        for t in reads:
            add(t.last_w)
            for e2 in ENGS:
                if e2 != eng:
                    assert not any(t is w for w in self.pend_w[e2]), ("RAW on unsignaled write", t.name, eng, e2)
        for t in writes:
            add(t.last_w)
            for r in t.readers:
                add(r)
            for e2 in ENGS:
                if e2 != eng:
                    assert not any(t is w for w in self.pend_r[e2]), ("WAR on unsignaled read", t.name, eng, e2)
                    assert not any(t is w for w in self.pend_w[e2]), ("WAW on unsignaled write", t.name, eng, e2)
        for tok in extra:
            add(tok)
        own = self.sem[eng]
        for k, tok in need.items():
            if eng == "tensor" and tok.sem is own:
                continue
            if self.waited[eng].get(k, 0) >= tok.val:
                continue
            self.waited[eng][k] = tok.val
            self.streams[eng].append(("wait", tok.sem, tok.val))

    def _commit(self, tok, reads, writes):
        for t in writes:
            t.last_w = tok
            t.readers = []
        for t in reads:
            rs = [r for r in t.readers if r.sem is not tok.sem]
            rs.append(tok)
            t.readers = rs

    def op(self, eng, fn, reads=(), writes=(), signal=True, extra=()):
        reads = list(reads)
        writes = list(writes)
        self._waits(eng, reads, writes, extra)
        if not signal:
            self.pend_r[eng] += reads
            self.pend_w[eng] += writes
            self.streams[eng].append(("op", fn, None))
            return None
        self.cnt[eng] += 1
        tok = Tok(self.sem[eng], self.cnt[eng])
        self.streams[eng].append(("op", fn, (tok.sem, 1)))
        self._commit(tok, reads + self.pend_r[eng], writes + self.pend_w[eng])
        self.pend_r[eng] = []
        self.pend_w[eng] = []
        return tok

    def dma(self, eng, fn, grp, reads=(), writes=(), extra=()):
        reads = list(reads)
        writes = list(writes)
        self._waits(eng, reads, writes, extra)
        if grp.count > 0:
            self.wait_tok(eng, Tok(grp.sem, 16 * grp.count))
        grp.count += 1
        tok = Tok(grp.sem, 16 * grp.count)
        self.streams[eng].append(("op", fn, (grp.sem, 16)))
        self._commit(tok, reads, writes)
        return tok

    def cond_begin(self, flag_ap, flag_T):
        for e in ENGS:
            assert not self.pend_r[e] and not self.pend_w[e]
        for e in ("tensor", "vector", "scalar", "gpsimd"):
            self._waits(e, [flag_T], [])
        if not hasattr(self, "_cond_stack"):
            self._cond_stack = []
        self._cond_stack.append((flag_ap, self.streams, {e: dict(self.waited[e]) for e in ENGS}))
        self.streams = {e: [] for e in ENGS}

    def cond_end(self):
        flag_ap, outer, saved_waited = self._cond_stack.pop()
        inner = self.streams
        self.streams = outer
        for e in ENGS:
            assert not self.pend_r[e] and not self.pend_w[e]
            items = inner[e]
            if not items:
                continue
            incs = {}

            def acc(sem, n):
                k = id(sem)
                if k not in incs:
                    incs[k] = [sem, 0]
                incs[k][1] += n

            for it in items:
                if it[0] == "op" and it[2] is not None:
                    acc(it[2][0], it[2][1])
                elif it[0] == "cond":
                    for sem, n in it[3]:
                        acc(sem, n)
            self.streams[e].append(("cond", flag_ap, items, list(incs.values())))
            self.waited[e] = saved_waited[e]

    def wait_tok(self, eng, tok):
        k = id(tok.sem)
        if self.waited[eng].get(k, 0) < tok.val:
            self.waited[eng][k] = tok.val
            self.streams[eng].append(("wait", tok.sem, tok.val))

    def barrier(self):
        toks = [Tok(self.sem[e], self.cnt[e]) for e in ENGS if self.cnt[e] > 0]
        toks += [Tok(g.sem, 16 * g.count) for g in self.groups if g.count > 0]
        for e in ENGS:
            for t in toks:
                if t.sem is self.sem[e]:
                    continue
                self.wait_tok(e, t)

    def finish(self):
        nc = self.nc
        streams = self.streams

        def run_items(e, items, regbox):
            for item in items:
                if item[0] == "wait":
                    e.wait_ge(item[1], item[2])
                elif item[0] == "cond":
                    if regbox[0] is None:
                        regbox[0] = e.alloc_register("flagreg")
                    r = regbox[0]
                    e.reg_load(r, item[1])
                    with e.If_eq(r, 1):
                        run_items(e, item[2], regbox)
                    with e.Else():
                        for sem, n in item[3]:
                            e.sem_inc(sem, n)
                else:
                    ins = item[1](e)
                    if item[2] is not None:
                        ins.then_inc(item[2][0], item[2][1])

        def replay(name):
            def f(e):
                run_items(e, streams[name], [None])
            return f

        with nc.Block() as block:
            block.tensor(replay("tensor"))
            block.vector(replay("vector"))
            block.scalar(replay("scalar"))
            block.gpsimd(replay("gpsimd"))
            block.sync(replay("sync"))


def build_program(stages=("mix0", "ffn0", "mix1", "ffn1", "final"), dbg=False):
    nc = bass.Bass("TRN2", target_bir_lowering=False)

    def din(name, shape, dt=F32):
        return nc.dram_tensor(name, shape, dt, kind="ExternalInput").ap()

    d_x = din("xT", [128, 8, L])
    d_gains = din("gains", [128, 5, 8])
    d_win = din("w_in", [2, 128, 8, 1024])
    d_wout = din("w_out", [2, 128, 8, 1024])
    d_wglu = din("w_glu", [2, 128, 4, 512])
    d_vec = din("vec512", [128, 2, 3, 4])
    d_poolw = din("pool_w", [2, 128, 4, 128])
    d_ssma = din("ssm_a", [2, 128, 3, 16])
    d_ssmbc = din("ssm_bc", [2, 128, 4, 16, 16])
    d_fgu = din("ffn_gu", [NF, 128, 2, 8, 128])
    d_fdn = din("ffn_dn", [NF, 128, 1024])
    d_mgu = din("moe_gu", [8 * NF, 128, 2, 8, 128])
    d_mdn = din("moe_dn", [8 * NF, 128, 1024])
    d_router = din("router", [128, 8, 8])
    d_out = nc.dram_tensor("yT", [128, 8, L], F32, kind="ExternalOutput").ap()
    d_dbg = None
    if dbg:
        d_dbg = nc.dram_tensor("dbg", [4, 128, 8, L], F32, kind="ExternalOutput").ap()
        d_dbgh = nc.dram_tensor("dbgh", [2, 128, 8, L], BF16, kind="ExternalOutput").ap()
        d_dbgs = nc.dram_tensor("dbgs", [128, 41456], BF16, kind="ExternalOutput").ap()
        d_dbgw = nc.dram_tensor("dbgw", [128, 8192], BF16, kind="ExternalOutput").ap()

    with ExitStack() as ctx:
        P = Prog(nc, ctx)
        op = P.op

        xres_t = P.sbuf("xres", [128, 8, L], F32)
        h_t = P.sbuf("hbf", [128, 8, L], BF16)
        wbuf_t = P.sbuf("wbuf", [128, 8 * 1024], BF16)
        wglu_t = P.sbuf("wglu", [128, 4, 512], BF16)
        poolw_t = P.sbuf("poolw", [128, 4, 128], BF16)
        gains_t = P.sbuf("gains", [128, 5, 8], F32)
        vec_t = P.sbuf("vec", [128, 2, 3, 4], F32)
        router_t = P.sbuf("router", [128, 8, 8], F32)
        ssma_t = P.sbuf("ssma", [128, 3, 16], F32)
        ssmbc_t = P.sbuf("ssmbc", [128, 4, 16, 16], F32)
        ident_t = P.sbuf("ident", [128, 128], F32)
        onesb_t = P.sbuf("onesb", [128, 128], BF16)
        onesf_t = P.sbuf("onesf", [128, 128], F32)
        iota1_t = P.sbuf("iota1", [128, 256], F32)
        iotaf_t = P.sbuf("iotaf", [128, 128], F32)
        iotap_t = P.sbuf("iotap", [128, 1], F32)
        maskq_t = P.sbuf("maskq", [128, 4], F32)
        maskb_t = P.sbuf("maskb", [128, 4, 128], F32)
        invc_t = P.sbuf("invc", [128, 4, 16], F32)
        SCR_ELEMS = 41456
        scr_t = P.sbuf("scr", [128, SCR_ELEMS], BF16)
        psum_t = [ctx.enter_context(nc.psum_tensor(f"ps{i}", [128, 512], F32)) for i in range(8)]

        XR = [[T(xres_t[:, c, tc * TC:(tc + 1) * TC], f"xr{c}_{tc}") for tc in range(NTC)] for c in range(8)]
        H = [[T(h_t[:, c, tc * TC:(tc + 1) * TC], f"h{c}_{tc}") for tc in range(NTC)] for c in range(8)]
        WBUF = T(wbuf_t[:, :], "wbuf")
        WGLU = T(wglu_t[:], "wglu")
        POOLW = T(poolw_t[:], "poolw")
        GAINS = T(gains_t[:], "gains")
        VEC = T(vec_t[:], "vec")
        ROUTER = T(router_t[:], "router")
        SSMA = T(ssma_t[:], "ssma")
        SSMBC = T(ssmbc_t[:], "ssmbc")
        IDENT = T(ident_t[:], "ident")
        ONESB = T(onesb_t[:], "onesb")
        ONESF = T(onesf_t[:], "onesf")
        IOTA1 = T(iota1_t[:], "iota1")
        IOTAF = T(iotaf_t[:], "iotaf")
        IOTAP = T(iotap_t[:], "iotap")
        MASKQ = T(maskq_t[:], "maskq")
        MASKB = T(maskb_t[:], "maskb")
        INVC = T(invc_t[:], "invc")
        PS = [T(psum_t[i][:], f"ps{i}") for i in range(8)]

        def next_ps():
            p = PS[P.ps_i % 8]
            P.ps_i += 1
            return p

        class Carver:
            def __init__(self):
                self.off = 0

            def take(self, shape, dtype, name):
                n = int(np.prod(shape))
                el = n * (2 if dtype in (F32, I32) else 1)
                if self.off % 2:
                    self.off += 1
                a = scr_t[:, self.off:self.off + el]
                CARVE_INFO[name] = (self.off, tuple(shape), str(dtype))
                self.off += el
                assert self.off <= SCR_ELEMS, (name, self.off)
                if dtype != BF16:
                    a = a.bitcast(dtype)
                if len(shape) == 2:
                    a = a.rearrange("p (a b) -> p a b", a=shape[0])
                elif len(shape) == 3:
                    a = a.rearrange("p (a b c) -> p a b c", a=shape[0], b=shape[1])
                elif len(shape) == 4:
                    a = a.rearrange("p (a b c d) -> p a b c d", a=shape[0], b=shape[1], c=shape[2])
                return T(a, name)

        g_out = P.dma_group("g_dbg")

        op("gpsimd", lambda e: e.iota(iotaf_t[:], [[1, 128]], base=0, channel_multiplier=0, allow_small_or_imprecise_dtypes=True), writes=[IOTAF])
        op("gpsimd", lambda e: e.iota(iotap_t[:], [[1, 1]], base=0, channel_multiplier=1, allow_small_or_imprecise_dtypes=True), writes=[IOTAP])
        op("gpsimd", lambda e: e.iota(iota1_t[:], [[1, 256]], base=1, channel_multiplier=0, allow_small_or_imprecise_dtypes=True), writes=[IOTA1])
        op("vector", lambda e: e.tensor_scalar(out=ident_t[:], in0=iotaf_t[:], scalar1=iotap_t[:, 0:1], scalar2=None, op0=ALU.is_equal), reads=[IOTAF, IOTAP], writes=[IDENT])
        op("vector", lambda e: e.memset(onesb_t[:], 1.0), writes=[ONESB])
        op("vector", lambda e: e.memset(onesf_t[:], 1.0), writes=[ONESF])
        tmpm_t = P.sbuf("tmpm", [128, 4], F32)
        TMPM = T(tmpm_t[:], "tmpm")
        for q4 in range(4):
            op("vector", lambda e, q4=q4: e.tensor_single_scalar(out=maskq_t[:, q4:q4 + 1], in_=iotap_t[:, 0:1], scalar=float(32 * q4), op=ALU.is_ge), reads=[IOTAP], writes=[MASKQ])
            op("vector", lambda e, q4=q4: e.tensor_single_scalar(out=tmpm_t[:, q4:q4 + 1], in_=iotap_t[:, 0:1], scalar=float(32 * q4 + 32), op=ALU.is_lt), reads=[IOTAP], writes=[TMPM])
        op("vector", lambda e: e.tensor_tensor(out=maskq_t[:], in0=maskq_t[:], in1=tmpm_t[:], op=ALU.mult), reads=[MASKQ, TMPM], writes=[MASKQ])
        for q4 in range(4):
            op("vector", lambda e, q4=q4: e.tensor_copy(out=maskb_t[:, :, 32 * q4:32 * q4 + 32], in_=maskq_t[:, q4:q4 + 1].unsqueeze(2).to_broadcast([128, 4, 32])),
               reads=[MASKQ], writes=[MASKB])
        for g in range(4):
            op("vector", lambda e, g=g: e.tensor_single_scalar(out=invc_t[:, g, :], in_=iota1_t[:, 0:16], scalar=float(2 ** (g + 1)), op=ALU.min), reads=[IOTA1], writes=[INVC])
        op("vector", lambda e: e.reciprocal(out=invc_t[:], in_=invc_t[:]), reads=[INVC], writes=[INVC])

        for c in range(8):
            P.dma("sync", lambda e, c=c: e.dma_start(out=xres_t[:, c, :], in_=d_x[:, c, :]), P.grp(f"x{c}"), writes=XR[c])
        P.dma("sync", lambda e: e.dma_start(out=gains_t[:], in_=d_gains), P.grp("gains"), writes=[GAINS])
        P.dma("sync", lambda e: e.dma_start(out=vec_t[:], in_=d_vec), P.grp("vec"), writes=[VEC])
        P.dma("sync", lambda e: e.dma_start(out=router_t[:], in_=d_router), P.grp("router"), writes=[ROUTER])

        def load_wbuf(src):
            P.dma("gpsimd", lambda e: e.dma_start(out=wbuf_t[:, :].rearrange("p (a b) -> p a b", a=8), in_=src), P.grp("wbuf"), writes=[WBUF])

        def load_mixer_small(l):
            P.dma("gpsimd", lambda e: e.dma_start(out=wglu_t[:], in_=d_wglu[l]), P.grp("wglu"), writes=[WGLU])
            P.dma("gpsimd", lambda e: e.dma_start(out=poolw_t[:], in_=d_poolw[l]), P.grp("poolw"), writes=[POOLW])
            P.dma("sync", lambda e: e.dma_start(out=ssma_t[:], in_=d_ssma[l]), P.grp("ssma"), writes=[SSMA])
            P.dma("sync", lambda e: e.dma_start(out=ssmbc_t[:], in_=d_ssmbc[l]), P.grp("ssmbc"), writes=[SSMBC])

        def rmsnorm(gi, sqb, rst, tmpl, sink, rstd_keep=None):
            for tc in range(NTC):
                ps = next_ps()
                for c in range(8):
                    sq = sqb[(tc * 8 + c) % len(sqb)]
                    op("scalar", lambda e, sq=sq, c=c, tc=tc: e.activation(out=sq[:], in_=XR[c][tc][:], func=AF.Square), reads=[XR[c][tc]], writes=[sq])
                    op("tensor", lambda e, sq=sq, ps=ps, c=c: e.matmul(ps[:], lhsT=onesb_t[:], rhs=sq[:], start=(c == 0), stop=(c == 7)),
                       reads=[sq, ONESB], writes=[ps], signal=True)
                r = rst[tc % len(rst)] if rstd_keep is None else rstd_keep[tc]
                op("scalar", lambda e, ps=ps: e.activation(out=tmpl[:], in_=ps[:], func=AF.Ln, scale=1.0 / 1024.0, bias=1e-6), reads=[ps], writes=[tmpl])
                op("scalar", lambda e, r=r: e.activation(out=r[:], in_=tmpl[:], func=AF.Exp, scale=-0.5), reads=[tmpl], writes=[r])
                for c in range(8):
                    sink(c, tc, r)

        def norm_to_h(gi):
            def sink(c, tc, r):
                op("vector", lambda e: e.scalar_tensor_tensor(out=H[c][tc][:], in0=XR[c][tc][:], scalar=gains_t[:, gi, c:c + 1], in1=r[:], op0=ALU.mult, op1=ALU.mult),
                   reads=[XR[c][tc], GAINS, r], writes=[H[c][tc]])
            return sink

        def mixer(l):
            cv = Carver()
            USSM = [[cv.take([8, 64], BF16, f"u{mc}_{tc}") for tc in range(NTC)] for mc in range(4)]
            SBF = [[cv.take([2, 260], BF16, f"sbf{s}_{q4}") for q4 in range(4)] for s in range(2)]
            NTMP = 12
            off_tmps = cv.off
            TMPS = [[cv.take([256], F32, f"st{s}_{i}") for i in range(NTMP)] for s in range(1)]
            off_pb = cv.off
            PBRE = cv.take([8, 4, 32], F32, "pbre")
            PBIM = cv.take([8, 4, 32], F32, "pbim")
            off_wx = cv.off
            WX = cv.take([16, 4, 128], BF16, "wx")
            KD = cv.take([8, 128], BF16, "kd")
            POOLED = [cv.take([512], BF16, f"pooled{i}") for i in range(4)]
            TAIL = [cv.take([16], F32, f"tail{g}") for g in range(4)]
            cva = Carver(); cva.off = off_tmps
            XH = [cva.take([528], F32, f"xh{i}") for i in range(2)]
            SA = cva.take([528], F32, "sa")
            SB_ = cva.take([528], F32, "sb")
            cvb = Carver(); cvb.off = off_wx
            SQB = [cvb.take([512], BF16, f"sqb{i}") for i in range(4)]
            RST = [cvb.take([512], F32, f"rst{i}") for i in range(2)]
            TMPL = cvb.take([512], F32, "tmpl")
            assert cvb.off <= off_wx + 8192
            cvc = Carver(); cvc.off = off_pb
            SGT = [cvc.take([512], F32, f"sgt{i}") for i in range(2)]
            def p16(name):
                return cv.take([16], F32, name)
            DT, AR, ZR, ZI, MAG, Q1, QF, PH, SN, PA, CS, LR, LI, DEN, RDEN, NR, CR, CI, TA, TB, RHO8, F8 = [p16(f"pp{i}") for i in range(22)]
            QI = cv.take([16], I32, "qi")
            PWR = cv.take([9, 16], F32, "pwr")
            PWI = cv.take([9, 16], F32, "pwi")
            BBRE = cv.take([16, 16], F32, "bbre")
            BBIM = cv.take([16, 16], F32, "bbim")
            BT1 = cv.take([16, 16], F32, "bt1")
            CRET = cv.take([16, 32], F32, "cret")
            NCIMT = cv.take([16, 32], F32, "ncimt")
            PBT = cv.take([8, 4, 16], F32, "pbt")
            WCT1 = cv.take([8, 16], F32, "wct1")
            WCT2 = cv.take([8, 16], F32, "wct2")
            DIAGD = cv.take([128], F32, "diagd")
            KTMP = cv.take([128], F32, "ktmp")
            TI = [cv.take([256], I32, f"ti{s}") for s in range(1)]

            V = lambda e: e

            def tt(eng, out, in0, in1, o, reads, writes):
                return op(eng, lambda e: e.tensor_tensor(out=out, in0=in0, in1=in1, op=o), reads=reads, writes=writes)

            load_wbuf(d_win[l])
            load_mixer_small(l)

            rmsnorm(2 * l, SQB, RST, TMPL, norm_to_h(2 * l))

            A_ldt = ssma_t[:, 0, :]
            A_are = ssma_t[:, 1, :]
            A_aim = ssma_t[:, 2, :]
            op("scalar", lambda e: e.activation(out=DT[:], in_=A_ldt, func=AF.Exp), reads=[SSMA], writes=[DT])
            op("vector", lambda e: e.tensor_single_scalar(out=AR[:], in_=A_are, scalar=-1e-4, op=ALU.min), reads=[SSMA], writes=[AR])
            tt("vector", ZR[:], AR[:], DT[:], ALU.mult, [AR, DT], [ZR])
            tt("vector", ZI[:], A_aim, DT[:], ALU.mult, [SSMA, DT], [ZI])
            op("scalar", lambda e: e.activation(out=MAG[:], in_=ZR[:], func=AF.Exp), reads=[ZR], writes=[MAG])

            def sincos(src, scale, SNo, CSo):
                op("vector", lambda e: e.tensor_single_scalar(out=Q1[:], in_=src[:], scalar=scale / TWO_PI, op=ALU.mult), reads=[src], writes=[Q1])
                op("vector", lambda e: e.tensor_copy(out=QI[:], in_=Q1[:]), reads=[Q1], writes=[QI])
                op("vector", lambda e: e.tensor_copy(out=QF[:], in_=QI[:]), reads=[QI], writes=[QF])
                tt("vector", PH[:], Q1[:], QF[:], ALU.subtract, [Q1, QF], [PH])
                if SNo is not None:
                    op("scalar", lambda e: e.activation(out=SNo[:], in_=PH[:], func=AF.Sin, scale=TWO_PI), reads=[PH], writes=[SNo])
                if CSo is not None:
                    op("vector", lambda e: e.scalar_tensor_tensor(out=PA[:], in0=PH[:], scalar=-1.0, in1=PH[:], op0=ALU.mult, op1=ALU.max), reads=[PH], writes=[PA])
                    op("scalar", lambda e: e.activation(out=CSo[:], in_=PA[:], func=AF.Sin, scale=-TWO_PI, bias=HALFPI[:, 0:1]), reads=[PA, HALFPI_T], writes=[CSo])

            sincos(ZI, 1.0, SN, CS)
            tt("vector", LR[:], MAG[:], CS[:], ALU.mult, [MAG, CS], [LR])
            tt("vector", LI[:], MAG[:], SN[:], ALU.mult, [MAG, SN], [LI])
            tt("vector", TA[:], AR[:], AR[:], ALU.mult, [AR], [TA])
            tt("vector", TB[:], A_aim, A_aim, ALU.mult, [SSMA], [TB])
            tt("vector", DEN[:], TA[:], TB[:], ALU.add, [TA, TB], [DEN])
            op("vector", lambda e: e.reciprocal(out=RDEN[:], in_=DEN[:]), reads=[DEN], writes=[RDEN])
            op("vector", lambda e: e.tensor_single_scalar(out=NR[:], in_=LR[:], scalar=-1.0, op=ALU.add), reads=[LR], writes=[NR])
            tt("vector", TA[:], NR[:], AR[:], ALU.mult, [NR, AR], [TA])
            tt("vector", TB[:], LI[:], A_aim, ALU.mult, [LI, SSMA], [TB])
            tt("vector", TA[:], TA[:], TB[:], ALU.add, [TA, TB], [TA])
            tt("vector", CR[:], TA[:], RDEN[:], ALU.mult, [TA, RDEN], [CR])
            tt("vector", TA[:], LI[:], AR[:], ALU.mult, [LI, AR], [TA])
            tt("vector", TB[:], NR[:], A_aim, ALU.mult, [NR, SSMA], [TB])
            tt("vector", TA[:], TA[:], TB[:], ALU.subtract, [TA, TB], [TA])
            tt("vector", CI[:], TA[:], RDEN[:], ALU.mult, [TA, RDEN], [CI])
            b_re = ssmbc_t[:, 0]
            b_im = ssmbc_t[:, 1]
            c_re = ssmbc_t[:, 2]
            c_im = ssmbc_t[:, 3]
            crb = CR[:].unsqueeze(2).to_broadcast([128, 16, 16])
            cib = CI[:].unsqueeze(2).to_broadcast([128, 16, 16])
            tt("vector", BBRE[:], b_re, crb, ALU.mult, [SSMBC, CR], [BBRE])
            tt("vector", BT1[:], b_im, cib, ALU.mult, [SSMBC, CI], [BT1])
            tt("vector", BBRE[:], BBRE[:], BT1[:], ALU.subtract, [BBRE, BT1], [BBRE])
            tt("vector", BBIM[:], b_im, crb, ALU.mult, [SSMBC, CR], [BBIM])
            tt("vector", BT1[:], b_re, cib, ALU.mult, [SSMBC, CI], [BT1])
            tt("vector", BBIM[:], BBIM[:], BT1[:], ALU.add, [BBIM, BT1], [BBIM])
            op("vector", lambda e: e.memset(PWR[:, 0, :], 1.0), writes=[PWR])
            op("vector", lambda e: e.memset(PWI[:, 0, :], 0.0), writes=[PWI])
            for k in range(8):
                tt("vector", TA[:], PWR[:, k, :], LR[:], ALU.mult, [PWR, LR], [TA])
                tt("vector", TB[:], PWI[:, k, :], LI[:], ALU.mult, [PWI, LI], [TB])
                tt("vector", PWR[:, k + 1, :], TA[:], TB[:], ALU.subtract, [TA, TB], [PWR])
                tt("vector", TA[:], PWR[:, k, :], LI[:], ALU.mult, [PWR, LI], [TA])
                tt("vector", TB[:], PWI[:, k, :], LR[:], ALU.mult, [PWI, LR], [TB])
                tt("vector", PWI[:, k + 1, :], TA[:], TB[:], ALU.add, [TA, TB], [PWI])
            op("scalar", lambda e: e.activation(out=RHO8[:], in_=ZR[:], func=AF.Exp, scale=float(J)), reads=[ZR], writes=[RHO8])
            op("vector", lambda e: e.tensor_single_scalar(out=Q1[:], in_=ZI[:], scalar=float(J) / TWO_PI, op=ALU.mult), reads=[ZI], writes=[Q1])
            op("vector", lambda e: e.tensor_copy(out=QI[:], in_=Q1[:]), reads=[Q1], writes=[QI])
            op("vector", lambda e: e.tensor_copy(out=QF[:], in_=QI[:]), reads=[QI], writes=[QF])
            tt("vector", F8[:], Q1[:], QF[:], ALU.subtract, [Q1, QF], [F8])
            op("vector", lambda e: e.memset(CRET[:], 0.0), writes=[CRET])
            op("vector", lambda e: e.memset(NCIMT[:], 0.0), writes=[NCIMT])
            for h2 in range(2):
                ps_ = slice(64 * h2, 64 * h2 + 64)
                cs_ = slice(16 * h2, 16 * h2 + 16)
                op("vector", lambda e, ps_=ps_, cs_=cs_: e.tensor_copy(out=CRET[ps_, :, cs_], in_=c_re[ps_]), reads=[SSMBC], writes=[CRET])
                op("vector", lambda e, ps_=ps_, cs_=cs_: e.tensor_single_scalar(out=NCIMT[ps_, :, cs_], in_=c_im[ps_], scalar=-1.0, op=ALU.mult), reads=[SSMBC], writes=[NCIMT])
            op("vector", lambda e: e.memset(PBRE[:], 0.0), writes=[PBRE])
            op("vector", lambda e: e.memset(PBIM[:], 0.0), writes=[PBIM])
            for s in range(2):
                for q4 in range(4):
                    op("vector", lambda e, s=s, q4=q4: e.memset(SBF[s][q4][:], 0.0), writes=[SBF[s][q4]])

            for tc in range(NTC):
                for mc in [4, 5, 6, 7, 0, 1, 2, 3]:
                    ps = next_ps()
                    for k in range(8):
                        op("tensor", lambda e, ps=ps, k=k, mc=mc, tc=tc: e.matmul(ps[:], lhsT=wbuf_t[:, k * 1024 + mc * 128:k * 1024 + mc * 128 + 128], rhs=H[k][tc][:],
                                                                                   start=(k == 0), stop=(k == 7)),
                           reads=[WBUF, H[k][tc]], writes=[ps], signal=(k == 7))
                    if mc < 4:
                        u = USSM[mc][tc]
                        op("scalar", lambda e, ps=ps, u=u: e.activation(out=u[:], in_=ps[:].rearrange("p (m i) -> p i m", i=8), func=AF.Copy), reads=[ps], writes=[u])
                    else:
                        g = mc - 4
                        w = 2 ** (g + 1)
                        xh = XH[(tc * 4 + g) % 2]
                        op("scalar", lambda e, ps=ps, xh=xh: e.activation(out=xh[:, 16:528], in_=ps[:], func=AF.Copy), reads=[ps], writes=[xh])
                        if tc == 0:
                            op("vector", lambda e, xh=xh: e.memset(xh[:, 0:16], 0.0), writes=[xh])
                        else:
                            op("vector", lambda e, xh=xh, g=g: e.tensor_copy(out=xh[:, 0:16], in_=TAIL[g][:]), reads=[TAIL[g]], writes=[xh])
                        op("vector", lambda e, xh=xh, g=g: e.tensor_copy(out=TAIL[g][:], in_=xh[:, 512:528]), reads=[xh], writes=[TAIL[g]])
                        src = xh
                        bufs = [SA, SB_]
                        sh = 1
                        lo = 1
                        for lev in range(g + 1):
                            dst = bufs[lev % 2]
                            op("vector", lambda e, src=src, dst=dst, sh=sh, lo=lo: e.tensor_tensor(out=dst[:, lo:528], in0=src[:, lo:528], in1=src[:, lo - sh:528 - sh], op=ALU.add),
                               reads=[src], writes=[dst])
                            src = dst
                            sh *= 2
                            lo += sh
                        pl = POOLED[g]
                        op("vector", lambda e, src=src, xh=xh, pl=pl, w=w: e.scalar_tensor_tensor(out=pl[:], in0=src[:, 16:528], scalar=1.0 / w, in1=xh[:, 16:528], op0=ALU.mult, op1=ALU.subtract),
                           reads=[src, xh], writes=[pl])
                        if tc == 0:
                            tmpc = TMPL
                            op("vector", lambda e, src=src, g=g: e.tensor_tensor(out=TMPL[:, 0:16], in0=src[:, 16:32], in1=invc_t[:, g, :], op=ALU.mult), reads=[src, INVC], writes=[TMPL])
                            op("vector", lambda e, xh=xh, pl=pl: e.tensor_tensor(out=pl[:, 0:16], in0=TMPL[:, 0:16], in1=xh[:, 16:32], op=ALU.subtract), reads=[TMPL, xh], writes=[pl])
                for g in range(4):
                    ps = next_ps()
                    op("tensor", lambda e, ps=ps, g=g: e.matmul(ps[:], lhsT=poolw_t[:, g, :], rhs=POOLED[g][:], start=True, stop=True), reads=[POOLW, POOLED[g]], writes=[ps])
                    op("scalar", lambda e, ps=ps, g=g, tc=tc: e.activation(out=H[4 + g][tc][:], in_=ps[:], func=AF.Identity, scale=vec_t[:, l, 1, g:g + 1]), reads=[ps, VEC], writes=[H[4 + g][tc]])

            op("gpsimd", lambda e: e.memset(wbuf_t[:, :], 0.0), writes=[WBUF])
            WC = wbuf_t[:, :].rearrange("p (j q r n) -> p j q r n", j=8, q=4, r=2)

            first_tab_extra = [Tok(P.sem[e_], P.cnt[e_]) for e_ in ("vector", "scalar", "tensor") if P.cnt[e_] > 0]
            for qt in range(4):
                for h2 in range(2):
                    pp = slice(64 * h2, 64 * h2 + 64)
                    cc = slice(16 * h2, 16 * h2 + 16)
                    pr = PWR[pp, 0:8, 4 * qt:4 * qt + 4].unsqueeze(3).to_broadcast([64, 8, 4, 16])
                    pi = PWI[pp, 0:8, 4 * qt:4 * qt + 4].unsqueeze(3).to_broadcast([64, 8, 4, 16])
                    br = BBRE[pp, 4 * qt:4 * qt + 4, :].unsqueeze(1).to_broadcast([64, 8, 4, 16])
                    bi = BBIM[pp, 4 * qt:4 * qt + 4, :].unsqueeze(1).to_broadcast([64, 8, 4, 16])
                    tt("vector", PBRE[pp, :, :, cc], pr, br, ALU.mult, [PWR, BBRE], [PBRE])
                    tt("vector", PBT[pp], pi, bi, ALU.mult, [PWI, BBIM], [PBT])
                    tt("vector", PBRE[pp, :, :, cc], PBRE[pp, :, :, cc], PBT[pp], ALU.subtract, [PBRE, PBT], [PBRE])
                    tt("vector", PBIM[pp, :, :, cc], pr, bi, ALU.mult, [PWR, BBIM], [PBIM])
                    tt("vector", PBT[pp], pi, br, ALU.mult, [PWI, BBRE], [PBT])
                    tt("vector", PBIM[pp, :, :, cc], PBIM[pp, :, :, cc], PBT[pp], ALU.add, [PBIM, PBT], [PBIM])
                for b in range(4):
                    ps = next_ps()
                    for i4 in range(4):
                        idx = 4 * b + i4
                        j = idx // 2
                        reim = idx % 2
                        src = PBRE if reim == 0 else PBIM
                        op("tensor", lambda e, ps=ps, i4=i4, src=src, j=j: e.transpose(ps[:, i4 * 128:(i4 + 1) * 128], src[:, 7 - j, :, :].rearrange("p a b -> p (a b)"), ident_t[:]),
                           reads=[src, IDENT], writes=[ps], signal=(i4 == 3))
                    for q4 in range(4):
                        op("scalar", lambda e, ps=ps, b=b, q4=q4: e.activation(out=WX[:, 4 * b:4 * b + 4, q4, :], in_=ps[:].rearrange("p (a n) -> p a n", a=4), func=AF.Identity,
                                                                                scale=maskq_t[:, q4:q4 + 1]), reads=[ps, MASKQ], writes=[WX],
                           extra=(first_tab_extra if (qt == 0 and b == 0 and q4 == 0) else ()))
                psk = [next_ps(), next_ps()]
                for d in range(8):
                    pk = psk[d // 4]
                    oc = slice((d % 4) * 128, (d % 4) * 128 + 128)
                    op("tensor", lambda e, pk=pk, oc=oc, d=d, qt=qt: e.matmul(pk[:, oc], lhsT=PBRE[:, d, :, :].rearrange("p a b -> p (a b)"),
                                                                       rhs=CRET[:, 4 * qt:4 * qt + 4, :].rearrange("p a b -> p (a b)"), start=True, stop=False),
                       reads=[PBRE, CRET], writes=[pk], signal=False)
                    op("tensor", lambda e, pk=pk, oc=oc, d=d, qt=qt: e.matmul(pk[:, oc], lhsT=PBIM[:, d, :, :].rearrange("p a b -> p (a b)"),
                                                                       rhs=NCIMT[:, 4 * qt:4 * qt + 4, :].rearrange("p a b -> p (a b)"), start=False, stop=True),
                       reads=[PBIM, NCIMT], writes=[pk], signal=(d % 4 == 3))
                op("vector", lambda e, qt=qt: e.tensor_scalar(out=DIAGD[:], in0=ident_t[:], scalar1=vec_t[:, l, 2, qt:qt + 1], scalar2=None, op0=ALU.mult), reads=[IDENT, VEC], writes=[DIAGD])
                tt("vector", KTMP[:], psk[0][:, 0:128], maskb_t[:, 0, :], ALU.mult, [psk[0], MASKB], [KTMP])
                tt("vector", KD[:, 0, :], KTMP[:], DIAGD[:], ALU.add, [KTMP, DIAGD], [KD])
                tt("vector", KD[:, 1:4, :], psk[0][:, 128:512].rearrange("p (a n) -> p a n", a=3), maskb_t[:, 0:3, :], ALU.mult, [psk[0], MASKB], [KD])
                tt("vector", KD[:, 4:8, :], psk[1][:].rearrange("p (a n) -> p a n", a=4), maskb_t[:], ALU.mult, [psk[1], MASKB], [KD])
                for q4 in range(4):
                    q = 4 * qt + q4
                    for h2 in range(2):
                        pp = slice(64 * h2, 64 * h2 + 64)
                        cc = slice(32 * q4 + 16 * h2, 32 * q4 + 16 * h2 + 16)
                        crb_ = c_re[pp, q, :].unsqueeze(1).to_broadcast([64, 8, 16])
                        cib_ = c_im[pp, q, :].unsqueeze(1).to_broadcast([64, 8, 16])
                        prb = PWR[pp, 1:9, q].unsqueeze(2).to_broadcast([64, 8, 16])
                        pib = PWI[pp, 1:9, q].unsqueeze(2).to_broadcast([64, 8, 16])
                        tt("vector", WCT1[pp], crb_, prb, ALU.mult, [SSMBC, PWR], [WCT1])
                        tt("vector", WCT2[pp], cib_, pib, ALU.mult, [SSMBC, PWI], [WCT2])
                        tt("vector", WC[pp, :, q4, 0, cc], WCT1[pp], WCT2[pp], ALU.subtract, [WCT1, WCT2], [WBUF])
                        tt("vector", WCT1[pp], crb_, pib, ALU.mult, [SSMBC, PWI], [WCT1])
                        tt("vector", WCT2[pp], cib_, prb, ALU.mult, [SSMBC, PWR], [WCT2])
                        op("vector", lambda e, pp=pp, q4=q4, cc=cc: e.scalar_tensor_tensor(out=WC[pp, :, q4, 1, cc], in0=WCT1[pp], scalar=-1.0, in1=WCT2[pp], op0=ALU.mult, op1=ALU.subtract),
                           reads=[WCT1, WCT2], writes=[WBUF])
                sset = qt % 2
                for q4 in range(4):
                    q = 4 * qt + q4
                    psx = next_ps()
                    for reim in range(2):
                        for j in range(8):
                            op("tensor", lambda e, psx=psx, reim=reim, j=j, q4=q4, qt=qt: e.matmul(
                                psx[:, reim * 256:(reim + 1) * 256].rearrange("p (a m) -> p a m", a=4),
                                lhsT=WX[:, 2 * j + reim, q4, :],
                                rhs=scr_u[qt][:, :, j, :], start=(j == 0), stop=(j == 7)),
                               reads=[WX] + USSM[qt], writes=[psx], signal=(reim == 1 and j == 7))
                    tm = TMPS[0]
                    PHr, TFp, SN0, SN1, CS0, CS1, T1, T2, VRE, VIM, RRE, RIM = tm
                    SNt = SN0 if q % 2 == 0 else SN1
                    CSt = CS0 if q % 2 == 0 else CS1
                    tix = TI[0]
                    ex_ = first_tab_extra if (qt == 0 and q4 == 0) else ()
                    op("vector", lambda e, q=q: e.tensor_scalar(out=PHr[:], in0=iota1_t[:], scalar1=F8[:, q:q + 1], scalar2=None, op0=ALU.mult), reads=[IOTA1, F8], writes=[PHr], extra=ex_)
                    op("vector", lambda e: e.tensor_copy(out=tix[:], in_=PHr[:]), reads=[PHr], writes=[tix])
                    op("vector", lambda e: e.tensor_copy(out=TFp[:], in_=tix[:]), reads=[tix], writes=[TFp])
                    op("vector", lambda e: e.tensor_tensor(out=PHr[:], in0=PHr[:], in1=TFp[:], op=ALU.subtract), reads=[PHr, TFp], writes=[PHr])
                    op("vector", lambda e: e.scalar_tensor_tensor(out=TFp[:], in0=PHr[:], scalar=-1.0, in1=PHr[:], op0=ALU.mult, op1=ALU.max), reads=[PHr], writes=[TFp])
                    op("scalar", lambda e, SNt=SNt: e.activation(out=SNt[:], in_=PHr[:], func=AF.Sin, scale=TWO_PI), reads=[PHr], writes=[SNt])
                    op("scalar", lambda e, CSt=CSt: e.activation(out=CSt[:], in_=TFp[:], func=AF.Sin, scale=-TWO_PI, bias=HALFPI[:, 0:1]), reads=[TFp, HALFPI_T], writes=[CSt])
                    xre = psx[:, 0:256]
                    xim = psx[:, 256:512]
                    tt("vector", T1[:], xre, CSt[:], ALU.mult, [psx, CSt], [T1])
                    tt("vector", T2[:], xim, SNt[:], ALU.mult, [psx, SNt], [T2])
                    tt("vector", VRE[:], T1[:], T2[:], ALU.add, [T1, T2], [VRE])
                    tt("vector", T1[:], xim, CSt[:], ALU.mult, [psx, CSt], [T1])
                    tt("vector", T2[:], xre, SNt[:], ALU.mult, [psx, SNt], [T2])
                    tt("vector", VIM[:], T1[:], T2[:], ALU.subtract, [T1, T2], [VIM])
                    rho = RHO8[:, q:q + 1].to_broadcast([128, 256])
                    op("vector", lambda e, VRE=VRE, RRE=RRE, rho=rho: e.tensor_tensor_scan(out=RRE[:], data0=rho, data1=VRE[:], initial=0.0, op0=ALU.mult, op1=ALU.add), reads=[VRE, RHO8], writes=[RRE])
                    op("vector", lambda e, VIM=VIM, RIM=RIM, rho=rho: e.tensor_tensor_scan(out=RIM[:], data0=rho, data1=VIM[:], initial=0.0, op0=ALU.mult, op1=ALU.add), reads=[VIM, RHO8], writes=[RIM])
                    sb = SBF[sset][q4]
                    tt("vector", T1[:], RRE[:], CSt[:], ALU.mult, [RRE, CSt], [T1])
                    tt("vector", T2[:], RIM[:], SNt[:], ALU.mult, [RIM, SNt], [T2])
                    tt("vector", sb[:, 0, 1:257], T1[:], T2[:], ALU.subtract, [T1, T2], [sb])
                    tt("vector", T1[:], RRE[:], SNt[:], ALU.mult, [RRE, SNt], [T1])
                    tt("vector", T2[:], RIM[:], CSt[:], ALU.mult, [RIM, CSt], [T2])
                    tt("vector", sb[:, 1, 1:257], T1[:], T2[:], ALU.add, [T1, T2], [sb])
                for tc in range(NTC):
                    psy = next_ps()
                    for j in range(8):
                        oc = slice(j * 64, j * 64 + 64)
                        first = True
                        for q4 in range(4):
                            for reim in range(2):
                                op("tensor", lambda e, psy=psy, oc=oc, j=j, q4=q4, reim=reim, tc=tc, first=first, sset=sset: e.matmul(
                                    psy[:, oc], lhsT=WC[:, j, q4, reim, :], rhs=SBF[sset][q4][:, reim, 64 * tc:64 * tc + 64], start=first, stop=False),
                                   reads=[WBUF, SBF[sset][q4]], writes=[psy], signal=False)
                                first = False
                        for i in range(j + 1):
                            op("tensor", lambda e, psy=psy, oc=oc, j=j, i=i, tc=tc, qt=qt: e.matmul(psy[:, oc], lhsT=KD[:, j - i, :], rhs=USSM[qt][tc][:, i, :], start=False, stop=(i == j)),
                               reads=[KD, USSM[qt][tc]], writes=[psy], signal=(j == 7 and i == j))
                    op("scalar", lambda e, psy=psy, tc=tc, qt=qt: e.activation(out=H[qt][tc][:].rearrange("p (m j) -> p m j", j=8), in_=psy[:].rearrange("p (j m) -> p m j", j=8),
                                                                        func=AF.Gelu_apprx_tanh), reads=[psy], writes=[H[qt][tc]])

            if dbg and l == 0:
                P.barrier()
                P.dma("sync", lambda e: e.dma_start(out=d_dbgw, in_=wbuf_t[:, :]), g_out)
                for c in range(8):
                    P.dma("sync", lambda e, c=c: e.dma_start(out=d_dbgh[1, :, c, :], in_=h_t[:, c, :]), g_out)
                P.barrier()
            for tc in range(NTC):
                pss = [next_ps() for _ in range(4)]
                for mo in range(4):
                    for k in range(4):
                        op("tensor", lambda e, mo=mo, k=k, tc=tc, pss=pss: e.matmul(pss[mo][:], lhsT=wglu_t[:, k, mo * 128:(mo + 1) * 128], rhs=H[k][tc][:], start=(k == 0), stop=(k == 3)),
                           reads=[WGLU, H[k][tc]], writes=[pss[mo]], signal=(k == 3))
                for mo in range(4):
                    sg = SGT[mo % 2]
                    op("scalar", lambda e, mo=mo, sg=sg, pss=pss: e.activation(out=sg[:], in_=pss[mo][:], func=AF.Sigmoid, bias=vec_t[:, l, 0, mo:mo + 1]), reads=[pss[mo], VEC], writes=[sg])
                    tt("vector", H[mo][tc][:], H[mo][tc][:], sg[:], ALU.mult, [H[mo][tc], sg], [H[mo][tc]])

            dumph(l)
            if dbg and l == 0:
                P.barrier()
                P.dma("sync", lambda e: e.dma_start(out=d_dbgs, in_=scr_t[:, :]), g_out)
                P.barrier()
            load_wbuf(d_wout[l])
            for tc in range(NTC):
                for mc in range(8):
                    ps = next_ps()
                    for k in range(8):
                        op("tensor", lambda e, ps=ps, k=k, mc=mc, tc=tc: e.matmul(ps[:], lhsT=wbuf_t[:, k * 1024 + mc * 128:k * 1024 + mc * 128 + 128], rhs=H[k][tc][:],
                                                                                   start=(k == 0), stop=(k == 7)),
                           reads=[WBUF, H[k][tc]], writes=[ps], signal=(k == 7))
                    if dbg and False:
                        pass
                    tt("vector", XR[mc][tc][:], ps[:], XR[mc][tc][:], ALU.add, [ps, XR[mc][tc]], [XR[mc][tc]])

        scr_u = {}

        halfpi_t = P.sbuf("halfpi", [128, 1], F32)
        HALFPI_T = T(halfpi_t[:], "halfpi")
        HALFPI = halfpi_t
        op("vector", lambda e: e.memset(halfpi_t[:], math.pi / 2.0), writes=[HALFPI_T])
        for mc in range(4):
            base = mc * (4 * 8 * 64)
            scr_u[mc] = scr_t[:, base:base + 4 * 8 * 64].rearrange("p (t i m) -> p t i m", t=4, i=8)

        class FFNState:
            pass

        def ffn_setup():
            cv = Carver()
            st = FFNState()
            st.GU = [cv.take([2, 8, 128], BF16, f"gu{s}") for s in range(NS)]
            st.DN = [cv.take([1024], BF16, f"dn{s}") for s in range(NS)]
            st.A = [[cv.take([512], BF16, f"a{p_}_{i}") for i in range(4)] for p_ in range(2)]
            st.SG = [cv.take([512], BF16, f"sg{i}") for i in range(2)]
            off_a = cv.off - 4096 - 1024
            off_g = cv.off
            st.GATEBC = [[cv.take([512], F32, f"gbc{p_}_{tc}") for tc in range(NTC)] for p_ in range(2)]
            cvg = Carver(); cvg.off = off_g
            st.RSTD = [cvg.take([512], F32, f"rstd{tc}") for tc in range(NTC)]
            st.SQB = [cvg.take([512], BF16, f"fsqb{i}") for i in range(4)]
            st.TMPL = cvg.take([512], F32, "ftmpl")
            assert cvg.off <= cv.off
            cva_ = Carver(); cva_.off = off_a
            st.HN32 = cva_.take([L], F32, "hn32")
            st.LG = cv.take([16, 8], F32, "lg")
            st.LG2 = cv.take([16, 8], F32, "lg2")
            st.EQ = cv.take([16, 8], F32, "eq")
            st.G = cv.take([16, 8], F32, "gates")
            st.M1 = cv.take([16], F32, "m1")
            st.M2 = cv.take([16], F32, "m2")
            st.DG = [cv.take([128], F32, f"dg{i}") for i in range(4)]
            st.issued = 0
            st.chunks = []
            return st

        def ffn_issue_loads(st, upto):
            while st.issued < min(upto, len(st.chunks)):
                ci = st.issued
                gu_src, dn_src = st.chunks[ci]
                s = ci % NS
                P.dma("gpsimd", lambda e, s=s, gu_src=gu_src: e.dma_start(out=st.GU[s][:], in_=gu_src), P.grp(f"gu{s}"), writes=[st.GU[s]])
                P.dma("gpsimd", lambda e, s=s, dn_src=dn_src: e.dma_start(out=st.DN[s][:], in_=dn_src), P.grp(f"dn{s}"), writes=[st.DN[s]])
                st.issued += 1

        def ffn_run(st, base_ci, gate_set=None, mid_hook=None):
            f0 = 0
            for gi_, gsz in enumerate(GROUPS):
                if gi_ == 3 and mid_hook is not None:
                    mid_hook()
                ffn_issue_loads(st, base_ci + f0 + NS)
                for tc in range(NTC):
                    par = tc % 2
                    for fl in range(gsz):
                        ci = base_ci + f0 + fl
                        s = ci % NS
                        psg = next_ps()
                        psu = next_ps()
                        for k in range(8):
                            op("tensor", lambda e, psg=psg, s=s, k=k, tc=tc: e.matmul(psg[:], lhsT=st.GU[s][:, 0, k, :], rhs=H[k][tc][:], start=(k == 0), stop=(k == 7)),
                               reads=[st.GU[s], H[k][tc]], writes=[psg], signal=(k == 7))
                        for k in range(8):
                            op("tensor", lambda e, psu=psu, s=s, k=k, tc=tc: e.matmul(psu[:], lhsT=st.GU[s][:, 1, k, :], rhs=H[k][tc][:], start=(k == 0), stop=(k == 7)),
                               reads=[st.GU[s], H[k][tc]], writes=[psu], signal=(k == 7))
                        sg = st.SG[(tc * 4 + fl) % 2]
                        a = st.A[par][fl]
                        op("scalar", lambda e, psg=psg, sg=sg: e.activation(out=sg[:], in_=psg[:], func=AF.Silu), reads=[psg], writes=[sg])
                        if gate_set is not None:
                            gb = st.GATEBC[gate_set][tc]
                            op("gpsimd", lambda e, sg=sg, gb=gb: e.tensor_tensor(out=sg[:], in0=sg[:], in1=gb[:], op=ALU.mult), reads=[sg, gb], writes=[sg])
                        op("vector", lambda e, psu=psu, sg=sg, a=a: e.tensor_tensor(out=a[:], in0=psu[:], in1=sg[:], op=ALU.mult), reads=[psu, sg], writes=[a])
                    for mo in range(8):
                        ps = next_ps()
                        for fl in range(gsz):
                            ci = base_ci + f0 + fl
                            s = ci % NS
                            op("tensor", lambda e, ps=ps, s=s, mo=mo, fl=fl, par=par, gsz=gsz: e.matmul(ps[:], lhsT=st.DN[s][:, mo * 128:(mo + 1) * 128], rhs=st.A[par][fl][:],
                                                                                                   start=(fl == 0), stop=(fl == gsz - 1)),
                               reads=[st.DN[s], st.A[par][fl]], writes=[ps], signal=(fl == gsz - 1))
                        xr = XR[mo][tc]
                        op("vector", lambda e, ps=ps, xr=xr: e.tensor_tensor(out=xr[:], in0=ps[:], in1=xr[:], op=ALU.add), reads=[ps, xr], writes=[xr])
                f0 += gsz

        def dump(idx):
            if not dbg:
                return
            for c in range(8):
                P.dma("sync", lambda e, c=c: e.dma_start(out=d_dbg[idx, :, c, :], in_=xres_t[:, c, :]), g_out, reads=XR[c])

        def dumph(idx):
            if not dbg:
                return
            for c in range(8):
                P.dma("sync", lambda e, c=c: e.dma_start(out=d_dbgh[idx, :, c, :], in_=h_t[:, c, :]), g_out, reads=H[c])

        if "mix0" in stages:
            mixer(0)
            dump(0)
        P.barrier()
        if "ffn0" in stages:
            st = ffn_setup()
            st.chunks = [(d_fgu[f], d_fdn[f]) for f in range(NF)]
            ffn_issue_loads(st, NS)
            rmsnorm(1, st.SQB, None, st.TMPL, norm_to_h(1), rstd_keep=st.RSTD)
            ffn_run(st, 0, None)
            dump(1)
        P.barrier()
        if "mix1" in stages:
            mixer(1)
            dump(2)
        P.barrier()
        if "ffn1" in stages:
            NSM = 4
            GROUPS_S = [2] * 11
            cv = Carver()
            st = FFNState()
            st.GU = [cv.take([2, 8, 128], BF16, f"mgu{s_}") for s_ in range(NSM)]
            st.DN = [cv.take([1024], BF16, f"mdn{s_}") for s_ in range(NSM)]
            off_a = cv.off
            SW = 640
            NOB = bool(os.environ.get("MOE_TEST_NOB"))
            CAP0 = 512 if NOB else SW
            st.A = [[cv.take([SW], BF16, f"ma{p_}_{i}") for i in range(2)] for p_ in range(2)]
            st.SG = [T(wglu_t[:, :, :].rearrange("p a b -> p (a b)")[:, i * SW:(i + 1) * SW], f"msg{i}") for i in range(2)]
            off_hc = cv.off
            HC = cv.take([8, SW], BF16, "hc")
            YSM = cv.take([5, 1024], BF16, "ysm")
            off_sel = cv.off
            SEL = [cv.take([SW], BF16, f"sel{i}") for i in range(4)]
            off_selt = cv.off
            SELT = [cv.take([512], BF16, f"selt{i}") for i in range(5)]
            cvs = Carver(); cvs.off = off_sel
            st.STG = [cvs.take([512], BF16, f"stg{i}") for i in range(2)]
            POSBC = [cv.take([512], F32, f"posbc{tc}") for tc in range(NTC)]
            st.GATEBC = [[cv.take([512], F32, f"mgbc{tc}") for tc in range(NTC)]]
            IOTA512 = cv.take([SW], F32, "iota512")
            st.LG = cv.take([16, 8], F32, "lg")
            st.LG2 = cv.take([16, 8], F32, "lg2")
            st.EQ = cv.take([16, 8], F32, "eq")
            st.G = cv.take([16, 8], F32, "gates")
            POSM = T(st.LG2.ap, "posm")
            TOT = T(st.LG.ap, "tot")
            OFFS = cv.take([16, 8], F32, "offs")
            st.M1 = cv.take([16], F32, "m1")
            st.M2 = cv.take([16], F32, "m2")
            NE = cv.take([8], F32, "ne")
            FL = cv.take([8, 4], F32, "fl")
            FLI = cv.take([8, 4], I32, "fli")
            FLANY = cv.take([2], I32, "flany")
            cvd = Carver(); cvd.off = off_selt
            st.DG = [cvd.take([128], F32, f"dg{i}") for i in range(2)]
            UT = T(poolw_t[:, :, :].rearrange("p a b -> p (a b)")[:, 0:256].bitcast(F32), "ut")
            IDENTB = cv.take([128], BF16, "identb")
            cvt = Carver(); cvt.off = off_a
            TMPS2 = [cvt.take([512], F32, f"tmps{i}") for i in range(2)]
            assert cvt.off <= off_a + 4 * SW
            cvn = Carver(); cvn.off = off_hc
            st.RSTD = [cvn.take([512], F32, f"mrstd{tc}") for tc in range(NTC)]
            st.SQB = [cvn.take([512], BF16, f"msqb{i}") for i in range(4)]
            st.TMPL = cvn.take([512], F32, "mtmpl")
            assert cvn.off <= off_hc + 8 * SW + 5120
            cvh = Carver(); cvh.off = 0
            st.HN32 = cvh.take([L], F32, "mhn32")
            YACC = [T(wbuf_t[:, :].bitcast(F32).rearrange("p (m n) -> p m n", m=8)[:, m, :], f"yacc{m}") for m in range(8)]
            YACC2 = T(ssmbc_t[:, :, :, :].rearrange("p a b c -> p (a b c)").rearrange("p (m n) -> p m n", m=8), "yacc2")
            hT_view = h_t[:, :, :].rearrange("p a b -> p (a b)").rearrange("p (k c) -> p k c", k=16)
            HTK = [T(hT_view[:, k, :], f"ht{k}") for k in range(16)]

            op("gpsimd", lambda e: e.iota(IOTA512[:], [[1, SW]], base=0, channel_multiplier=0, allow_small_or_imprecise_dtypes=True), writes=[IOTA512])
            op("vector", lambda e: e.tensor_scalar(out=UT[:], in0=iotaf_t[:], scalar1=iotap_t[:, 0:1], scalar2=None, op0=ALU.is_gt), reads=[IOTAF, IOTAP], writes=[UT])
            op("vector", lambda e: e.tensor_copy(out=IDENTB[:], in_=ident_t[:]), reads=[IDENT], writes=[IDENTB])

            cnt_ = [0]

            def sink_t(c, tc, r):
                stg = st.STG[cnt_[0] % 2]
                cnt_[0] += 1
                op("vector", lambda e: e.scalar_tensor_tensor(out=stg[:], in0=XR[c][tc][:], scalar=gains_t[:, 3, c:c + 1], in1=r[:], op0=ALU.mult, op1=ALU.mult),
                   reads=[XR[c][tc], GAINS, r], writes=[stg])
                ps = next_ps()
                psb = ps[:].bitcast(BF16)
                for j in range(4):
                    op("tensor", lambda e, j=j: e.transpose(psb[:, j * 128:(j + 1) * 128], stg[:, j * 128:(j + 1) * 128], IDENTB[:]), reads=[stg, IDENTB], writes=[ps], signal=(j == 3))
                op("scalar", lambda e: e.activation(out=hT_view[:, 4 * tc:4 * tc + 4, c * 128:(c + 1) * 128], in_=psb[:, 0:512].rearrange("p (a n) -> p a n", a=4), func=AF.Copy),
                   reads=[ps], writes=[HTK[4 * tc + j] for j in range(4)])

            rmsnorm(3, st.SQB, None, st.TMPL, sink_t, rstd_keep=st.RSTD)

            psl = next_ps()
            hn_v = st.HN32[:].rearrange("p (b c n) -> p b c n", b=2, c=8)
            g3b = gains_t[:, 3, :].unsqueeze(2).to_broadcast([128, 8, 128])
            for tl in range(16):
                tc_, o_ = tl // 4, (tl % 4) * 128
                hv = hn_v[:, tl % 2]
                op("vector", lambda e, hv=hv, tl=tl: e.tensor_tensor(out=hv, in0=xres_t[:, :, tl * 128:(tl + 1) * 128], in1=g3b, op=ALU.mult),
                   reads=[XR[c][tc_] for c in range(8)] + [GAINS], writes=[st.HN32])
                op("vector", lambda e, hv=hv, tc_=tc_, o_=o_: e.tensor_tensor(out=hv, in0=hv, in1=st.RSTD[tc_][:, o_:o_ + 128].unsqueeze(1).to_broadcast([128, 8, 128]), op=ALU.mult),
                   reads=[st.RSTD[tc_], st.HN32], writes=[st.HN32])
                for c in range(8):
                    op("tensor", lambda e, hv=hv, c=c, tl=tl: e.matmul(psl[:, tl * 8:(tl + 1) * 8], lhsT=hv[:, c, :], rhs=router_t[:, c, :], start=(c == 0), stop=(c == 7)),
                       reads=[st.HN32, ROUTER], writes=[psl], signal=(c == 7))
            lg3 = psl[:, 0:128].rearrange("p (t e) -> p t e", e=8)
            op("vector", lambda e: e.tensor_copy(out=st.LG[:], in_=lg3), reads=[psl], writes=[st.LG])
            op("vector", lambda e: e.tensor_reduce(out=st.M1[:], in_=st.LG[:], axis=AX.X, op=ALU.max), reads=[st.LG], writes=[st.M1])
            m1b = st.M1[:].unsqueeze(2).to_broadcast([128, 16, 8])
            op("vector", lambda e: e.tensor_tensor(out=st.EQ[:], in0=st.LG[:], in1=m1b, op=ALU.is_equal), reads=[st.LG, st.M1], writes=[st.EQ])
            op("vector", lambda e: e.scalar_tensor_tensor(out=st.LG2[:], in0=st.EQ[:], scalar=-1e30, in1=st.LG[:], op0=ALU.mult, op1=ALU.add), reads=[st.EQ, st.LG], writes=[st.LG2])
            op("vector", lambda e: e.tensor_reduce(out=st.M2[:], in_=st.LG2[:], axis=AX.X, op=ALU.max), reads=[st.LG2], writes=[st.M2])
            m2b = st.M2[:].unsqueeze(2).to_broadcast([128, 16, 8])
            op("vector", lambda e: e.tensor_tensor(out=st.EQ[:], in0=st.LG[:], in1=m2b, op=ALU.is_ge), reads=[st.LG, st.M2], writes=[st.EQ])
            op("vector", lambda e: e.tensor_tensor(out=st.LG2[:], in0=st.LG[:], in1=m1b, op=ALU.subtract), reads=[st.LG, st.M1], writes=[st.LG2])
            op("scalar", lambda e: e.activation(out=st.LG2[:], in_=st.LG2[:], func=AF.Exp), reads=[st.LG2], writes=[st.LG2])
            op("vector", lambda e: e.tensor_tensor(out=st.LG2[:], in0=st.LG2[:], in1=st.EQ[:], op=ALU.mult), reads=[st.LG2, st.EQ], writes=[st.LG2])
            op("vector", lambda e: e.tensor_reduce(out=st.M1[:], in_=st.LG2[:], axis=AX.X, op=ALU.add), reads=[st.LG2], writes=[st.M1])
            op("vector", lambda e: e.reciprocal(out=st.M1[:], in_=st.M1[:]), reads=[st.M1], writes=[st.M1])
            op("vector", lambda e: e.tensor_tensor(out=st.G[:], in0=st.LG2[:], in1=m1b, op=ALU.mult), reads=[st.LG2, st.M1], writes=[st.G])

            psw = next_ps()
            eq2 = st.EQ[:].rearrange("p t e -> p (t e)")
            op("tensor", lambda e: e.matmul(psw[:, 0:128], lhsT=UT[:], rhs=eq2, start=True, stop=True), reads=[UT, st.EQ], writes=[psw])
            op("tensor", lambda e: e.matmul(psw[:, 128:256], lhsT=onesf_t[:], rhs=eq2, start=True, stop=True), reads=[ONESF, st.EQ], writes=[psw])
            op("vector", lambda e: e.tensor_copy(out=TOT[:], in_=psw[:, 128:256].rearrange("p (t e) -> p t e", e=8)), reads=[psw], writes=[TOT])
            op("vector", lambda e: e.memset(OFFS[:, 0, :], 0.0), writes=[OFFS])
            for k in range(15):
                op("vector", lambda e, k=k: e.tensor_tensor(out=OFFS[:, k + 1, :], in0=OFFS[:, k, :], in1=TOT[:, k, :], op=ALU.add), reads=[OFFS, TOT], writes=[OFFS])
            op("vector", lambda e: e.tensor_tensor(out=NE[:], in0=OFFS[:, 15, :], in1=TOT[:, 15, :], op=ALU.add), reads=[OFFS, TOT], writes=[NE])
            op("vector", lambda e: e.tensor_tensor(out=POSM[:], in0=psw[:, 0:128].rearrange("p (t e) -> p t e", e=8), in1=OFFS[:], op=ALU.add), reads=[psw, OFFS], writes=[POSM])
            op("vector", lambda e: e.tensor_single_scalar(out=POSM[:], in_=POSM[:], scalar=1.0e6, op=ALU.add), reads=[POSM], writes=[POSM])
            op("vector", lambda e: e.tensor_tensor(out=POSM[:], in0=POSM[:], in1=st.EQ[:], op=ALU.mult), reads=[POSM, st.EQ], writes=[POSM])
            op("vector", lambda e: e.tensor_single_scalar(out=POSM[:], in_=POSM[:], scalar=-1.0e6, op=ALU.add), reads=[POSM], writes=[POSM])
            for cch in range(4):
                op("vector", lambda e, cch=cch: e.tensor_single_scalar(out=FL[:, :, cch], in_=NE[:], scalar=float(0 if cch == 0 else CAP0 + 512 * (cch - 1)) + 0.5, op=ALU.is_gt), reads=[NE], writes=[FL])
            op("vector", lambda e: e.tensor_reduce(out=st.M2[:, 0:1], in_=NE[:], axis=AX.X, op=ALU.max), reads=[NE], writes=[st.M2])
            op("vector", lambda e: e.tensor_single_scalar(out=st.M2[:, 1:2], in_=st.M2[:, 0:1], scalar=float(CAP0) + 0.5, op=ALU.is_gt), reads=[st.M2], writes=[st.M2])
            op("vector", lambda e: e.tensor_copy(out=FLANY[:, 0:1], in_=st.M2[:, 1:2]), reads=[st.M2], writes=[FLANY])
            op("vector", lambda e: e.tensor_copy(out=FLI[:], in_=FL[:]), reads=[FL], writes=[FLI])

            def build_bc(src, dst, ex):
                for tc in range(NTC):
                    ps = next_ps()
                    for t4 in range(4):
                        tl = tc * 4 + t4
                        dg = st.DG[t4 % 2]
                        op("vector", lambda e, dg=dg, tl=tl: e.tensor_scalar(out=dg[:], in0=ident_t[:], scalar1=src[:, tl, ex:ex + 1], scalar2=None, op0=ALU.mult), reads=[IDENT, src], writes=[dg])
                        op("tensor", lambda e, ps=ps, dg=dg, t4=t4: e.matmul(ps[:, t4 * 128:(t4 + 1) * 128], lhsT=onesf_t[:], rhs=dg[:], start=True, stop=True),
                           reads=[ONESF, dg], writes=[ps], signal=True)
                    gb = dst[tc]
                    op("scalar", lambda e, ps=ps, gb=gb: e.activation(out=gb[:], in_=ps[:], func=AF.Copy), reads=[ps], writes=[gb])

            gci = [0]

            pre_state = {}

            def prefetch_first(ex):
                slot_of = [(gci[0] + f) % NSM for f in range(NF)]
                pre_state[ex] = (gci[0], slot_of)
                gci[0] += NF
                ex_dep = [FLI.last_w]
                for f in range(NSM):
                    s_ = slot_of[f]
                    gu_src, dn_src = d_mgu[ex * NF + f], d_mdn[ex * NF + f]
                    P.dma("gpsimd", lambda e, s_=s_, gu_src=gu_src: e.dma_start(out=st.GU[s_][:], in_=gu_src), P.grp(f"mgu{s_}"), writes=[st.GU[s_]], extra=ex_dep)
                    P.dma("gpsimd", lambda e, s_=s_, dn_src=dn_src: e.dma_start(out=st.DN[s_][:], in_=dn_src), P.grp(f"mdn{s_}"), writes=[st.DN[s_]], extra=ex_dep)

            def chunk_pass(ex, cch, flag_ap, mid_hook=None):
                base = 0 if cch == 0 else CAP0 + 512 * (cch - 1)
                has_b = (cch == 0) and not NOB
                nsl = SW if has_b else 512
                nst = 5 if has_b else 4
                loads = [(d_mgu[ex * NF + f], d_mdn[ex * NF + f]) for f in range(NF)]
                if cch == 0 and ex in pre_state:
                    slot_of = pre_state[ex][1]
                    issued = [NSM]
                else:
                    slot_of = [(gci[0] + f) % NSM for f in range(NF)]
                    gci[0] += NF
                    issued = [0]
                P.cond_begin(flag_ap, FLI)

                def issue(upto):
                    while issued[0] < min(upto, NF):
                        f = issued[0]
                        s_ = slot_of[f]
                        gu_src, dn_src = loads[f]
                        P.dma("gpsimd", lambda e, s_=s_, gu_src=gu_src: e.dma_start(out=st.GU[s_][:], in_=gu_src), P.grp(f"mgu{s_}"), writes=[st.GU[s_]])
                        P.dma("gpsimd", lambda e, s_=s_, dn_src=dn_src: e.dma_start(out=st.DN[s_][:], in_=dn_src), P.grp(f"mdn{s_}"), writes=[st.DN[s_]])
                        issued[0] += 1

                issue(NSM)
                for half in range(2):
                    pss = [next_ps() for _ in range(4)]
                    psbl = [next_ps() for _ in range(4)] if has_b else None
                    for k in range(16):
                        sel = SEL[k % 4]
                        op("vector", lambda e, sel=sel, k=k: e.tensor_scalar(out=sel[:, 0:nsl], in0=IOTA512[:, 0:nsl], scalar1=float(base), scalar2=POSM[:, k, ex:ex + 1], op0=ALU.add, op1=ALU.is_equal),
                           reads=[IOTA512, POSM], writes=[sel])
                        for mi in range(4):
                            m = half * 4 + mi
                            op("tensor", lambda e, sel=sel, k=k, mi=mi, m=m, pss=pss: e.matmul(pss[mi][:], lhsT=HTK[k][:, m * 128:(m + 1) * 128], rhs=sel[:, 0:512], start=(k == 0), stop=(k == 15)),
                               reads=[HTK[k], sel], writes=[pss[mi]], signal=(mi == 3 and not has_b))
                        if has_b:
                            for mi in range(4):
                                m = half * 4 + mi
                                op("tensor", lambda e, sel=sel, k=k, mi=mi, m=m, psbl=psbl: e.matmul(psbl[mi][:, 0:128], lhsT=HTK[k][:, m * 128:(m + 1) * 128], rhs=sel[:, 512:SW],
                                                                                                      start=(k == 0), stop=(k == 15)),
                                   reads=[HTK[k], sel], writes=[psbl[mi]], signal=(mi == 3))
                    for mi in range(4):
                        m = half * 4 + mi
                        op("scalar", lambda e, mi=mi, m=m, pss=pss: e.activation(out=HC[:, m, 0:512], in_=pss[mi][:], func=AF.Copy), reads=[pss[mi]], writes=[HC])
                    if has_b:
                        for mi in range(4):
                            m = half * 4 + mi
                            op("scalar", lambda e, mi=mi, m=m, psbl=psbl: e.activation(out=HC[:, m, 512:SW], in_=psbl[mi][:, 0:128], func=AF.Copy), reads=[psbl[mi]], writes=[HC])
                f0 = 0
                for gi_, gsz in enumerate(GROUPS_S):
                    issue(f0 + NSM)
                    par = gi_ % 2
                    for fl in range(gsz):
                        s_ = slot_of[f0 + fl]
                        psg = next_ps()
                        psu = next_ps()
                        psb = next_ps() if has_b else None
                        for k in range(8):
                            op("tensor", lambda e, psg=psg, s_=s_, k=k: e.matmul(psg[:], lhsT=st.GU[s_][:, 0, k, :], rhs=HC[:, k, 0:512], start=(k == 0), stop=(k == 7)),
                               reads=[st.GU[s_], HC], writes=[psg], signal=(k == 7))
                        for k in range(8):
                            op("tensor", lambda e, psu=psu, s_=s_, k=k: e.matmul(psu[:], lhsT=st.GU[s_][:, 1, k, :], rhs=HC[:, k, 0:512], start=(k == 0), stop=(k == 7)),
                               reads=[st.GU[s_], HC], writes=[psu], signal=(k == 7))
                        if has_b:
                            for r_ in range(2):
                                for k in range(8):
                                    op("tensor", lambda e, psb=psb, s_=s_, k=k, r_=r_: e.matmul(psb[:, r_ * 128:(r_ + 1) * 128], lhsT=st.GU[s_][:, r_, k, :], rhs=HC[:, k, 512:SW], start=(k == 0), stop=(k == 7)),
                                       reads=[st.GU[s_], HC], writes=[psb], signal=(k == 7))
                        sg = st.SG[fl % 2]
                        a = st.A[par][fl]
                        op("scalar", lambda e, psg=psg, sg=sg: e.activation(out=sg[:, 0:512], in_=psg[:], func=AF.Silu), reads=[psg], writes=[sg])
                        if has_b:
                            op("scalar", lambda e, psb=psb, sg=sg: e.activation(out=sg[:, 512:SW], in_=psb[:, 0:128], func=AF.Silu), reads=[psb], writes=[sg])
                        op("vector", lambda e, psu=psu, sg=sg, a=a: e.tensor_tensor(out=a[:, 0:512], in0=psu[:], in1=sg[:, 0:512], op=ALU.mult), reads=[psu, sg], writes=[a])
                        if has_b:
                            op("vector", lambda e, psb=psb, sg=sg, a=a: e.tensor_tensor(out=a[:, 512:SW], in0=psb[:, 128:256], in1=sg[:, 512:SW], op=ALU.mult), reads=[psb, sg], writes=[a])
                    psd = [next_ps(), next_ps()] if has_b else None
                    for m in range(8):
                        ps = next_ps()
                        while psd is not None and (ps is psd[0] or ps is psd[1]):
                            ps = next_ps()
                        for fl in range(gsz):
                            s_ = slot_of[f0 + fl]
                            op("tensor", lambda e, ps=ps, s_=s_, m=m, fl=fl, par=par, gsz=gsz: e.matmul(ps[:], lhsT=st.DN[s_][:, m * 128:(m + 1) * 128], rhs=st.A[par][fl][:, 0:512],
                                                                                                     start=(fl == 0), stop=(fl == gsz - 1)),
                               reads=[st.DN[s_], st.A[par][fl]], writes=[ps], signal=(fl == gsz - 1))
                        if has_b:
                            for fl in range(gsz):
                                s_ = slot_of[f0 + fl]
                                op("tensor", lambda e, psd=psd, s_=s_, m=m, fl=fl, par=par, gsz=gsz: e.matmul(psd[m // 4][:, (m % 4) * 128:(m % 4 + 1) * 128], lhsT=st.DN[s_][:, m * 128:(m + 1) * 128],
                                                                                                           rhs=st.A[par][fl][:, 512:SW], start=(fl == 0), stop=(fl == gsz - 1)),
                                   reads=[st.DN[s_], st.A[par][fl]], writes=[psd[m // 4]], signal=(fl == gsz - 1))
                        ya = YACC[m]
                        if gi_ == 0:
                            op("scalar", lambda e, ps=ps, ya=ya: e.activation(out=ya[:], in_=ps[:], func=AF.Copy), reads=[ps], writes=[ya])
                        else:
                            op("vector", lambda e, ps=ps, ya=ya: e.tensor_tensor(out=ya[:], in0=ps[:], in1=ya[:], op=ALU.add), reads=[ps, ya], writes=[ya])
                    if has_b:
                        for hf in range(2):
                            y2 = YACC2[:, hf * 4:hf * 4 + 4, :]
                            pv = psd[hf][:].rearrange("p (a n) -> p a n", a=4)
                            if gi_ == 0:
                                op("scalar", lambda e, pv=pv, y2=y2: e.activation(out=y2, in_=pv, func=AF.Copy), reads=[psd[hf]], writes=[YACC2])
                            else:
                                op("vector", lambda e, pv=pv, y2=y2: e.tensor_tensor(out=y2, in0=pv, in1=y2, op=ALU.add), reads=[psd[hf], YACC2], writes=[YACC2])
                    f0 += gsz
                P.cond_end()
                if mid_hook is not None:
                    mid_hook()
                P.cond_begin(flag_ap, FLI)
                for st4 in range(nst):
                    for hf in range(2):
                        ps = next_ps()
                        for mi in range(4):
                            m = hf * 4 + mi
                            if st4 < 4:
                                op("tensor", lambda e, ps=ps, mi=mi, m=m, st4=st4: e.transpose(ps[:, mi * 128:(mi + 1) * 128], YACC[m][:, st4 * 128:(st4 + 1) * 128], ident_t[:]),
                                   reads=[YACC[m], IDENT], writes=[ps], signal=(mi == 3))
                            else:
                                op("tensor", lambda e, ps=ps, mi=mi, m=m: e.transpose(ps[:, mi * 128:(mi + 1) * 128], YACC2[:, m, :], ident_t[:]),
                                   reads=[YACC2, IDENT], writes=[ps], signal=(mi == 3))
                        op("scalar", lambda e, ps=ps, st4=st4, hf=hf: e.activation(out=YSM[:, st4, hf * 512:(hf + 1) * 512], in_=ps[:], func=AF.Copy), reads=[ps], writes=[YSM])
                for tcx in range(NTC):
                    for st4 in range(nst):
                        op("vector", lambda e, st4=st4, tcx=tcx: e.tensor_scalar(out=SELT[st4][:], in0=POSBC[tcx][:], scalar1=float(base + 128 * st4), scalar2=iotap_t[:, 0:1],
                                                                                  op0=ALU.subtract, op1=ALU.is_equal), reads=[POSBC[tcx], IOTAP], writes=[SELT[st4]])
                    for m in range(8):
                        ps = next_ps()
                        for st4 in range(nst):
                            op("tensor", lambda e, ps=ps, st4=st4, m=m: e.matmul(ps[:], lhsT=YSM[:, st4, m * 128:(m + 1) * 128], rhs=SELT[st4][:], start=(st4 == 0), stop=(st4 == nst - 1)),
                               reads=[YSM, SELT[st4]], writes=[ps], signal=(st4 == nst - 1))
                        gb = st.GATEBC[0][tcx]
                        xr = XR[m][tcx]
                        tq = TMPS2[m % 2]
                        op("scalar", lambda e, ps=ps, tq=tq: e.activation(out=tq[:], in_=ps[:], func=AF.Copy), reads=[ps], writes=[tq])
                        op("gpsimd", lambda e, gb=gb, tq=tq: e.tensor_tensor(out=tq[:], in0=tq[:], in1=gb[:], op=ALU.mult), reads=[tq, gb], writes=[tq])
                        op("vector", lambda e, xr=xr, tq=tq: e.tensor_tensor(out=xr[:], in0=tq[:], in1=xr[:], op=ALU.add), reads=[tq, xr], writes=[xr])
                P.cond_end()

            prefetch_first(0)
            for ex in range(8):
                build_bc(st.G, st.GATEBC[0], ex)
                build_bc(POSM, POSBC, ex)
                chunk_pass(ex, 0, FLI[0:1, ex, 0:1], mid_hook=(lambda ex=ex: prefetch_first(ex + 1)) if ex < 7 else None)
            P.cond_begin(FLANY[0:1, 0:1], FLANY)
            for ex in range(8):
                P.cond_begin(FLI[0:1, ex, 1:2], FLI)
                build_bc(st.G, st.GATEBC[0], ex)
                build_bc(POSM, POSBC, ex)
                for cch in range(1, 4):
                    chunk_pass(ex, cch, FLI[0:1, ex, cch:cch + 1])
                P.cond_end()
            P.cond_end()
            dump(3)
        if "final" in stages:
            cv = Carver()
            cv.off = SCR_ELEMS - 16 * 1024
            P.barrier()
            OST = [cv.take([512], F32, f"ost{i}") for i in range(4)]
            SQB = [cv.take([512], BF16, f"osq{i}") for i in range(4)]
            RST = [cv.take([512], F32, f"orst{i}") for i in range(2)]
            TMPL2 = cv.take([512], F32, "otmpl")
            cnt = [0]
            last = [None]

            def sink(c, tc, r):
                oi = cnt[0] % 4
                o = OST[oi]
                cnt[0] += 1
                op("vector", lambda e: e.scalar_tensor_tensor(out=o[:], in0=XR[c][tc][:], scalar=gains_t[:, 4, c:c + 1], in1=r[:], op0=ALU.mult, op1=ALU.mult),
                   reads=[XR[c][tc], GAINS, r], writes=[o])
                last[0] = P.dma("sync", lambda e: e.dma_start(out=d_out[:, c, tc * TC:(tc + 1) * TC], in_=o[:]), P.grp(f"ost{oi}"), reads=[o])

            rmsnorm(4, SQB, RST, TMPL2, sink)
        else:
            last = [None]
            for c in range(8):
                last[0] = P.dma("sync", lambda e, c=c: e.dma_start(out=d_out[:, c, :], in_=xres_t[:, c, :]), P.grp(f"ost{c % 4}"), reads=XR[c])
        for g_ in P.groups:
            if g_.count > 0:
                P.wait_tok("sync", Tok(g_.sem, 16 * g_.count))
        P.barrier()
        P.finish()
    return nc


def _prep_shared(inp):
    f = np.float32

    def cmaj(v):
        return np.ascontiguousarray(np.asarray(v, f).reshape(8, 128).T)

    def c4(v):
        return np.ascontiguousarray(np.asarray(v, f).reshape(4, 128).T)

    gains = np.stack([cmaj(inp["norm_mix_g"][0]), cmaj(inp["norm_ffn_g"][0]), cmaj(inp["norm_mix_g"][1]), cmaj(inp["norm_ffn_g"][1]),
                      cmaj(inp["final_norm_g"])], axis=1)
    w_in = np.ascontiguousarray(np.asarray(inp["w_in"], f).reshape(2, 8, 128, 1024).transpose(0, 2, 1, 3))
    w_out = np.ascontiguousarray(np.asarray(inp["w_out"], f).reshape(2, 8, 128, 1024).transpose(0, 2, 1, 3))
    w_glu = np.ascontiguousarray(np.asarray(inp["ssm_w_glu"], f).reshape(2, 4, 128, 512).transpose(0, 2, 1, 3))
    vec = np.stack([np.stack([c4(inp["ssm_b_glu"][l]), c4(inp["pool_scale"][l]), c4(inp["ssm_d"][l])], axis=1) for l in range(2)], axis=1)
    pool_w = np.ascontiguousarray(np.asarray(inp["pool_w"], f).transpose(0, 2, 1, 3))

    def hn(v):
        return np.asarray(v, f).reshape(16, 2, 64).transpose(1, 2, 0).reshape(128, 16)

    ssm_a = np.zeros((2, 128, 3, 16), f)
    ssm_bc = np.zeros((2, 128, 4, 16, 16), f)
    for l in range(2):
        ssm_a[l, :, 0] = hn(np.repeat(np.asarray(inp["ssm_log_dt"][l], f)[:, None], 64, axis=1))
        ssm_a[l, :, 1] = hn(inp["ssm_a_re"][l])
        ssm_a[l, :, 2] = hn(inp["ssm_a_im"][l])
        for i, key in enumerate(["ssm_b_re", "ssm_b_im"]):
            b = np.asarray(inp[key][l], f).reshape(16, 2, 64, 16)
            ssm_bc[l, :, i] = b.transpose(1, 2, 0, 3).reshape(128, 16, 16)
        for i, key in enumerate(["ssm_c_re", "ssm_c_im"]):
            c = np.asarray(inp[key][l], f).reshape(16, 2, 16, 64)
            ssm_bc[l, :, 2 + i] = c.transpose(1, 3, 0, 2).reshape(128, 16, 16)

    def gu(wg, wu):
        a = np.stack([np.asarray(wg, f), np.asarray(wu, f)], axis=0)
        a = a.reshape(2, 8, 128, NF, 128)
        return np.ascontiguousarray(a.transpose(3, 2, 0, 1, 4))

    ffn_gu = gu(inp["ffn_w_gate"][0], inp["ffn_w_up"][0])
    ffn_dn = np.ascontiguousarray(np.asarray(inp["ffn_w_down"][0], f).reshape(NF, 128, 1024))
    moe_gu = np.concatenate([gu(inp["moe_w_gate"][0][e], inp["moe_w_up"][0][e]) for e in range(8)], axis=0)
    moe_dn = np.ascontiguousarray(np.asarray(inp["moe_w_down"][0], f).reshape(8 * NF, 128, 1024))
    router = np.ascontiguousarray(np.asarray(inp["router_w"][0], f).reshape(8, 128, 8).transpose(1, 0, 2))
    return dict(gains=np.ascontiguousarray(gains), w_in=w_in, w_out=w_out, w_glu=w_glu, vec512=np.ascontiguousarray(vec), pool_w=pool_w,
                ssm_a=ssm_a, ssm_bc=ssm_bc, ffn_gu=ffn_gu, ffn_dn=ffn_dn, moe_gu=moe_gu, moe_dn=moe_dn, router=router)


def _x_layout(xb):
    return np.ascontiguousarray(np.asarray(xb, np.float32).T.reshape(8, 128, L).transpose(1, 0, 2))


def _out_layout(y):
    return np.ascontiguousarray(y.transpose(1, 0, 2).reshape(1024, L).T)


def kernel(**inputs):
    shared = _prep_shared(inputs)
    x = np.asarray(inputs["x"], np.float32)
    nb = x.shape[0]
    nc = build_program()
    in_maps = []
    for b in range(nb):
        m = dict(shared)
        m["xT"] = _x_layout(x[b])
        in_maps.append(m)
    res = run_bass_kernel_spmd(nc, in_maps, core_ids=list(range(nb)))
    out = np.stack([_out_layout(np.asarray(r["yT"])) for r in res.results], axis=0)
    return out.astype(np.float32)
```

```python
import math
import os
import numpy as np
from contextlib import ExitStack
import concourse.bass as bass
import concourse.mybir as mybir
from concourse.bass_utils import run_bass_kernel_spmd

F32 = mybir.dt.float32
BF16 = mybir.dt.bfloat16
I32 = mybir.dt.int32
ALU = mybir.AluOpType
AF = mybir.ActivationFunctionType
AX = mybir.AxisListType
ENGS = ["tensor", "vector", "scalar", "gpsimd", "sync"]

L = 2048
NTC = 4
TC = 512
J = 8
MB = L // J
NF = 22
NS = 8
GROUPS = [4, 4, 4, 4, 3, 3]
TWO_PI = 2.0 * math.pi
CARVE_INFO = {}


class Tok:
    __slots__ = ("sem", "val")

    def __init__(self, sem, val):
        self.sem = sem
        self.val = val


class T:
    def __init__(self, ap, name=""):
        self.ap = ap
        self.name = name
        self.last_w = None
        self.readers = []

    def __getitem__(self, k):
        return self.ap[k]


class DmaGroup:
    def __init__(self, sem):
        self.sem = sem
        self.count = 0


class Prog:
    def __init__(self, nc, ctx):
        self.nc = nc
        self.ctx = ctx
        self.streams = {e: [] for e in ENGS}
        self.sem = {e: ctx.enter_context(nc.semaphore("s_" + e)) for e in ENGS}
        self.cnt = {e: 0 for e in ENGS}
        self.waited = {e: {} for e in ENGS}
        self.pend_r = {e: [] for e in ENGS}
        self.pend_w = {e: [] for e in ENGS}
        self.groups = []
        self.ps_i = 0

    def sbuf(self, name, shape, dtype):
        return self.ctx.enter_context(self.nc.sbuf_tensor("sb_" + name, shape, dtype))

    def dma_group(self, name):
        g = DmaGroup(self.ctx.enter_context(self.nc.semaphore(name)))
        self.groups.append(g)
        return g

    def grp(self, key):
        if not hasattr(self, "_grps"):
            self._grps = {}
        if key not in self._grps:
            self._grps[key] = self.dma_group("dg_" + key)
        return self._grps[key]

    def _waits(self, eng, reads, writes, extra=()):
        need = {}

        def add(tok):
            if tok is None:
                return
            k = id(tok.sem)
            if k not in need or need[k].val < tok.val:
                need[k] = tok

        for t in reads:
            add(t.last_w)
            for e2 in ENGS:
                if e2 != eng:
                    assert not any(t is w for w in self.pend_w[e2]), ("RAW on unsignaled write", t.name, eng, e2)
        for t in writes:
            add(t.last_w)
            for r in t.readers:
                add(r)
            for e2 in ENGS:
                if e2 != eng:
                    assert not any(t is w for w in self.pend_r[e2]), ("WAR on unsignaled read", t.name, eng, e2)
                    assert not any(t is w for w in self.pend_w[e2]), ("WAW on unsignaled write", t.name, eng, e2)
        for tok in extra:
            add(tok)
        own = self.sem[eng]
        for k, tok in need.items():
            if eng == "tensor" and tok.sem is own:
                continue
            if self.waited[eng].get(k, 0) >= tok.val:
                continue
            self.waited[eng][k] = tok.val
            self.streams[eng].append(("wait", tok.sem, tok.val))

    def _commit(self, tok, reads, writes):
        for t in writes:
            t.last_w = tok
            t.readers = []
        for t in reads:
            rs = [r for r in t.readers if r.sem is not tok.sem]
            rs.append(tok)
            t.readers = rs

    def op(self, eng, fn, reads=(), writes=(), signal=True, extra=()):
        reads = list(reads)
        writes = list(writes)
        self._waits(eng, reads, writes, extra)
        if not signal:
            self.pend_r[eng] += reads
            self.pend_w[eng] += writes
            self.streams[eng].append(("op", fn, None))
            return None
        self.cnt[eng] += 1
        tok = Tok(self.sem[eng], self.cnt[eng])
        self.streams[eng].append(("op", fn, (tok.sem, 1)))
        self._commit(tok, reads + self.pend_r[eng], writes + self.pend_w[eng])
        self.pend_r[eng] = []
        self.pend_w[eng] = []
        return tok

    def dma(self, eng, fn, grp, reads=(), writes=(), extra=()):
        reads = list(reads)
        writes = list(writes)
        self._waits(eng, reads, writes, extra)
        if grp.count > 0:
            self.wait_tok(eng, Tok(grp.sem, 16 * grp.count))
        grp.count += 1
        tok = Tok(grp.sem, 16 * grp.count)
        self.streams[eng].append(("op", fn, (grp.sem, 16)))
        self._commit(tok, reads, writes)
        return tok

    def cond_begin(self, flag_ap, flag_T):
        for e in ENGS:
            assert not self.pend_r[e] and not self.pend_w[e]
        for e in ("tensor", "vector", "scalar", "gpsimd"):
            self._waits(e, [flag_T], [])
        if not hasattr(self, "_cond_stack"):
            self._cond_stack = []
        self._cond_stack.append((flag_ap, self.streams, {e: dict(self.waited[e]) for e in ENGS}))
        self.streams = {e: [] for e in ENGS}

    def cond_end(self):
        flag_ap, outer, saved_waited = self._cond_stack.pop()
        inner = self.streams
        self.streams = outer
        for e in ENGS:
            assert not self.pend_r[e] and not self.pend_w[e]
            items = inner[e]
            if not items:
                continue
            incs = {}

            def acc(sem, n):
                k = id(sem)
                if k not in incs:
                    incs[k] = [sem, 0]
                incs[k][1] += n

            for it in items:
                if it[0] == "op" and it[2] is not None:
                    acc(it[2][0], it[2][1])
                elif it[0] == "cond":
                    for sem, n in it[3]:
                        acc(sem, n)
            self.streams[e].append(("cond", flag_ap, items, list(incs.values())))
            self.waited[e] = saved_waited[e]

    def wait_tok(self, eng, tok):
        k = id(tok.sem)
        if self.waited[eng].get(k, 0) < tok.val:
            self.waited[eng][k] = tok.val
            self.streams[eng].append(("wait", tok.sem, tok.val))

    def barrier(self):
        toks = [Tok(self.sem[e], self.cnt[e]) for e in ENGS if self.cnt[e] > 0]
        toks += [Tok(g.sem, 16 * g.count) for g in self.groups if g.count > 0]
        for e in ENGS:
            for t in toks:
                if t.sem is self.sem[e]:
                    continue
                self.wait_tok(e, t)

    def finish(self):
        nc = self.nc
        streams = self.streams

        def run_items(e, items, regbox):
            for item in items:
                if item[0] == "wait":
                    e.wait_ge(item[1], item[2])
                elif item[0] == "cond":
                    if regbox[0] is None:
                        regbox[0] = e.alloc_register("flagreg")
                    r = regbox[0]
                    e.reg_load(r, item[1])
                    with e.If_eq(r, 1):
                        run_items(e, item[2], regbox)
                    with e.Else():
                        for sem, n in item[3]:
                            e.sem_inc(sem, n)
                else:
                    ins = item[1](e)
                    if item[2] is not None:
                        ins.then_inc(item[2][0], item[2][1])

        def replay(name):
            def f(e):
                run_items(e, streams[name], [None])
            return f

        with nc.Block() as block:
            block.tensor(replay("tensor"))
            block.vector(replay("vector"))
            block.scalar(replay("scalar"))
            block.gpsimd(replay("gpsimd"))
            block.sync(replay("sync"))


def build_program(stages=("mix0", "ffn0", "mix1", "ffn1", "final"), dbg=False):
    nc = bass.Bass("TRN2", target_bir_lowering=False)

    def din(name, shape, dt=F32):
        return nc.dram_tensor(name, shape, dt, kind="ExternalInput").ap()

    d_x = din("xT", [128, 8, L])
    d_gains = din("gains", [128, 5, 8])
    d_win = din("w_in", [2, 128, 8, 1024])
    d_wout = din("w_out", [2, 128, 8, 1024])
    d_wglu = din("w_glu", [2, 128, 4, 512])
    d_vec = din("vec512", [128, 2, 3, 4])
    d_poolw = din("pool_w", [2, 128, 4, 128])
    d_ssma = din("ssm_a", [2, 128, 3, 16])
    d_ssmbc = din("ssm_bc", [2, 128, 4, 16, 16])
    d_fgu = din("ffn_gu", [NF, 128, 2, 8, 128])
    d_fdn = din("ffn_dn", [NF, 128, 1024])
    d_mgu = din("moe_gu", [8 * NF, 128, 2, 8, 128])
    d_mdn = din("moe_dn", [8 * NF, 128, 1024])
    d_router = din("router", [128, 8, 8])
    d_out = nc.dram_tensor("yT", [128, 8, L], F32, kind="ExternalOutput").ap()
    d_dbg = None
    if dbg:
        d_dbg = nc.dram_tensor("dbg", [4, 128, 8, L], F32, kind="ExternalOutput").ap()
        d_dbgh = nc.dram_tensor("dbgh", [2, 128, 8, L], BF16, kind="ExternalOutput").ap()
        d_dbgs = nc.dram_tensor("dbgs", [128, 41456], BF16, kind="ExternalOutput").ap()
        d_dbgw = nc.dram_tensor("dbgw", [128, 8192], BF16, kind="ExternalOutput").ap()

    with ExitStack() as ctx:
        P = Prog(nc, ctx)
        op = P.op

        xres_t = P.sbuf("xres", [128, 8, L], F32)
        h_t = P.sbuf("hbf", [128, 8, L], BF16)
        wbuf_t = P.sbuf("wbuf", [128, 8 * 1024], BF16)
        wglu_t = P.sbuf("wglu", [128, 4, 512], BF16)
        poolw_t = P.sbuf("poolw", [128, 4, 128], BF16)
        gains_t = P.sbuf("gains", [128, 5, 8], F32)
        vec_t = P.sbuf("vec", [128, 2, 3, 4], F32)
        router_t = P.sbuf("router", [128, 8, 8], F32)
        ssma_t = P.sbuf("ssma", [128, 3, 16], F32)
        ssmbc_t = P.sbuf("ssmbc", [128, 4, 16, 16], F32)
        ident_t = P.sbuf("ident", [128, 128], F32)
        onesb_t = P.sbuf("onesb", [128, 128], BF16)
        onesf_t = P.sbuf("onesf", [128, 128], F32)
        iota1_t = P.sbuf("iota1", [128, 256], F32)
        iotaf_t = P.sbuf("iotaf", [128, 128], F32)
        iotap_t = P.sbuf("iotap", [128, 1], F32)
        maskq_t = P.sbuf("maskq", [128, 4], F32)
        maskb_t = P.sbuf("maskb", [128, 4, 128], F32)
        invc_t = P.sbuf("invc", [128, 4, 16], F32)
        SCR_ELEMS = 41456
        scr_t = P.sbuf("scr", [128, SCR_ELEMS], BF16)
        psum_t = [ctx.enter_context(nc.psum_tensor(f"ps{i}", [128, 512], F32)) for i in range(8)]

        XR = [[T(xres_t[:, c, tc * TC:(tc + 1) * TC], f"xr{c}_{tc}") for tc in range(NTC)] for c in range(8)]
        H = [[T(h_t[:, c, tc * TC:(tc + 1) * TC], f"h{c}_{tc}") for tc in range(NTC)] for c in range(8)]
        WBUF = T(wbuf_t[:, :], "wbuf")
        WGLU = T(wglu_t[:], "wglu")
        POOLW = T(poolw_t[:], "poolw")
        GAINS = T(gains_t[:], "gains")
        VEC = T(vec_t[:], "vec")
        ROUTER = T(router_t[:], "router")
        SSMA = T(ssma_t[:], "ssma")
        SSMBC = T(ssmbc_t[:], "ssmbc")
        IDENT = T(ident_t[:], "ident")
        ONESB = T(onesb_t[:], "onesb")
        ONESF = T(onesf_t[:], "onesf")
        IOTA1 = T(iota1_t[:], "iota1")
        IOTAF = T(iotaf_t[:], "iotaf")
        IOTAP = T(iotap_t[:], "iotap")
        MASKQ = T(maskq_t[:], "maskq")
        MASKB = T(maskb_t[:], "maskb")
        INVC = T(invc_t[:], "invc")
        PS = [T(psum_t[i][:], f"ps{i}") for i in range(8)]

        def next_ps():
            p = PS[P.ps_i % 8]
            P.ps_i += 1
            return p

        class Carver:
            def __init__(self):
                self.off = 0

            def take(self, shape, dtype, name):
                n = int(np.prod(shape))
                el = n * (2 if dtype in (F32, I32) else 1)
                if self.off % 2:
                    self.off += 1
                a = scr_t[:, self.off:self.off + el]
                CARVE_INFO[name] = (self.off, tuple(shape), str(dtype))
                self.off += el
                assert self.off <= SCR_ELEMS, (name, self.off)
                if dtype != BF16:
                    a = a.bitcast(dtype)
                if len(shape) == 2:
                    a = a.rearrange("p (a b) -> p a b", a=shape[0])
                elif len(shape) == 3:
                    a = a.rearrange("p (a b c) -> p a b c", a=shape[0], b=shape[1])
                elif len(shape) == 4:
                    a = a.rearrange("p (a b c d) -> p a b c d", a=shape[0], b=shape[1], c=shape[2])
                return T(a, name)

        g_out = P.dma_group("g_dbg")

        op("gpsimd", lambda e: e.iota(iotaf_t[:], [[1, 128]], base=0, channel_multiplier=0, allow_small_or_imprecise_dtypes=True), writes=[IOTAF])
        op("gpsimd", lambda e: e.iota(iotap_t[:], [[1, 1]], base=0, channel_multiplier=1, allow_small_or_imprecise_dtypes=True), writes=[IOTAP])
        op("gpsimd", lambda e: e.iota(iota1_t[:], [[1, 256]], base=1, channel_multiplier=0, allow_small_or_imprecise_dtypes=True), writes=[IOTA1])
        op("vector", lambda e: e.tensor_scalar(out=ident_t[:], in0=iotaf_t[:], scalar1=iotap_t[:, 0:1], scalar2=None, op0=ALU.is_equal), reads=[IOTAF, IOTAP], writes=[IDENT])
        op("vector", lambda e: e.memset(onesb_t[:], 1.0), writes=[ONESB])
        op("vector", lambda e: e.memset(onesf_t[:], 1.0), writes=[ONESF])
        tmpm_t = P.sbuf("tmpm", [128, 4], F32)
        TMPM = T(tmpm_t[:], "tmpm")
        for q4 in range(4):
            op("vector", lambda e, q4=q4: e.tensor_single_scalar(out=maskq_t[:, q4:q4 + 1], in_=iotap_t[:, 0:1], scalar=float(32 * q4), op=ALU.is_ge), reads=[IOTAP], writes=[MASKQ])
            op("vector", lambda e, q4=q4: e.tensor_single_scalar(out=tmpm_t[:, q4:q4 + 1], in_=iotap_t[:, 0:1], scalar=float(32 * q4 + 32), op=ALU.is_lt), reads=[IOTAP], writes=[TMPM])
        op("vector", lambda e: e.tensor_tensor(out=maskq_t[:], in0=maskq_t[:], in1=tmpm_t[:], op=ALU.mult), reads=[MASKQ, TMPM], writes=[MASKQ])
        for q4 in range(4):
            op("vector", lambda e, q4=q4: e.tensor_copy(out=maskb_t[:, :, 32 * q4:32 * q4 + 32], in_=maskq_t[:, q4:q4 + 1].unsqueeze(2).to_broadcast([128, 4, 32])),
               reads=[MASKQ], writes=[MASKB])
        for g in range(4):
            op("vector", lambda e, g=g: e.tensor_single_scalar(out=invc_t[:, g, :], in_=iota1_t[:, 0:16], scalar=float(2 ** (g + 1)), op=ALU.min), reads=[IOTA1], writes=[INVC])
        op("vector", lambda e: e.reciprocal(out=invc_t[:], in_=invc_t[:]), reads=[INVC], writes=[INVC])

        for c in range(8):
            P.dma("sync", lambda e, c=c: e.dma_start(out=xres_t[:, c, :], in_=d_x[:, c, :]), P.grp(f"x{c}"), writes=XR[c])
        P.dma("sync", lambda e: e.dma_start(out=gains_t[:], in_=d_gains), P.grp("gains"), writes=[GAINS])
        P.dma("sync", lambda e: e.dma_start(out=vec_t[:], in_=d_vec), P.grp("vec"), writes=[VEC])
        P.dma("sync", lambda e: e.dma_start(out=router_t[:], in_=d_router), P.grp("router"), writes=[ROUTER])

        def load_wbuf(src):
            P.dma("gpsimd", lambda e: e.dma_start(out=wbuf_t[:, :].rearrange("p (a b) -> p a b", a=8), in_=src), P.grp("wbuf"), writes=[WBUF])

        def load_mixer_small(l):
            P.dma("gpsimd", lambda e: e.dma_start(out=wglu_t[:], in_=d_wglu[l]), P.grp("wglu"), writes=[WGLU])
            P.dma("gpsimd", lambda e: e.dma_start(out=poolw_t[:], in_=d_poolw[l]), P.grp("poolw"), writes=[POOLW])
            P.dma("sync", lambda e: e.dma_start(out=ssma_t[:], in_=d_ssma[l]), P.grp("ssma"), writes=[SSMA])
            P.dma("sync", lambda e: e.dma_start(out=ssmbc_t[:], in_=d_ssmbc[l]), P.grp("ssmbc"), writes=[SSMBC])

        def rmsnorm(gi, sqb, rst, tmpl, sink, rstd_keep=None):
            for tc in range(NTC):
                ps = next_ps()
                for c in range(8):
                    sq = sqb[(tc * 8 + c) % len(sqb)]
                    op("scalar", lambda e, sq=sq, c=c, tc=tc: e.activation(out=sq[:], in_=XR[c][tc][:], func=AF.Square), reads=[XR[c][tc]], writes=[sq])
                    op("tensor", lambda e, sq=sq, ps=ps, c=c: e.matmul(ps[:], lhsT=onesb_t[:], rhs=sq[:], start=(c == 0), stop=(c == 7)),
                       reads=[sq, ONESB], writes=[ps], signal=True)
                r = rst[tc % len(rst)] if rstd_keep is None else rstd_keep[tc]
                op("scalar", lambda e, ps=ps: e.activation(out=tmpl[:], in_=ps[:], func=AF.Ln, scale=1.0 / 1024.0, bias=1e-6), reads=[ps], writes=[tmpl])
                op("scalar", lambda e, r=r: e.activation(out=r[:], in_=tmpl[:], func=AF.Exp, scale=-0.5), reads=[tmpl], writes=[r])
                for c in range(8):
                    sink(c, tc, r)

        def norm_to_h(gi):
            def sink(c, tc, r):
                op("vector", lambda e: e.scalar_tensor_tensor(out=H[c][tc][:], in0=XR[c][tc][:], scalar=gains_t[:, gi, c:c + 1], in1=r[:], op0=ALU.mult, op1=ALU.mult),
                   reads=[XR[c][tc], GAINS, r], writes=[H[c][tc]])
            return sink

        def mixer(l):
            cv = Carver()
            USSM = [[cv.take([8, 64], BF16, f"u{mc}_{tc}") for tc in range(NTC)] for mc in range(4)]
            SBF = [[cv.take([2, 260], BF16, f"sbf{s}_{q4}") for q4 in range(4)] for s in range(2)]
            NTMP = 12
            off_tmps = cv.off
            TMPS = [[cv.take([256], F32, f"st{s}_{i}") for i in range(NTMP)] for s in range(1)]
            off_pb = cv.off
            PBRE = cv.take([8, 4, 32], F32, "pbre")
            PBIM = cv.take([8, 4, 32], F32, "pbim")
            off_wx = cv.off
            WX = cv.take([16, 4, 128], BF16, "wx")
            KD = cv.take([8, 128], BF16, "kd")
            POOLED = [cv.take([512], BF16, f"pooled{i}") for i in range(4)]
            TAIL = [cv.take([16], F32, f"tail{g}") for g in range(4)]
            cva = Carver(); cva.off = off_tmps
            XH = [cva.take([528], F32, f"xh{i}") for i in range(2)]
            SA = cva.take([528], F32, "sa")
            SB_ = cva.take([528], F32, "sb")
            cvb = Carver(); cvb.off = off_wx
            SQB = [cvb.take([512], BF16, f"sqb{i}") for i in range(4)]
            RST = [cvb.take([512], F32, f"rst{i}") for i in range(2)]
            TMPL = cvb.take([512], F32, "tmpl")
            assert cvb.off <= off_wx + 8192
            cvc = Carver(); cvc.off = off_pb
            SGT = [cvc.take([512], F32, f"sgt{i}") for i in range(2)]
            def p16(name):
                return cv.take([16], F32, name)
            DT, AR, ZR, ZI, MAG, Q1, QF, PH, SN, PA, CS, LR, LI, DEN, RDEN, NR, CR, CI, TA, TB, RHO8, F8 = [p16(f"pp{i}") for i in range(22)]
            QI = cv.take([16], I32, "qi")
            PWR = cv.take([9, 16], F32, "pwr")
            PWI = cv.take([9, 16], F32, "pwi")
            BBRE = cv.take([16, 16], F32, "bbre")
            BBIM = cv.take([16, 16], F32, "bbim")
            BT1 = cv.take([16, 16], F32, "bt1")
            CRET = cv.take([16, 32], F32, "cret")
            NCIMT = cv.take([16, 32], F32, "ncimt")
            PBT = cv.take([8, 4, 16], F32, "pbt")
            WCT1 = cv.take([8, 16], F32, "wct1")
            WCT2 = cv.take([8, 16], F32, "wct2")
            DIAGD = cv.take([128], F32, "diagd")
            KTMP = cv.take([128], F32, "ktmp")
            TI = [cv.take([256], I32, f"ti{s}") for s in range(1)]

            V = lambda e: e

            def tt(eng, out, in0, in1, o, reads, writes):
                return op(eng, lambda e: e.tensor_tensor(out=out, in0=in0, in1=in1, op=o), reads=reads, writes=writes)

            load_wbuf(d_win[l])
            load_mixer_small(l)

            rmsnorm(2 * l, SQB, RST, TMPL, norm_to_h(2 * l))

            A_ldt = ssma_t[:, 0, :]
            A_are = ssma_t[:, 1, :]
            A_aim = ssma_t[:, 2, :]
            op("scalar", lambda e: e.activation(out=DT[:], in_=A_ldt, func=AF.Exp), reads=[SSMA], writes=[DT])
            op("vector", lambda e: e.tensor_single_scalar(out=AR[:], in_=A_are, scalar=-1e-4, op=ALU.min), reads=[SSMA], writes=[AR])
            tt("vector", ZR[:], AR[:], DT[:], ALU.mult, [AR, DT], [ZR])
            tt("vector", ZI[:], A_aim, DT[:], ALU.mult, [SSMA, DT], [ZI])
            op("scalar", lambda e: e.activation(out=MAG[:], in_=ZR[:], func=AF.Exp), reads=[ZR], writes=[MAG])

            def sincos(src, scale, SNo, CSo):
                op("vector", lambda e: e.tensor_single_scalar(out=Q1[:], in_=src[:], scalar=scale / TWO_PI, op=ALU.mult), reads=[src], writes=[Q1])
                op("vector", lambda e: e.tensor_copy(out=QI[:], in_=Q1[:]), reads=[Q1], writes=[QI])
                op("vector", lambda e: e.tensor_copy(out=QF[:], in_=QI[:]), reads=[QI], writes=[QF])
                tt("vector", PH[:], Q1[:], QF[:], ALU.subtract, [Q1, QF], [PH])
                if SNo is not None:
                    op("scalar", lambda e: e.activation(out=SNo[:], in_=PH[:], func=AF.Sin, scale=TWO_PI), reads=[PH], writes=[SNo])
                if CSo is not None:
                    op("vector", lambda e: e.scalar_tensor_tensor(out=PA[:], in0=PH[:], scalar=-1.0, in1=PH[:], op0=ALU.mult, op1=ALU.max), reads=[PH], writes=[PA])
                    op("scalar", lambda e: e.activation(out=CSo[:], in_=PA[:], func=AF.Sin, scale=-TWO_PI, bias=HALFPI[:, 0:1]), reads=[PA, HALFPI_T], writes=[CSo])

            sincos(ZI, 1.0, SN, CS)
            tt("vector", LR[:], MAG[:], CS[:], ALU.mult, [MAG, CS], [LR])
            tt("vector", LI[:], MAG[:], SN[:], ALU.mult, [MAG, SN], [LI])
            tt("vector", TA[:], AR[:], AR[:], ALU.mult, [AR], [TA])
            tt("vector", TB[:], A_aim, A_aim, ALU.mult, [SSMA], [TB])
            tt("vector", DEN[:], TA[:], TB[:], ALU.add, [TA, TB], [DEN])
            op("vector", lambda e: e.reciprocal(out=RDEN[:], in_=DEN[:]), reads=[DEN], writes=[RDEN])
            op("vector", lambda e: e.tensor_single_scalar(out=NR[:], in_=LR[:], scalar=-1.0, op=ALU.add), reads=[LR], writes=[NR])
            tt("vector", TA[:], NR[:], AR[:], ALU.mult, [NR, AR], [TA])
            tt("vector", TB[:], LI[:], A_aim, ALU.mult, [LI, SSMA], [TB])
            tt("vector", TA[:], TA[:], TB[:], ALU.add, [TA, TB], [TA])
            tt("vector", CR[:], TA[:], RDEN[:], ALU.mult, [TA, RDEN], [CR])
            tt("vector", TA[:], LI[:], AR[:], ALU.mult, [LI, AR], [TA])
            tt("vector", TB[:], NR[:], A_aim, ALU.mult, [NR, SSMA], [TB])
            tt("vector", TA[:], TA[:], TB[:], ALU.subtract, [TA, TB], [TA])
            tt("vector", CI[:], TA[:], RDEN[:], ALU.mult, [TA, RDEN], [CI])
            b_re = ssmbc_t[:, 0]
            b_im = ssmbc_t[:, 1]
            c_re = ssmbc_t[:, 2]
            c_im = ssmbc_t[:, 3]
            crb = CR[:].unsqueeze(2).to_broadcast([128, 16, 16])
            cib = CI[:].unsqueeze(2).to_broadcast([128, 16, 16])
            tt("vector", BBRE[:], b_re, crb, ALU.mult, [SSMBC, CR], [BBRE])
            tt("vector", BT1[:], b_im, cib, ALU.mult, [SSMBC, CI], [BT1])
            tt("vector", BBRE[:], BBRE[:], BT1[:], ALU.subtract, [BBRE, BT1], [BBRE])
            tt("vector", BBIM[:], b_im, crb, ALU.mult, [SSMBC, CR], [BBIM])
            tt("vector", BT1[:], b_re, cib, ALU.mult, [SSMBC, CI], [BT1])
            tt("vector", BBIM[:], BBIM[:], BT1[:], ALU.add, [BBIM, BT1], [BBIM])
            op("vector", lambda e: e.memset(PWR[:, 0, :], 1.0), writes=[PWR])
            op("vector", lambda e: e.memset(PWI[:, 0, :], 0.0), writes=[PWI])
            for k in range(8):
                tt("vector", TA[:], PWR[:, k, :], LR[:], ALU.mult, [PWR, LR], [TA])
                tt("vector", TB[:], PWI[:, k, :], LI[:], ALU.mult, [PWI, LI], [TB])
                tt("vector", PWR[:, k + 1, :], TA[:], TB[:], ALU.subtract, [TA, TB], [PWR])
                tt("vector", TA[:], PWR[:, k, :], LI[:], ALU.mult, [PWR, LI], [TA])
                tt("vector", TB[:], PWI[:, k, :], LR[:], ALU.mult, [PWI, LR], [TB])
                tt("vector", PWI[:, k + 1, :], TA[:], TB[:], ALU.add, [TA, TB], [PWI])
            op("scalar", lambda e: e.activation(out=RHO8[:], in_=ZR[:], func=AF.Exp, scale=float(J)), reads=[ZR], writes=[RHO8])
            op("vector", lambda e: e.tensor_single_scalar(out=Q1[:], in_=ZI[:], scalar=float(J) / TWO_PI, op=ALU.mult), reads=[ZI], writes=[Q1])
            op("vector", lambda e: e.tensor_copy(out=QI[:], in_=Q1[:]), reads=[Q1], writes=[QI])
            op("vector", lambda e: e.tensor_copy(out=QF[:], in_=QI[:]), reads=[QI], writes=[QF])
            tt("vector", F8[:], Q1[:], QF[:], ALU.subtract, [Q1, QF], [F8])
            op("vector", lambda e: e.memset(CRET[:], 0.0), writes=[CRET])
            op("vector", lambda e: e.memset(NCIMT[:], 0.0), writes=[NCIMT])
            for h2 in range(2):
                ps_ = slice(64 * h2, 64 * h2 + 64)
                cs_ = slice(16 * h2, 16 * h2 + 16)
                op("vector", lambda e, ps_=ps_, cs_=cs_: e.tensor_copy(out=CRET[ps_, :, cs_], in_=c_re[ps_]), reads=[SSMBC], writes=[CRET])
                op("vector", lambda e, ps_=ps_, cs_=cs_: e.tensor_single_scalar(out=NCIMT[ps_, :, cs_], in_=c_im[ps_], scalar=-1.0, op=ALU.mult), reads=[SSMBC], writes=[NCIMT])
            op("vector", lambda e: e.memset(PBRE[:], 0.0), writes=[PBRE])
            op("vector", lambda e: e.memset(PBIM[:], 0.0), writes=[PBIM])
            for s in range(2):
                for q4 in range(4):
                    op("vector", lambda e, s=s, q4=q4: e.memset(SBF[s][q4][:], 0.0), writes=[SBF[s][q4]])

            for tc in range(NTC):
                for mc in [4, 5, 6, 7, 0, 1, 2, 3]:
                    ps = next_ps()
                    for k in range(8):
                        op("tensor", lambda e, ps=ps, k=k, mc=mc, tc=tc: e.matmul(ps[:], lhsT=wbuf_t[:, k * 1024 + mc * 128:k * 1024 + mc * 128 + 128], rhs=H[k][tc][:],
                                                                                   start=(k == 0), stop=(k == 7)),
                           reads=[WBUF, H[k][tc]], writes=[ps], signal=(k == 7))
                    if mc < 4:
                        u = USSM[mc][tc]
                        op("scalar", lambda e, ps=ps, u=u: e.activation(out=u[:], in_=ps[:].rearrange("p (m i) -> p i m", i=8), func=AF.Copy), reads=[ps], writes=[u])
                    else:
                        g = mc - 4
                        w = 2 ** (g + 1)
                        xh = XH[(tc * 4 + g) % 2]
                        op("scalar", lambda e, ps=ps, xh=xh: e.activation(out=xh[:, 16:528], in_=ps[:], func=AF.Copy), reads=[ps], writes=[xh])
                        if tc == 0:
                            op("vector", lambda e, xh=xh: e.memset(xh[:, 0:16], 0.0), writes=[xh])
                        else:
                            op("vector", lambda e, xh=xh, g=g: e.tensor_copy(out=xh[:, 0:16], in_=TAIL[g][:]), reads=[TAIL[g]], writes=[xh])
                        op("vector", lambda e, xh=xh, g=g: e.tensor_copy(out=TAIL[g][:], in_=xh[:, 512:528]), reads=[xh], writes=[TAIL[g]])
                        src = xh
                        bufs = [SA, SB_]
                        sh = 1
                        lo = 1
                        for lev in range(g + 1):
                            dst = bufs[lev % 2]
                            op("vector", lambda e, src=src, dst=dst, sh=sh, lo=lo: e.tensor_tensor(out=dst[:, lo:528], in0=src[:, lo:528], in1=src[:, lo - sh:528 - sh], op=ALU.add),
                               reads=[src], writes=[dst])
                            src = dst
                            sh *= 2
                            lo += sh
                        pl = POOLED[g]
                        op("vector", lambda e, src=src, xh=xh, pl=pl, w=w: e.scalar_tensor_tensor(out=pl[:], in0=src[:, 16:528], scalar=1.0 / w, in1=xh[:, 16:528], op0=ALU.mult, op1=ALU.subtract),
                           reads=[src, xh], writes=[pl])
                        if tc == 0:
                            tmpc = TMPL
                            op("vector", lambda e, src=src, g=g: e.tensor_tensor(out=TMPL[:, 0:16], in0=src[:, 16:32], in1=invc_t[:, g, :], op=ALU.mult), reads=[src, INVC], writes=[TMPL])
                            op("vector", lambda e, xh=xh, pl=pl: e.tensor_tensor(out=pl[:, 0:16], in0=TMPL[:, 0:16], in1=xh[:, 16:32], op=ALU.subtract), reads=[TMPL, xh], writes=[pl])
                for g in range(4):
                    ps = next_ps()
                    op("tensor", lambda e, ps=ps, g=g: e.matmul(ps[:], lhsT=poolw_t[:, g, :], rhs=POOLED[g][:], start=True, stop=True), reads=[POOLW, POOLED[g]], writes=[ps])
                    op("scalar", lambda e, ps=ps, g=g, tc=tc: e.activation(out=H[4 + g][tc][:], in_=ps[:], func=AF.Identity, scale=vec_t[:, l, 1, g:g + 1]), reads=[ps, VEC], writes=[H[4 + g][tc]])

            op("gpsimd", lambda e: e.memset(wbuf_t[:, :], 0.0), writes=[WBUF])
            WC = wbuf_t[:, :].rearrange("p (j q r n) -> p j q r n", j=8, q=4, r=2)

            first_tab_extra = [Tok(P.sem[e_], P.cnt[e_]) for e_ in ("vector", "scalar", "tensor") if P.cnt[e_] > 0]
            for qt in range(4):
                for h2 in range(2):
                    pp = slice(64 * h2, 64 * h2 + 64)
                    cc = slice(16 * h2, 16 * h2 + 16)
                    pr = PWR[pp, 0:8, 4 * qt:4 * qt + 4].unsqueeze(3).to_broadcast([64, 8, 4, 16])
                    pi = PWI[pp, 0:8, 4 * qt:4 * qt + 4].unsqueeze(3).to_broadcast([64, 8, 4, 16])
                    br = BBRE[pp, 4 * qt:4 * qt + 4, :].unsqueeze(1).to_broadcast([64, 8, 4, 16])
                    bi = BBIM[pp, 4 * qt:4 * qt + 4, :].unsqueeze(1).to_broadcast([64, 8, 4, 16])
                    tt("vector", PBRE[pp, :, :, cc], pr, br, ALU.mult, [PWR, BBRE], [PBRE])
                    tt("vector", PBT[pp], pi, bi, ALU.mult, [PWI, BBIM], [PBT])
                    tt("vector", PBRE[pp, :, :, cc], PBRE[pp, :, :, cc], PBT[pp], ALU.subtract, [PBRE, PBT], [PBRE])
                    tt("vector", PBIM[pp, :, :, cc], pr, bi, ALU.mult, [PWR, BBIM], [PBIM])
                    tt("vector", PBT[pp], pi, br, ALU.mult, [PWI, BBRE], [PBT])
                    tt("vector", PBIM[pp, :, :, cc], PBIM[pp, :, :, cc], PBT[pp], ALU.add, [PBIM, PBT], [PBIM])
                for b in range(4):
                    ps = next_ps()
                    for i4 in range(4):
                        idx = 4 * b + i4
                        j = idx // 2
                        reim = idx % 2
                        src = PBRE if reim == 0 else PBIM
                        op("tensor", lambda e, ps=ps, i4=i4, src=src, j=j: e.transpose(ps[:, i4 * 128:(i4 + 1) * 128], src[:, 7 - j, :, :].rearrange("p a b -> p (a b)"), ident_t[:]),
                           reads=[src, IDENT], writes=[ps], signal=(i4 == 3))
                    for q4 in range(4):
                        op("scalar", lambda e, ps=ps, b=b, q4=q4: e.activation(out=WX[:, 4 * b:4 * b + 4, q4, :], in_=ps[:].rearrange("p (a n) -> p a n", a=4), func=AF.Identity,
                                                                                scale=maskq_t[:, q4:q4 + 1]), reads=[ps, MASKQ], writes=[WX],
                           extra=(first_tab_extra if (qt == 0 and b == 0 and q4 == 0) else ()))
                psk = [next_ps(), next_ps()]
                for d in range(8):
                    pk = psk[d // 4]
                    oc = slice((d % 4) * 128, (d % 4) * 128 + 128)
                    op("tensor", lambda e, pk=pk, oc=oc, d=d, qt=qt: e.matmul(pk[:, oc], lhsT=PBRE[:, d, :, :].rearrange("p a b -> p (a b)"),
                                                                       rhs=CRET[:, 4 * qt:4 * qt + 4, :].rearrange("p a b -> p (a b)"), start=True, stop=False),
                       reads=[PBRE, CRET], writes=[pk], signal=False)
                    op("tensor", lambda e, pk=pk, oc=oc, d=d, qt=qt: e.matmul(pk[:, oc], lhsT=PBIM[:, d, :, :].rearrange("p a b -> p (a b)"),
                                                                       rhs=NCIMT[:, 4 * qt:4 * qt + 4, :].rearrange("p a b -> p (a b)"), start=False, stop=True),
                       reads=[PBIM, NCIMT], writes=[pk], signal=(d % 4 == 3))
                op("vector", lambda e, qt=qt: e.tensor_scalar(out=DIAGD[:], in0=ident_t[:], scalar1=vec_t[:, l, 2, qt:qt + 1], scalar2=None, op0=ALU.mult), reads=[IDENT, VEC], writes=[DIAGD])
                tt("vector", KTMP[:], psk[0][:, 0:128], maskb_t[:, 0, :], ALU.mult, [psk[0], MASKB], [KTMP])
                tt("vector", KD[:, 0, :], KTMP[:], DIAGD[:], ALU.add, [KTMP, DIAGD], [KD])
                tt("vector", KD[:, 1:4, :], psk[0][:, 128:512].rearrange("p (a n) -> p a n", a=3), maskb_t[:, 0:3, :], ALU.mult, [psk[0], MASKB], [KD])
                tt("vector", KD[:, 4:8, :], psk[1][:].rearrange("p (a n) -> p a n", a=4), maskb_t[:], ALU.mult, [psk[1], MASKB], [KD])
                for q4 in range(4):
                    q = 4 * qt + q4
                    for h2 in range(2):
                        pp = slice(64 * h2, 64 * h2 + 64)
                        cc = slice(32 * q4 + 16 * h2, 32 * q4 + 16 * h2 + 16)
                        crb_ = c_re[pp, q, :].unsqueeze(1).to_broadcast([64, 8, 16])
                        cib_ = c_im[pp, q, :].unsqueeze(1).to_broadcast([64, 8, 16])
                        prb = PWR[pp, 1:9, q].unsqueeze(2).to_broadcast([64, 8, 16])
                        pib = PWI[pp, 1:9, q].unsqueeze(2).to_broadcast([64, 8, 16])
                        tt("vector", WCT1[pp], crb_, prb, ALU.mult, [SSMBC, PWR], [WCT1])
                        tt("vector", WCT2[pp], cib_, pib, ALU.mult, [SSMBC, PWI], [WCT2])
                        tt("vector", WC[pp, :, q4, 0, cc], WCT1[pp], WCT2[pp], ALU.subtract, [WCT1, WCT2], [WBUF])
                        tt("vector", WCT1[pp], crb_, pib, ALU.mult, [SSMBC, PWI], [WCT1])
                        tt("vector", WCT2[pp], cib_, prb, ALU.mult, [SSMBC, PWR], [WCT2])
                        op("vector", lambda e, pp=pp, q4=q4, cc=cc: e.scalar_tensor_tensor(out=WC[pp, :, q4, 1, cc], in0=WCT1[pp], scalar=-1.0, in1=WCT2[pp], op0=ALU.mult, op1=ALU.subtract),
                           reads=[WCT1, WCT2], writes=[WBUF])
                sset = qt % 2
                for q4 in range(4):
                    q = 4 * qt + q4
                    psx = next_ps()
                    for reim in range(2):
                        for j in range(8):
                            op("tensor", lambda e, psx=psx, reim=reim, j=j, q4=q4, qt=qt: e.matmul(
                                psx[:, reim * 256:(reim + 1) * 256].rearrange("p (a m) -> p a m", a=4),
                                lhsT=WX[:, 2 * j + reim, q4, :],
                                rhs=scr_u[qt][:, :, j, :], start=(j == 0), stop=(j == 7)),
                               reads=[WX] + USSM[qt], writes=[psx], signal=(reim == 1 and j == 7))
                    tm = TMPS[0]
                    PHr, TFp, SN0, SN1, CS0, CS1, T1, T2, VRE, VIM, RRE, RIM = tm
                    SNt = SN0 if q % 2 == 0 else SN1
                    CSt = CS0 if q % 2 == 0 else CS1
                    tix = TI[0]
                    ex_ = first_tab_extra if (qt == 0 and q4 == 0) else ()
                    op("vector", lambda e, q=q: e.tensor_scalar(out=PHr[:], in0=iota1_t[:], scalar1=F8[:, q:q + 1], scalar2=None, op0=ALU.mult), reads=[IOTA1, F8], writes=[PHr], extra=ex_)
                    op("vector", lambda e: e.tensor_copy(out=tix[:], in_=PHr[:]), reads=[PHr], writes=[tix])
                    op("vector", lambda e: e.tensor_copy(out=TFp[:], in_=tix[:]), reads=[tix], writes=[TFp])
                    op("vector", lambda e: e.tensor_tensor(out=PHr[:], in0=PHr[:], in1=TFp[:], op=ALU.subtract), reads=[PHr, TFp], writes=[PHr])
                    op("vector", lambda e: e.scalar_tensor_tensor(out=TFp[:], in0=PHr[:], scalar=-1.0, in1=PHr[:], op0=ALU.mult, op1=ALU.max), reads=[PHr], writes=[TFp])
                    op("scalar", lambda e, SNt=SNt: e.activation(out=SNt[:], in_=PHr[:], func=AF.Sin, scale=TWO_PI), reads=[PHr], writes=[SNt])
                    op("scalar", lambda e, CSt=CSt: e.activation(out=CSt[:], in_=TFp[:], func=AF.Sin, scale=-TWO_PI, bias=HALFPI[:, 0:1]), reads=[TFp, HALFPI_T], writes=[CSt])
                    xre = psx[:, 0:256]
                    xim = psx[:, 256:512]
                    tt("vector", T1[:], xre, CSt[:], ALU.mult, [psx, CSt], [T1])
                    tt("vector", T2[:], xim, SNt[:], ALU.mult, [psx, SNt], [T2])
                    tt("vector", VRE[:], T1[:], T2[:], ALU.add, [T1, T2], [VRE])
                    tt("vector", T1[:], xim, CSt[:], ALU.mult, [psx, CSt], [T1])
                    tt("vector", T2[:], xre, SNt[:], ALU.mult, [psx, SNt], [T2])
                    tt("vector", VIM[:], T1[:], T2[:], ALU.subtract, [T1, T2], [VIM])
                    rho = RHO8[:, q:q + 1].to_broadcast([128, 256])
                    op("vector", lambda e, VRE=VRE, RRE=RRE, rho=rho: e.tensor_tensor_scan(out=RRE[:], data0=rho, data1=VRE[:], initial=0.0, op0=ALU.mult, op1=ALU.add), reads=[VRE, RHO8], writes=[RRE])
                    op("vector", lambda e, VIM=VIM, RIM=RIM, rho=rho: e.tensor_tensor_scan(out=RIM[:], data0=rho, data1=VIM[:], initial=0.0, op0=ALU.mult, op1=ALU.add), reads=[VIM, RHO8], writes=[RIM])
                    sb = SBF[sset][q4]
                    tt("vector", T1[:], RRE[:], CSt[:], ALU.mult, [RRE, CSt], [T1])
                    tt("vector", T2[:], RIM[:], SNt[:], ALU.mult, [RIM, SNt], [T2])
                    tt("vector", sb[:, 0, 1:257], T1[:], T2[:], ALU.subtract, [T1, T2], [sb])
                    tt("vector", T1[:], RRE[:], SNt[:], ALU.mult, [RRE, SNt], [T1])
                    tt("vector", T2[:], RIM[:], CSt[:], ALU.mult, [RIM, CSt], [T2])
                    tt("vector", sb[:, 1, 1:257], T1[:], T2[:], ALU.add, [T1, T2], [sb])
                for tc in range(NTC):
                    psy = next_ps()
                    for j in range(8):
                        oc = slice(j * 64, j * 64 + 64)
                        first = True
                        for q4 in range(4):
                            for reim in range(2):
                                op("tensor", lambda e, psy=psy, oc=oc, j=j, q4=q4, reim=reim, tc=tc, first=first, sset=sset: e.matmul(
                                    psy[:, oc], lhsT=WC[:, j, q4, reim, :], rhs=SBF[sset][q4][:, reim, 64 * tc:64 * tc + 64], start=first, stop=False),
                                   reads=[WBUF, SBF[sset][q4]], writes=[psy], signal=False)
                                first = False
                        for i in range(j + 1):
                            op("tensor", lambda e, psy=psy, oc=oc, j=j, i=i, tc=tc, qt=qt: e.matmul(psy[:, oc], lhsT=KD[:, j - i, :], rhs=USSM[qt][tc][:, i, :], start=False, stop=(i == j)),
                               reads=[KD, USSM[qt][tc]], writes=[psy], signal=(j == 7 and i == j))
                    op("scalar", lambda e, psy=psy, tc=tc, qt=qt: e.activation(out=H[qt][tc][:].rearrange("p (m j) -> p m j", j=8), in_=psy[:].rearrange("p (j m) -> p m j", j=8),
                                                                        func=AF.Gelu_apprx_tanh), reads=[psy], writes=[H[qt][tc]])

            if dbg and l == 0:
                P.barrier()
                P.dma("sync", lambda e: e.dma_start(out=d_dbgw, in_=wbuf_t[:, :]), g_out)
                for c in range(8):
                    P.dma("sync", lambda e, c=c: e.dma_start(out=d_dbgh[1, :, c, :], in_=h_t[:, c, :]), g_out)
                P.barrier()
            for tc in range(NTC):
                pss = [next_ps() for _ in range(4)]
                for mo in range(4):
                    for k in range(4):
                        op("tensor", lambda e, mo=mo, k=k, tc=tc, pss=pss: e.matmul(pss[mo][:], lhsT=wglu_t[:, k, mo * 128:(mo + 1) * 128], rhs=H[k][tc][:], start=(k == 0), stop=(k == 3)),
                           reads=[WGLU, H[k][tc]], writes=[pss[mo]], signal=(k == 3))
                for mo in range(4):
                    sg = SGT[mo % 2]
                    op("scalar", lambda e, mo=mo, sg=sg, pss=pss: e.activation(out=sg[:], in_=pss[mo][:], func=AF.Sigmoid, bias=vec_t[:, l, 0, mo:mo + 1]), reads=[pss[mo], VEC], writes=[sg])
                    tt("vector", H[mo][tc][:], H[mo][tc][:], sg[:], ALU.mult, [H[mo][tc], sg], [H[mo][tc]])

            dumph(l)
            if dbg and l == 0:
                P.barrier()
                P.dma("sync", lambda e: e.dma_start(out=d_dbgs, in_=scr_t[:, :]), g_out)
                P.barrier()
            load_wbuf(d_wout[l])
            for tc in range(NTC):
                for mc in range(8):
                    ps = next_ps()
                    for k in range(8):
                        op("tensor", lambda e, ps=ps, k=k, mc=mc, tc=tc: e.matmul(ps[:], lhsT=wbuf_t[:, k * 1024 + mc * 128:k * 1024 + mc * 128 + 128], rhs=H[k][tc][:],
                                                                                   start=(k == 0), stop=(k == 7)),
                           reads=[WBUF, H[k][tc]], writes=[ps], signal=(k == 7))
                    if dbg and False:
                        pass
                    tt("vector", XR[mc][tc][:], ps[:], XR[mc][tc][:], ALU.add, [ps, XR[mc][tc]], [XR[mc][tc]])

        scr_u = {}

        halfpi_t = P.sbuf("halfpi", [128, 1], F32)
        HALFPI_T = T(halfpi_t[:], "halfpi")
        HALFPI = halfpi_t
        op("vector", lambda e: e.memset(halfpi_t[:], math.pi / 2.0), writes=[HALFPI_T])
        for mc in range(4):
            base = mc * (4 * 8 * 64)
            scr_u[mc] = scr_t[:, base:base + 4 * 8 * 64].rearrange("p (t i m) -> p t i m", t=4, i=8)

        class FFNState:
            pass

        def ffn_setup():
            cv = Carver()
            st = FFNState()
            st.GU = [cv.take([2, 8, 128], BF16, f"gu{s}") for s in range(NS)]
            st.DN = [cv.take([1024], BF16, f"dn{s}") for s in range(NS)]
            st.A = [[cv.take([512], BF16, f"a{p_}_{i}") for i in range(4)] for p_ in range(2)]
            st.SG = [cv.take([512], BF16, f"sg{i}") for i in range(2)]
            off_a = cv.off - 4096 - 1024
            off_g = cv.off
            st.GATEBC = [[cv.take([512], F32, f"gbc{p_}_{tc}") for tc in range(NTC)] for p_ in range(2)]
            cvg = Carver(); cvg.off = off_g
            st.RSTD = [cvg.take([512], F32, f"rstd{tc}") for tc in range(NTC)]
            st.SQB = [cvg.take([512], BF16, f"fsqb{i}") for i in range(4)]
            st.TMPL = cvg.take([512], F32, "ftmpl")
            assert cvg.off <= cv.off
            cva_ = Carver(); cva_.off = off_a
            st.HN32 = cva_.take([L], F32, "hn32")
            st.LG = cv.take([16, 8], F32, "lg")
            st.LG2 = cv.take([16, 8], F32, "lg2")
            st.EQ = cv.take([16, 8], F32, "eq")
            st.G = cv.take([16, 8], F32, "gates")
            st.M1 = cv.take([16], F32, "m1")
            st.M2 = cv.take([16], F32, "m2")
            st.DG = [cv.take([128], F32, f"dg{i}") for i in range(4)]
            st.issued = 0
            st.chunks = []
            return st

        def ffn_issue_loads(st, upto):
            while st.issued < min(upto, len(st.chunks)):
                ci = st.issued
                gu_src, dn_src = st.chunks[ci]
                s = ci % NS
                P.dma("gpsimd", lambda e, s=s, gu_src=gu_src: e.dma_start(out=st.GU[s][:], in_=gu_src), P.grp(f"gu{s}"), writes=[st.GU[s]])
                P.dma("gpsimd", lambda e, s=s, dn_src=dn_src: e.dma_start(out=st.DN[s][:], in_=dn_src), P.grp(f"dn{s}"), writes=[st.DN[s]])
                st.issued += 1

        def ffn_run(st, base_ci, gate_set=None, mid_hook=None):
            f0 = 0
            for gi_, gsz in enumerate(GROUPS):
                if gi_ == 3 and mid_hook is not None:
                    mid_hook()
                ffn_issue_loads(st, base_ci + f0 + NS)
                for tc in range(NTC):
                    par = tc % 2
                    for fl in range(gsz):
                        ci = base_ci + f0 + fl
                        s = ci % NS
                        psg = next_ps()
                        psu = next_ps()
                        for k in range(8):
                            op("tensor", lambda e, psg=psg, s=s, k=k, tc=tc: e.matmul(psg[:], lhsT=st.GU[s][:, 0, k, :], rhs=H[k][tc][:], start=(k == 0), stop=(k == 7)),
                               reads=[st.GU[s], H[k][tc]], writes=[psg], signal=(k == 7))
                        for k in range(8):
                            op("tensor", lambda e, psu=psu, s=s, k=k, tc=tc: e.matmul(psu[:], lhsT=st.GU[s][:, 1, k, :], rhs=H[k][tc][:], start=(k == 0), stop=(k == 7)),
                               reads=[st.GU[s], H[k][tc]], writes=[psu], signal=(k == 7))
                        sg = st.SG[(tc * 4 + fl) % 2]
                        a = st.A[par][fl]
                        op("scalar", lambda e, psg=psg, sg=sg: e.activation(out=sg[:], in_=psg[:], func=AF.Silu), reads=[psg], writes=[sg])
                        if gate_set is not None:
                            gb = st.GATEBC[gate_set][tc]
                            op("gpsimd", lambda e, sg=sg, gb=gb: e.tensor_tensor(out=sg[:], in0=sg[:], in1=gb[:], op=ALU.mult), reads=[sg, gb], writes=[sg])
                        op("vector", lambda e, psu=psu, sg=sg, a=a: e.tensor_tensor(out=a[:], in0=psu[:], in1=sg[:], op=ALU.mult), reads=[psu, sg], writes=[a])
                    for mo in range(8):
                        ps = next_ps()
                        for fl in range(gsz):
                            ci = base_ci + f0 + fl
                            s = ci % NS
                            op("tensor", lambda e, ps=ps, s=s, mo=mo, fl=fl, par=par, gsz=gsz: e.matmul(ps[:], lhsT=st.DN[s][:, mo * 128:(mo + 1) * 128], rhs=st.A[par][fl][:],
                                                                                                   start=(fl == 0), stop=(fl == gsz - 1)),
                               reads=[st.DN[s], st.A[par][fl]], writes=[ps], signal=(fl == gsz - 1))
                        xr = XR[mo][tc]
                        op("vector", lambda e, ps=ps, xr=xr: e.tensor_tensor(out=xr[:], in0=ps[:], in1=xr[:], op=ALU.add), reads=[ps, xr], writes=[xr])
                f0 += gsz

        def dump(idx):
            if not dbg:
                return
            for c in range(8):
                P.dma("sync", lambda e, c=c: e.dma_start(out=d_dbg[idx, :, c, :], in_=xres_t[:, c, :]), g_out, reads=XR[c])

        def dumph(idx):
            if not dbg:
                return
            for c in range(8):
                P.dma("sync", lambda e, c=c: e.dma_start(out=d_dbgh[idx, :, c, :], in_=h_t[:, c, :]), g_out, reads=H[c])

        if "mix0" in stages:
            mixer(0)
            dump(0)
        P.barrier()
        if "ffn0" in stages:
            st = ffn_setup()
            st.chunks = [(d_fgu[f], d_fdn[f]) for f in range(NF)]
            ffn_issue_loads(st, NS)
            rmsnorm(1, st.SQB, None, st.TMPL, norm_to_h(1), rstd_keep=st.RSTD)
            ffn_run(st, 0, None)
            dump(1)
        P.barrier()
        if "mix1" in stages:
            mixer(1)
            dump(2)
        P.barrier()
        if "ffn1" in stages:
            NSM = 4
            GROUPS_S = [2] * 11
            cv = Carver()
            st = FFNState()
            st.GU = [cv.take([2, 8, 128], BF16, f"mgu{s_}") for s_ in range(NSM)]
            st.DN = [cv.take([1024], BF16, f"mdn{s_}") for s_ in range(NSM)]
            off_a = cv.off
            SW = 640
            NOB = False
            CAP0 = 512 if NOB else SW
            st.A = [[cv.take([SW], BF16, f"ma{p_}_{i}") for i in range(2)] for p_ in range(2)]
            st.SG = [T(wglu_t[:, :, :].rearrange("p a b -> p (a b)")[:, i * SW:(i + 1) * SW], f"msg{i}") for i in range(2)]
            off_hc = cv.off
            HC = cv.take([8, SW], BF16, "hc")
            YSM = cv.take([5, 1024], BF16, "ysm")
            off_sel = cv.off
            SEL = [cv.take([SW], BF16, f"sel{i}") for i in range(4)]
            off_selt = cv.off
            SELT = [cv.take([512], BF16, f"selt{i}") for i in range(5)]
            cvs = Carver(); cvs.off = off_sel
            st.STG = [cvs.take([512], BF16, f"stg{i}") for i in range(2)]
            POSBC = [cv.take([512], F32, f"posbc{tc}") for tc in range(NTC)]
            st.GATEBC = [[cv.take([512], F32, f"mgbc{tc}") for tc in range(NTC)]]
            IOTA512 = cv.take([SW], F32, "iota512")
            st.LG = cv.take([16, 8], F32, "lg")
            st.LG2 = cv.take([16, 8], F32, "lg2")
            st.EQ = cv.take([16, 8], F32, "eq")
            st.G = cv.take([16, 8], F32, "gates")
            POSM = T(st.LG2.ap, "posm")
            TOT = T(st.LG.ap, "tot")
            OFFS = cv.take([16, 8], F32, "offs")
            st.M1 = cv.take([16], F32, "m1")
            st.M2 = cv.take([16], F32, "m2")
            NE = cv.take([8], F32, "ne")
            FL = cv.take([8, 4], F32, "fl")
            FLI = cv.take([8, 4], I32, "fli")
            FLANY = cv.take([2], I32, "flany")
            cvd = Carver(); cvd.off = off_selt
            st.DG = [cvd.take([128], F32, f"dg{i}") for i in range(2)]
            UT = T(poolw_t[:, :, :].rearrange("p a b -> p (a b)")[:, 0:256].bitcast(F32), "ut")
            IDENTB = cv.take([128], BF16, "identb")
            cvt = Carver(); cvt.off = off_a
            TMPS_ = cvt.take([512], F32, "tmps")
            cvn = Carver(); cvn.off = off_hc
            st.RSTD = [cvn.take([512], F32, f"mrstd{tc}") for tc in range(NTC)]
            st.SQB = [cvn.take([512], BF16, f"msqb{i}") for i in range(4)]
            st.TMPL = cvn.take([512], F32, "mtmpl")
            assert cvn.off <= off_hc + 8 * SW + 5120
            cvh = Carver(); cvh.off = 0
            st.HN32 = cvh.take([L], F32, "mhn32")
            YACC = [T(wbuf_t[:, :].bitcast(F32).rearrange("p (m n) -> p m n", m=8)[:, m, :], f"yacc{m}") for m in range(8)]
            YACC2 = T(ssmbc_t[:, :, :, :].rearrange("p a b c -> p (a b c)").rearrange("p (m n) -> p m n", m=8), "yacc2")
            hT_view = h_t[:, :, :].rearrange("p a b -> p (a b)").rearrange("p (k c) -> p k c", k=16)
            HTK = [T(hT_view[:, k, :], f"ht{k}") for k in range(16)]

            op("gpsimd", lambda e: e.iota(IOTA512[:], [[1, SW]], base=0, channel_multiplier=0, allow_small_or_imprecise_dtypes=True), writes=[IOTA512])
            op("vector", lambda e: e.tensor_scalar(out=UT[:], in0=iotaf_t[:], scalar1=iotap_t[:, 0:1], scalar2=None, op0=ALU.is_gt), reads=[IOTAF, IOTAP], writes=[UT])
            op("vector", lambda e: e.tensor_copy(out=IDENTB[:], in_=ident_t[:]), reads=[IDENT], writes=[IDENTB])

            cnt_ = [0]

            def sink_t(c, tc, r):
                stg = st.STG[cnt_[0] % 2]
                cnt_[0] += 1
                op("vector", lambda e: e.scalar_tensor_tensor(out=stg[:], in0=XR[c][tc][:], scalar=gains_t[:, 3, c:c + 1], in1=r[:], op0=ALU.mult, op1=ALU.mult),
                   reads=[XR[c][tc], GAINS, r], writes=[stg])
                ps = next_ps()
                psb = ps[:].bitcast(BF16)
                for j in range(4):
                    op("tensor", lambda e, j=j: e.transpose(psb[:, j * 128:(j + 1) * 128], stg[:, j * 128:(j + 1) * 128], IDENTB[:]), reads=[stg, IDENTB], writes=[ps], signal=(j == 3))
                op("scalar", lambda e: e.activation(out=hT_view[:, 4 * tc:4 * tc + 4, c * 128:(c + 1) * 128], in_=psb[:, 0:512].rearrange("p (a n) -> p a n", a=4), func=AF.Copy),
                   reads=[ps], writes=[HTK[4 * tc + j] for j in range(4)])

            rmsnorm(3, st.SQB, None, st.TMPL, sink_t, rstd_keep=st.RSTD)

            psl = next_ps()
            hn_v = st.HN32[:].rearrange("p (b c n) -> p b c n", b=2, c=8)
            g3b = gains_t[:, 3, :].unsqueeze(2).to_broadcast([128, 8, 128])
            for tl in range(16):
                tc_, o_ = tl // 4, (tl % 4) * 128
                hv = hn_v[:, tl % 2]
                op("vector", lambda e, hv=hv, tl=tl: e.tensor_tensor(out=hv, in0=xres_t[:, :, tl * 128:(tl + 1) * 128], in1=g3b, op=ALU.mult),
                   reads=[XR[c][tc_] for c in range(8)] + [GAINS], writes=[st.HN32])
                op("vector", lambda e, hv=hv, tc_=tc_, o_=o_: e.tensor_tensor(out=hv, in0=hv, in1=st.RSTD[tc_][:, o_:o_ + 128].unsqueeze(1).to_broadcast([128, 8, 128]), op=ALU.mult),
                   reads=[st.RSTD[tc_], st.HN32], writes=[st.HN32])
                for c in range(8):
                    op("tensor", lambda e, hv=hv, c=c, tl=tl: e.matmul(psl[:, tl * 8:(tl + 1) * 8], lhsT=hv[:, c, :], rhs=router_t[:, c, :], start=(c == 0), stop=(c == 7)),
                       reads=[st.HN32, ROUTER], writes=[psl], signal=(c == 7))
            lg3 = psl[:, 0:128].rearrange("p (t e) -> p t e", e=8)
            op("vector", lambda e: e.tensor_copy(out=st.LG[:], in_=lg3), reads=[psl], writes=[st.LG])
            op("vector", lambda e: e.tensor_reduce(out=st.M1[:], in_=st.LG[:], axis=AX.X, op=ALU.max), reads=[st.LG], writes=[st.M1])
            m1b = st.M1[:].unsqueeze(2).to_broadcast([128, 16, 8])
            op("vector", lambda e: e.tensor_tensor(out=st.EQ[:], in0=st.LG[:], in1=m1b, op=ALU.is_equal), reads=[st.LG, st.M1], writes=[st.EQ])
            op("vector", lambda e: e.scalar_tensor_tensor(out=st.LG2[:], in0=st.EQ[:], scalar=-1e30, in1=st.LG[:], op0=ALU.mult, op1=ALU.add), reads=[st.EQ, st.LG], writes=[st.LG2])
            op("vector", lambda e: e.tensor_reduce(out=st.M2[:], in_=st.LG2[:], axis=AX.X, op=ALU.max), reads=[st.LG2], writes=[st.M2])
            m2b = st.M2[:].unsqueeze(2).to_broadcast([128, 16, 8])
            op("vector", lambda e: e.tensor_tensor(out=st.EQ[:], in0=st.LG[:], in1=m2b, op=ALU.is_ge), reads=[st.LG, st.M2], writes=[st.EQ])
            op("vector", lambda e: e.tensor_tensor(out=st.LG2[:], in0=st.LG[:], in1=m1b, op=ALU.subtract), reads=[st.LG, st.M1], writes=[st.LG2])
            op("scalar", lambda e: e.activation(out=st.LG2[:], in_=st.LG2[:], func=AF.Exp), reads=[st.LG2], writes=[st.LG2])
            op("vector", lambda e: e.tensor_tensor(out=st.LG2[:], in0=st.LG2[:], in1=st.EQ[:], op=ALU.mult), reads=[st.LG2, st.EQ], writes=[st.LG2])
            op("vector", lambda e: e.tensor_reduce(out=st.M1[:], in_=st.LG2[:], axis=AX.X, op=ALU.add), reads=[st.LG2], writes=[st.M1])
            op("vector", lambda e: e.reciprocal(out=st.M1[:], in_=st.M1[:]), reads=[st.M1], writes=[st.M1])
            op("vector", lambda e: e.tensor_tensor(out=st.G[:], in0=st.LG2[:], in1=m1b, op=ALU.mult), reads=[st.LG2, st.M1], writes=[st.G])

            psw = next_ps()
            eq2 = st.EQ[:].rearrange("p t e -> p (t e)")
            op("tensor", lambda e: e.matmul(psw[:, 0:128], lhsT=UT[:], rhs=eq2, start=True, stop=True), reads=[UT, st.EQ], writes=[psw])
            op("tensor", lambda e: e.matmul(psw[:, 128:256], lhsT=onesf_t[:], rhs=eq2, start=True, stop=True), reads=[ONESF, st.EQ], writes=[psw])
            op("vector", lambda e: e.tensor_copy(out=TOT[:], in_=psw[:, 128:256].rearrange("p (t e) -> p t e", e=8)), reads=[psw], writes=[TOT])
            op("vector", lambda e: e.memset(OFFS[:, 0, :], 0.0), writes=[OFFS])
            for k in range(15):
                op("vector", lambda e, k=k: e.tensor_tensor(out=OFFS[:, k + 1, :], in0=OFFS[:, k, :], in1=TOT[:, k, :], op=ALU.add), reads=[OFFS, TOT], writes=[OFFS])
            op("vector", lambda e: e.tensor_tensor(out=NE[:], in0=OFFS[:, 15, :], in1=TOT[:, 15, :], op=ALU.add), reads=[OFFS, TOT], writes=[NE])
            op("vector", lambda e: e.tensor_tensor(out=POSM[:], in0=psw[:, 0:128].rearrange("p (t e) -> p t e", e=8), in1=OFFS[:], op=ALU.add), reads=[psw, OFFS], writes=[POSM])
            op("vector", lambda e: e.tensor_single_scalar(out=POSM[:], in_=POSM[:], scalar=1.0e6, op=ALU.add), reads=[POSM], writes=[POSM])
            op("vector", lambda e: e.tensor_tensor(out=POSM[:], in0=POSM[:], in1=st.EQ[:], op=ALU.mult), reads=[POSM, st.EQ], writes=[POSM])
            op("vector", lambda e: e.tensor_single_scalar(out=POSM[:], in_=POSM[:], scalar=-1.0e6, op=ALU.add), reads=[POSM], writes=[POSM])
            for cch in range(4):
                op("vector", lambda e, cch=cch: e.tensor_single_scalar(out=FL[:, :, cch], in_=NE[:], scalar=float(0 if cch == 0 else CAP0 + 512 * (cch - 1)) + 0.5, op=ALU.is_gt), reads=[NE], writes=[FL])
            op("vector", lambda e: e.tensor_reduce(out=st.M2[:, 0:1], in_=NE[:], axis=AX.X, op=ALU.max), reads=[NE], writes=[st.M2])
            op("vector", lambda e: e.tensor_single_scalar(out=st.M2[:, 1:2], in_=st.M2[:, 0:1], scalar=float(CAP0) + 0.5, op=ALU.is_gt), reads=[st.M2], writes=[st.M2])
            op("vector", lambda e: e.tensor_copy(out=FLANY[:, 0:1], in_=st.M2[:, 1:2]), reads=[st.M2], writes=[FLANY])
            op("vector", lambda e: e.tensor_copy(out=FLI[:], in_=FL[:]), reads=[FL], writes=[FLI])

            def build_bc(src, dst, ex):
                for tc in range(NTC):
                    ps = next_ps()
                    for t4 in range(4):
                        tl = tc * 4 + t4
                        dg = st.DG[t4 % 2]
                        op("vector", lambda e, dg=dg, tl=tl: e.tensor_scalar(out=dg[:], in0=ident_t[:], scalar1=src[:, tl, ex:ex + 1], scalar2=None, op0=ALU.mult), reads=[IDENT, src], writes=[dg])
                        op("tensor", lambda e, ps=ps, dg=dg, t4=t4: e.matmul(ps[:, t4 * 128:(t4 + 1) * 128], lhsT=onesf_t[:], rhs=dg[:], start=True, stop=True),
                           reads=[ONESF, dg], writes=[ps], signal=True)
                    gb = dst[tc]
                    op("scalar", lambda e, ps=ps, gb=gb: e.activation(out=gb[:], in_=ps[:], func=AF.Copy), reads=[ps], writes=[gb])

            gci = [0]

            pre_state = {}

            def prefetch_first(ex):
                slot_of = [(gci[0] + f) % NSM for f in range(NF)]
                pre_state[ex] = (gci[0], slot_of)
                gci[0] += NF
                ex_dep = [FLI.last_w]
                for f in range(NSM):
                    s_ = slot_of[f]
                    gu_src, dn_src = d_mgu[ex * NF + f], d_mdn[ex * NF + f]
                    P.dma("gpsimd", lambda e, s_=s_, gu_src=gu_src: e.dma_start(out=st.GU[s_][:], in_=gu_src), P.grp(f"mgu{s_}"), writes=[st.GU[s_]], extra=ex_dep)
                    P.dma("gpsimd", lambda e, s_=s_, dn_src=dn_src: e.dma_start(out=st.DN[s_][:], in_=dn_src), P.grp(f"mdn{s_}"), writes=[st.DN[s_]], extra=ex_dep)

            def chunk_pass(ex, cch, flag_ap, mid_hook=None):
                base = 0 if cch == 0 else CAP0 + 512 * (cch - 1)
                has_b = (cch == 0) and not NOB
                nsl = SW if has_b else 512
                nst = 5 if has_b else 4
                loads = [(d_mgu[ex * NF + f], d_mdn[ex * NF + f]) for f in range(NF)]
                if cch == 0 and ex in pre_state:
                    slot_of = pre_state[ex][1]
                    issued = [NSM]
                else:
                    slot_of = [(gci[0] + f) % NSM for f in range(NF)]
                    gci[0] += NF
                    issued = [0]
                P.cond_begin(flag_ap, FLI)

                def issue(upto):
                    while issued[0] < min(upto, NF):
                        f = issued[0]
                        s_ = slot_of[f]
                        gu_src, dn_src = loads[f]
                        P.dma("gpsimd", lambda e, s_=s_, gu_src=gu_src: e.dma_start(out=st.GU[s_][:], in_=gu_src), P.grp(f"mgu{s_}"), writes=[st.GU[s_]])
                        P.dma("gpsimd", lambda e, s_=s_, dn_src=dn_src: e.dma_start(out=st.DN[s_][:], in_=dn_src), P.grp(f"mdn{s_}"), writes=[st.DN[s_]])
                        issued[0] += 1

                issue(NSM)
                for half in range(2):
                    pss = [next_ps() for _ in range(4)]
                    psbl = [next_ps() for _ in range(4)] if has_b else None
                    for k in range(16):
                        sel = SEL[k % 4]
                        op("vector", lambda e, sel=sel, k=k: e.tensor_scalar(out=sel[:, 0:nsl], in0=IOTA512[:, 0:nsl], scalar1=float(base), scalar2=POSM[:, k, ex:ex + 1], op0=ALU.add, op1=ALU.is_equal),
                           reads=[IOTA512, POSM], writes=[sel])
                        for mi in range(4):
                            m = half * 4 + mi
                            op("tensor", lambda e, sel=sel, k=k, mi=mi, m=m, pss=pss: e.matmul(pss[mi][:], lhsT=HTK[k][:, m * 128:(m + 1) * 128], rhs=sel[:, 0:512], start=(k == 0), stop=(k == 15)),
                               reads=[HTK[k], sel], writes=[pss[mi]], signal=(mi == 3 and not has_b))
                        if has_b:
                            for mi in range(4):
                                m = half * 4 + mi
                                op("tensor", lambda e, sel=sel, k=k, mi=mi, m=m, psbl=psbl: e.matmul(psbl[mi][:, 0:128], lhsT=HTK[k][:, m * 128:(m + 1) * 128], rhs=sel[:, 512:SW],
                                                                                                      start=(k == 0), stop=(k == 15)),
                                   reads=[HTK[k], sel], writes=[psbl[mi]], signal=(mi == 3))
                    for mi in range(4):
                        m = half * 4 + mi
                        op("scalar", lambda e, mi=mi, m=m, pss=pss: e.activation(out=HC[:, m, 0:512], in_=pss[mi][:], func=AF.Copy), reads=[pss[mi]], writes=[HC])
                    if has_b:
                        for mi in range(4):
                            m = half * 4 + mi
                            op("scalar", lambda e, mi=mi, m=m, psbl=psbl: e.activation(out=HC[:, m, 512:SW], in_=psbl[mi][:, 0:128], func=AF.Copy), reads=[psbl[mi]], writes=[HC])
                f0 = 0
                for gi_, gsz in enumerate(GROUPS_S):
                    issue(f0 + NSM)
                    par = gi_ % 2
                    for fl in range(gsz):
                        s_ = slot_of[f0 + fl]
                        psg = next_ps()
                        psu = next_ps()
                        psb = next_ps() if has_b else None
                        for k in range(8):
                            op("tensor", lambda e, psg=psg, s_=s_, k=k: e.matmul(psg[:], lhsT=st.GU[s_][:, 0, k, :], rhs=HC[:, k, 0:512], start=(k == 0), stop=(k == 7)),
                               reads=[st.GU[s_], HC], writes=[psg], signal=(k == 7))
                        for k in range(8):
                            op("tensor", lambda e, psu=psu, s_=s_, k=k: e.matmul(psu[:], lhsT=st.GU[s_][:, 1, k, :], rhs=HC[:, k, 0:512], start=(k == 0), stop=(k == 7)),
                               reads=[st.GU[s_], HC], writes=[psu], signal=(k == 7))
                        if has_b:
                            for r_ in range(2):
                                for k in range(8):
                                    op("tensor", lambda e, psb=psb, s_=s_, k=k, r_=r_: e.matmul(psb[:, r_ * 128:(r_ + 1) * 128], lhsT=st.GU[s_][:, r_, k, :], rhs=HC[:, k, 512:SW], start=(k == 0), stop=(k == 7)),
                                       reads=[st.GU[s_], HC], writes=[psb], signal=(k == 7))
                        sg = st.SG[fl % 2]
                        a = st.A[par][fl]
                        op("scalar", lambda e, psg=psg, sg=sg: e.activation(out=sg[:, 0:512], in_=psg[:], func=AF.Silu), reads=[psg], writes=[sg])
                        if has_b:
                            op("scalar", lambda e, psb=psb, sg=sg: e.activation(out=sg[:, 512:SW], in_=psb[:, 0:128], func=AF.Silu), reads=[psb], writes=[sg])
                        op("vector", lambda e, psu=psu, sg=sg, a=a: e.tensor_tensor(out=a[:, 0:512], in0=psu[:], in1=sg[:, 0:512], op=ALU.mult), reads=[psu, sg], writes=[a])
                        if has_b:
                            op("vector", lambda e, psb=psb, sg=sg, a=a: e.tensor_tensor(out=a[:, 512:SW], in0=psb[:, 128:256], in1=sg[:, 512:SW], op=ALU.mult), reads=[psb, sg], writes=[a])
                    psd = [next_ps(), next_ps()] if has_b else None
                    for m in range(8):
                        ps = next_ps()
                        while psd is not None and (ps is psd[0] or ps is psd[1]):
                            ps = next_ps()
                        for fl in range(gsz):
                            s_ = slot_of[f0 + fl]
                            op("tensor", lambda e, ps=ps, s_=s_, m=m, fl=fl, par=par, gsz=gsz: e.matmul(ps[:], lhsT=st.DN[s_][:, m * 128:(m + 1) * 128], rhs=st.A[par][fl][:, 0:512],
                                                                                                     start=(fl == 0), stop=(fl == gsz - 1)),
                               reads=[st.DN[s_], st.A[par][fl]], writes=[ps], signal=(fl == gsz - 1))
                        if has_b:
                            for fl in range(gsz):
                                s_ = slot_of[f0 + fl]
                                op("tensor", lambda e, psd=psd, s_=s_, m=m, fl=fl, par=par, gsz=gsz: e.matmul(psd[m // 4][:, (m % 4) * 128:(m % 4 + 1) * 128], lhsT=st.DN[s_][:, m * 128:(m + 1) * 128],
                                                                                                           rhs=st.A[par][fl][:, 512:SW], start=(fl == 0), stop=(fl == gsz - 1)),
                                   reads=[st.DN[s_], st.A[par][fl]], writes=[psd[m // 4]], signal=(fl == gsz - 1))
                        ya = YACC[m]
                        if gi_ == 0:
                            op("scalar", lambda e, ps=ps, ya=ya: e.activation(out=ya[:], in_=ps[:], func=AF.Copy), reads=[ps], writes=[ya])
                        else:
                            op("vector", lambda e, ps=ps, ya=ya: e.tensor_tensor(out=ya[:], in0=ps[:], in1=ya[:], op=ALU.add), reads=[ps, ya], writes=[ya])
                    if has_b:
                        for hf in range(2):
                            y2 = YACC2[:, hf * 4:hf * 4 + 4, :]
                            pv = psd[hf][:].rearrange("p (a n) -> p a n", a=4)
                            if gi_ == 0:
                                op("scalar", lambda e, pv=pv, y2=y2: e.activation(out=y2, in_=pv, func=AF.Copy), reads=[psd[hf]], writes=[YACC2])
                            else:
                                op("vector", lambda e, pv=pv, y2=y2: e.tensor_tensor(out=y2, in0=pv, in1=y2, op=ALU.add), reads=[psd[hf], YACC2], writes=[YACC2])
                    f0 += gsz
                P.cond_end()
                if mid_hook is not None:
                    mid_hook()
                P.cond_begin(flag_ap, FLI)
                for st4 in range(nst):
                    for hf in range(2):
                        ps = next_ps()
                        for mi in range(4):
                            m = hf * 4 + mi
                            if st4 < 4:
                                op("tensor", lambda e, ps=ps, mi=mi, m=m, st4=st4: e.transpose(ps[:, mi * 128:(mi + 1) * 128], YACC[m][:, st4 * 128:(st4 + 1) * 128], ident_t[:]),
                                   reads=[YACC[m], IDENT], writes=[ps], signal=(mi == 3))
                            else:
                                op("tensor", lambda e, ps=ps, mi=mi, m=m: e.transpose(ps[:, mi * 128:(mi + 1) * 128], YACC2[:, m, :], ident_t[:]),
                                   reads=[YACC2, IDENT], writes=[ps], signal=(mi == 3))
                        op("scalar", lambda e, ps=ps, st4=st4, hf=hf: e.activation(out=YSM[:, st4, hf * 512:(hf + 1) * 512], in_=ps[:], func=AF.Copy), reads=[ps], writes=[YSM])
                for tcx in range(NTC):
                    for st4 in range(nst):
                        op("vector", lambda e, st4=st4, tcx=tcx: e.tensor_scalar(out=SELT[st4][:], in0=POSBC[tcx][:], scalar1=float(base + 128 * st4), scalar2=iotap_t[:, 0:1],
                                                                                  op0=ALU.subtract, op1=ALU.is_equal), reads=[POSBC[tcx], IOTAP], writes=[SELT[st4]])
                    for m in range(8):
                        ps = next_ps()
                        for st4 in range(nst):
                            op("tensor", lambda e, ps=ps, st4=st4, m=m: e.matmul(ps[:], lhsT=YSM[:, st4, m * 128:(m + 1) * 128], rhs=SELT[st4][:], start=(st4 == 0), stop=(st4 == nst - 1)),
                               reads=[YSM, SELT[st4]], writes=[ps], signal=(st4 == nst - 1))
                        gb = st.GATEBC[0][tcx]
                        xr = XR[m][tcx]
                        op("vector", lambda e, ps=ps, gb=gb: e.tensor_tensor(out=TMPS_[:], in0=ps[:], in1=gb[:], op=ALU.mult), reads=[ps, gb], writes=[TMPS_])
                        op("vector", lambda e, xr=xr: e.tensor_tensor(out=xr[:], in0=TMPS_[:], in1=xr[:], op=ALU.add), reads=[TMPS_, xr], writes=[xr])
                P.cond_end()

            prefetch_first(0)
            for ex in range(8):
                build_bc(st.G, st.GATEBC[0], ex)
                build_bc(POSM, POSBC, ex)
                chunk_pass(ex, 0, FLI[0:1, ex, 0:1], mid_hook=(lambda ex=ex: prefetch_first(ex + 1)) if ex < 7 else None)
            P.cond_begin(FLANY[0:1, 0:1], FLANY)
            for ex in range(8):
                P.cond_begin(FLI[0:1, ex, 1:2], FLI)
                build_bc(st.G, st.GATEBC[0], ex)
                build_bc(POSM, POSBC, ex)
                for cch in range(1, 4):
                    chunk_pass(ex, cch, FLI[0:1, ex, cch:cch + 1])
                P.cond_end()
            P.cond_end()
            dump(3)
        if "final" in stages:
            cv = Carver()
            cv.off = SCR_ELEMS - 16 * 1024
            P.barrier()
            OST = [cv.take([512], F32, f"ost{i}") for i in range(4)]
            SQB = [cv.take([512], BF16, f"osq{i}") for i in range(4)]
            RST = [cv.take([512], F32, f"orst{i}") for i in range(2)]
            TMPL2 = cv.take([512], F32, "otmpl")
            cnt = [0]
            last = [None]

            def sink(c, tc, r):
                oi = cnt[0] % 4
                o = OST[oi]
                cnt[0] += 1
                op("vector", lambda e: e.scalar_tensor_tensor(out=o[:], in0=XR[c][tc][:], scalar=gains_t[:, 4, c:c + 1], in1=r[:], op0=ALU.mult, op1=ALU.mult),
                   reads=[XR[c][tc], GAINS, r], writes=[o])
                last[0] = P.dma("sync", lambda e: e.dma_start(out=d_out[:, c, tc * TC:(tc + 1) * TC], in_=o[:]), P.grp(f"ost{oi}"), reads=[o])

            rmsnorm(4, SQB, RST, TMPL2, sink)
        else:
            last = [None]
            for c in range(8):
                last[0] = P.dma("sync", lambda e, c=c: e.dma_start(out=d_out[:, c, :], in_=xres_t[:, c, :]), P.grp(f"ost{c % 4}"), reads=XR[c])
        for g_ in P.groups:
            if g_.count > 0:
                P.wait_tok("sync", Tok(g_.sem, 16 * g_.count))
        P.barrier()
        P.finish()
    return nc


def _prep_shared(inp):
    f = np.float32

    def cmaj(v):
        return np.ascontiguousarray(np.asarray(v, f).reshape(8, 128).T)

    def c4(v):
        return np.ascontiguousarray(np.asarray(v, f).reshape(4, 128).T)

    gains = np.stack([cmaj(inp["norm_mix_g"][0]), cmaj(inp["norm_ffn_g"][0]), cmaj(inp["norm_mix_g"][1]), cmaj(inp["norm_ffn_g"][1]),
                      cmaj(inp["final_norm_g"])], axis=1)
    w_in = np.ascontiguousarray(np.asarray(inp["w_in"], f).reshape(2, 8, 128, 1024).transpose(0, 2, 1, 3))
    w_out = np.ascontiguousarray(np.asarray(inp["w_out"], f).reshape(2, 8, 128, 1024).transpose(0, 2, 1, 3))
    w_glu = np.ascontiguousarray(np.asarray(inp["ssm_w_glu"], f).reshape(2, 4, 128, 512).transpose(0, 2, 1, 3))
    vec = np.stack([np.stack([c4(inp["ssm_b_glu"][l]), c4(inp["pool_scale"][l]), c4(inp["ssm_d"][l])], axis=1) for l in range(2)], axis=1)
    pool_w = np.ascontiguousarray(np.asarray(inp["pool_w"], f).transpose(0, 2, 1, 3))

    def hn(v):
        return np.asarray(v, f).reshape(16, 2, 64).transpose(1, 2, 0).reshape(128, 16)

    ssm_a = np.zeros((2, 128, 3, 16), f)
    ssm_bc = np.zeros((2, 128, 4, 16, 16), f)
    for l in range(2):
        ssm_a[l, :, 0] = hn(np.repeat(np.asarray(inp["ssm_log_dt"][l], f)[:, None], 64, axis=1))
        ssm_a[l, :, 1] = hn(inp["ssm_a_re"][l])
        ssm_a[l, :, 2] = hn(inp["ssm_a_im"][l])
        for i, key in enumerate(["ssm_b_re", "ssm_b_im"]):
            b = np.asarray(inp[key][l], f).reshape(16, 2, 64, 16)
            ssm_bc[l, :, i] = b.transpose(1, 2, 0, 3).reshape(128, 16, 16)
        for i, key in enumerate(["ssm_c_re", "ssm_c_im"]):
            c = np.asarray(inp[key][l], f).reshape(16, 2, 16, 64)
            ssm_bc[l, :, 2 + i] = c.transpose(1, 3, 0, 2).reshape(128, 16, 16)

    def gu(wg, wu):
        a = np.stack([np.asarray(wg, f), np.asarray(wu, f)], axis=0)
        a = a.reshape(2, 8, 128, NF, 128)
        return np.ascontiguousarray(a.transpose(3, 2, 0, 1, 4))

    ffn_gu = gu(inp["ffn_w_gate"][0], inp["ffn_w_up"][0])
    ffn_dn = np.ascontiguousarray(np.asarray(inp["ffn_w_down"][0], f).reshape(NF, 128, 1024))
    moe_gu = np.concatenate([gu(inp["moe_w_gate"][0][e], inp["moe_w_up"][0][e]) for e in range(8)], axis=0)
    moe_dn = np.ascontiguousarray(np.asarray(inp["moe_w_down"][0], f).reshape(8 * NF, 128, 1024))
    router = np.ascontiguousarray(np.asarray(inp["router_w"][0], f).reshape(8, 128, 8).transpose(1, 0, 2))
    return dict(gains=np.ascontiguousarray(gains), w_in=w_in, w_out=w_out, w_glu=w_glu, vec512=np.ascontiguousarray(vec), pool_w=pool_w,
                ssm_a=ssm_a, ssm_bc=ssm_bc, ffn_gu=ffn_gu, ffn_dn=ffn_dn, moe_gu=moe_gu, moe_dn=moe_dn, router=router)


def _x_layout(xb):
    return np.ascontiguousarray(np.asarray(xb, np.float32).T.reshape(8, 128, L).transpose(1, 0, 2))


def _out_layout(y):
    return np.ascontiguousarray(y.transpose(1, 0, 2).reshape(1024, L).T)


def kernel(**inputs):
    shared = _prep_shared(inputs)
    x = np.asarray(inputs["x"], np.float32)
    nb = x.shape[0]
    nc = build_program()
    in_maps = []
    for b in range(nb):
        m = dict(shared)
        m["xT"] = _x_layout(x[b])
        in_maps.append(m)
    res = run_bass_kernel_spmd(nc, in_maps, core_ids=list(range(nb)))
    out = np.stack([_out_layout(np.asarray(r["yT"])) for r in res.results], axis=0)
    return out.astype(np.float32)
```

```python
import math
import os
import numpy as np
from contextlib import ExitStack
import concourse.bass as bass
import concourse.mybir as mybir
from concourse.bass_utils import run_bass_kernel_spmd

F32 = mybir.dt.float32
BF16 = mybir.dt.bfloat16
I32 = mybir.dt.int32
ALU = mybir.AluOpType
AF = mybir.ActivationFunctionType
AX = mybir.AxisListType
ENGS = ["tensor", "vector", "scalar", "gpsimd", "sync"]

L = 2048
NTC = 4
TC = 512
J = 8
MB = L // J
NF = 22
NS = 8
GROUPS = [4, 4, 4, 4, 3, 3]
TWO_PI = 2.0 * math.pi
CARVE_INFO = {}


class Tok:
    __slots__ = ("sem", "val")

    def __init__(self, sem, val):
        self.sem = sem
        self.val = val


class T:
    def __init__(self, ap, name=""):
        self.ap = ap
        self.name = name
        self.last_w = None
        self.readers = []

    def __getitem__(self, k):
        return self.ap[k]


class DmaGroup:
    def __init__(self, sem):
        self.sem = sem
        self.count = 0


class Prog:
    def __init__(self, nc, ctx):
        self.nc = nc
        self.ctx = ctx
        self.streams = {e: [] for e in ENGS}
        self.sem = {e: ctx.enter_context(nc.semaphore("s_" + e)) for e in ENGS}
        self.cnt = {e: 0 for e in ENGS}
        self.waited = {e: {} for e in ENGS}
        self.pend_r = {e: [] for e in ENGS}
        self.pend_w = {e: [] for e in ENGS}
        self.groups = []
        self.ps_i = 0

    def sbuf(self, name, shape, dtype):
        return self.ctx.enter_context(self.nc.sbuf_tensor("sb_" + name, shape, dtype))

    def dma_group(self, name):
        g = DmaGroup(self.ctx.enter_context(self.nc.semaphore(name)))
        self.groups.append(g)
        return g

    def grp(self, key):
        if not hasattr(self, "_grps"):
            self._grps = {}
        if key not in self._grps:
            self._grps[key] = self.dma_group("dg_" + key)
        return self._grps[key]

    def _waits(self, eng, reads, writes, extra=()):
        need = {}

        def add(tok):
            if tok is None:
                return
            k = id(tok.sem)
            if k not in need or need[k].val < tok.val:
                need[k] = tok

        for t in reads:
            add(t.last_w)
            for e2 in ENGS:
                if e2 != eng:
                    assert not any(t is w for w in self.pend_w[e2]), ("RAW on unsignaled write", t.name, eng, e2)
        for t in writes:
            add(t.last_w)
            for r in t.readers:
                add(r)
            for e2 in ENGS:
                if e2 != eng:
                    assert not any(t is w for w in self.pend_r[e2]), ("WAR on unsignaled read", t.name, eng, e2)
                    assert not any(t is w for w in self.pend_w[e2]), ("WAW on unsignaled write", t.name, eng, e2)
        for tok in extra:
            add(tok)
        own = self.sem[eng]
        for k, tok in need.items():
            if eng == "tensor" and tok.sem is own:
                continue
            if self.waited[eng].get(k, 0) >= tok.val:
                continue
            self.waited[eng][k] = tok.val
            self.streams[eng].append(("wait", tok.sem, tok.val))

    def _commit(self, tok, reads, writes):
        for t in writes:
            t.last_w = tok
            t.readers = []
        for t in reads:
            rs = [r for r in t.readers if r.sem is not tok.sem]
            rs.append(tok)
            t.readers = rs

    def op(self, eng, fn, reads=(), writes=(), signal=True, extra=()):
        reads = list(reads)
        writes = list(writes)
        self._waits(eng, reads, writes, extra)
        if not signal:
            self.pend_r[eng] += reads
            self.pend_w[eng] += writes
            self.streams[eng].append(("op", fn, None))
            return None
        self.cnt[eng] += 1
        tok = Tok(self.sem[eng], self.cnt[eng])
        self.streams[eng].append(("op", fn, (tok.sem, 1)))
        self._commit(tok, reads + self.pend_r[eng], writes + self.pend_w[eng])
        self.pend_r[eng] = []
        self.pend_w[eng] = []
        return tok

    def dma(self, eng, fn, grp, reads=(), writes=(), extra=()):
        reads = list(reads)
        writes = list(writes)
        self._waits(eng, reads, writes, extra)
        if grp.count > 0:
            self.wait_tok(eng, Tok(grp.sem, 16 * grp.count))
        grp.count += 1
        tok = Tok(grp.sem, 16 * grp.count)
        self.streams[eng].append(("op", fn, (grp.sem, 16)))
        self._commit(tok, reads, writes)
        return tok

    def cond_begin(self, flag_ap, flag_T):
        for e in ENGS:
            assert not self.pend_r[e] and not self.pend_w[e]
        for e in ("tensor", "vector", "scalar", "gpsimd"):
            self._waits(e, [flag_T], [])
        if not hasattr(self, "_cond_stack"):
            self._cond_stack = []
        base = {id(self.sem[e]): self.cnt[e] for e in ENGS}
        for g in self.groups:
            base[id(g.sem)] = 16 * g.count
        self._cond_stack.append((flag_ap, self.streams, {e: dict(self.waited[e]) for e in ENGS}, base))
        self.streams = {e: [] for e in ENGS}

    def cond_end(self):
        flag_ap, outer, saved_waited, base = self._cond_stack.pop()
        inner = self.streams
        self.streams = outer
        for e in ENGS:
            assert not self.pend_r[e] and not self.pend_w[e]
            items = inner[e]
            if not items:
                continue
            incs = {}

            def acc(sem, n):
                k = id(sem)
                if k not in incs:
                    incs[k] = [sem, 0]
                incs[k][1] += n

            for it in items:
                if it[0] == "op" and it[2] is not None:
                    acc(it[2][0], it[2][1])
                elif it[0] == "cond":
                    for sem, n, _b in it[3]:
                        acc(sem, n)
            self.streams[e].append(("cond", flag_ap, items, [(sem, n, base.get(id(sem), 0)) for sem, n in incs.values()]))
            self.waited[e] = saved_waited[e]

    def wait_tok(self, eng, tok):
        k = id(tok.sem)
        if self.waited[eng].get(k, 0) < tok.val:
            self.waited[eng][k] = tok.val
            self.streams[eng].append(("wait", tok.sem, tok.val))

    def barrier(self):
        toks = [Tok(self.sem[e], self.cnt[e]) for e in ENGS if self.cnt[e] > 0]
        toks += [Tok(g.sem, 16 * g.count) for g in self.groups if g.count > 0]
        for e in ENGS:
            for t in toks:
                if t.sem is self.sem[e]:
                    continue
                self.wait_tok(e, t)

    def finish(self):
        nc = self.nc
        streams = self.streams

        dma_sem_ids = {id(g.sem) for g in self.groups}

        def run_items(e, items, regbox):
            for item in items:
                if item[0] == "wait":
                    e.wait_ge(item[1], item[2])
                elif item[0] == "cond":
                    if regbox[0] is None:
                        regbox[0] = e.alloc_register("flagreg")
                    r = regbox[0]
                    e.reg_load(r, item[1])
                    with e.If_eq(r, 1):
                        run_items(e, item[2], regbox)
                    with e.Else():
                        for sem, n, b in item[3]:
                            if b > 0 and id(sem) in dma_sem_ids:
                                e.wait_ge(sem, b)
                            e.sem_inc(sem, n)
                else:
                    ins = item[1](e)
                    if item[2] is not None:
                        ins.then_inc(item[2][0], item[2][1])

        def replay(name):
            def f(e):
                run_items(e, streams[name], [None])
            return f

        with nc.Block() as block:
            block.tensor(replay("tensor"))
            block.vector(replay("vector"))
            block.scalar(replay("scalar"))
            block.gpsimd(replay("gpsimd"))
            block.sync(replay("sync"))


def build_program(stages=("mix0", "ffn0", "mix1", "ffn1", "final"), dbg=False):
    nc = bass.Bass("TRN2", target_bir_lowering=False)

    def din(name, shape, dt=F32):
        return nc.dram_tensor(name, shape, dt, kind="ExternalInput").ap()

    d_x = din("xT", [128, 8, L])
    d_gains = din("gains", [128, 5, 8])
    d_win = din("w_in", [2, 128, 8, 1024])
    d_wout = din("w_out", [2, 128, 8, 1024])
    d_wglu = din("w_glu", [2, 128, 4, 512])
    d_vec = din("vec512", [128, 2, 3, 4])
    d_poolw = din("pool_w", [2, 128, 4, 128])
    d_ssma = din("ssm_a", [2, 128, 3, 16])
    d_ssmbc = din("ssm_bc", [2, 128, 4, 16, 16])
    d_fgu = din("ffn_gu", [NF, 128, 2, 8, 128])
    d_fdn = din("ffn_dn", [NF, 128, 1024])
    d_mgu = din("moe_gu", [8 * NF, 128, 2, 8, 128])
    d_mdn = din("moe_dn", [8 * NF, 128, 1024])
    d_router = din("router", [128, 8, 8])
    d_out = nc.dram_tensor("yT", [128, 8, L], F32, kind="ExternalOutput").ap()
    d_dbg = None
    if dbg:
        d_dbg = nc.dram_tensor("dbg", [4, 128, 8, L], F32, kind="ExternalOutput").ap()
        d_dbgh = nc.dram_tensor("dbgh", [2, 128, 8, L], BF16, kind="ExternalOutput").ap()
        d_dbgs = nc.dram_tensor("dbgs", [128, 41456], BF16, kind="ExternalOutput").ap()
        d_dbgw = nc.dram_tensor("dbgw", [128, 8192], BF16, kind="ExternalOutput").ap()

    with ExitStack() as ctx:
        P = Prog(nc, ctx)
        op = P.op

        xres_t = P.sbuf("xres", [128, 8, L], F32)
        h_t = P.sbuf("hbf", [128, 8, L], BF16)
        wbuf_t = P.sbuf("wbuf", [128, 8 * 1024], BF16)
        wglu_t = P.sbuf("wglu", [128, 4, 512], BF16)
        poolw_t = P.sbuf("poolw", [128, 4, 128], BF16)
        gains_t = P.sbuf("gains", [128, 5, 8], F32)
        vec_t = P.sbuf("vec", [128, 2, 3, 4], F32)
        router_t = P.sbuf("router", [128, 8, 8], F32)
        ssma_t = P.sbuf("ssma", [128, 3, 16], F32)
        ssmbc_t = P.sbuf("ssmbc", [128, 4, 16, 16], F32)
        ident_t = P.sbuf("ident", [128, 128], F32)
        onesb_t = P.sbuf("onesb", [128, 128], BF16)
        onesf_t = P.sbuf("onesf", [128, 128], F32)
        iota1_t = P.sbuf("iota1", [128, 256], F32)
        iotaf_t = P.sbuf("iotaf", [128, 128], F32)
        iotap_t = P.sbuf("iotap", [128, 1], F32)
        maskq_t = P.sbuf("maskq", [128, 4], F32)
        maskb_t = P.sbuf("maskb", [128, 4, 128], F32)
        invc_t = P.sbuf("invc", [128, 4, 16], F32)
        SCR_ELEMS = 41456
        scr_t = P.sbuf("scr", [128, SCR_ELEMS], BF16)
        psum_t = [ctx.enter_context(nc.psum_tensor(f"ps{i}", [128, 512], F32)) for i in range(8)]

        XR = [[T(xres_t[:, c, tc * TC:(tc + 1) * TC], f"xr{c}_{tc}") for tc in range(NTC)] for c in range(8)]
        H = [[T(h_t[:, c, tc * TC:(tc + 1) * TC], f"h{c}_{tc}") for tc in range(NTC)] for c in range(8)]
        WBUF = T(wbuf_t[:, :], "wbuf")
        WGLU = T(wglu_t[:], "wglu")
        POOLW = T(poolw_t[:], "poolw")
        GAINS = T(gains_t[:], "gains")
        VEC = T(vec_t[:], "vec")
        ROUTER = T(router_t[:], "router")
        SSMA = T(ssma_t[:], "ssma")
        SSMBC = T(ssmbc_t[:], "ssmbc")
        IDENT = T(ident_t[:], "ident")
        ONESB = T(onesb_t[:], "onesb")
        ONESF = T(onesf_t[:], "onesf")
        IOTA1 = T(iota1_t[:], "iota1")
        IOTAF = T(iotaf_t[:], "iotaf")
        IOTAP = T(iotap_t[:], "iotap")
        MASKQ = T(maskq_t[:], "maskq")
        MASKB = T(maskb_t[:], "maskb")
        INVC = T(invc_t[:], "invc")
        PS = [T(psum_t[i][:], f"ps{i}") for i in range(8)]

        def next_ps():
            p = PS[P.ps_i % 8]
            P.ps_i += 1
            return p

        class Carver:
            def __init__(self):
                self.off = 0

            def take(self, shape, dtype, name):
                n = int(np.prod(shape))
                el = n * (2 if dtype in (F32, I32) else 1)
                if self.off % 2:
                    self.off += 1
                a = scr_t[:, self.off:self.off + el]
                CARVE_INFO[name] = (self.off, tuple(shape), str(dtype))
                self.off += el
                assert self.off <= SCR_ELEMS, (name, self.off)
                if dtype != BF16:
                    a = a.bitcast(dtype)
                if len(shape) == 2:
                    a = a.rearrange("p (a b) -> p a b", a=shape[0])
                elif len(shape) == 3:
                    a = a.rearrange("p (a b c) -> p a b c", a=shape[0], b=shape[1])
                elif len(shape) == 4:
                    a = a.rearrange("p (a b c d) -> p a b c d", a=shape[0], b=shape[1], c=shape[2])
                return T(a, name)

        g_out = P.dma_group("g_dbg")

        op("gpsimd", lambda e: e.iota(iotaf_t[:], [[1, 128]], base=0, channel_multiplier=0, allow_small_or_imprecise_dtypes=True), writes=[IOTAF])
        op("gpsimd", lambda e: e.iota(iotap_t[:], [[1, 1]], base=0, channel_multiplier=1, allow_small_or_imprecise_dtypes=True), writes=[IOTAP])
        op("gpsimd", lambda e: e.iota(iota1_t[:], [[1, 256]], base=1, channel_multiplier=0, allow_small_or_imprecise_dtypes=True), writes=[IOTA1])
        op("vector", lambda e: e.tensor_scalar(out=ident_t[:], in0=iotaf_t[:], scalar1=iotap_t[:, 0:1], scalar2=None, op0=ALU.is_equal), reads=[IOTAF, IOTAP], writes=[IDENT])
        op("vector", lambda e: e.memset(onesb_t[:], 1.0), writes=[ONESB])
        op("vector", lambda e: e.memset(onesf_t[:], 1.0), writes=[ONESF])
        tmpm_t = P.sbuf("tmpm", [128, 4], F32)
        TMPM = T(tmpm_t[:], "tmpm")
        for q4 in range(4):
            op("vector", lambda e, q4=q4: e.tensor_single_scalar(out=maskq_t[:, q4:q4 + 1], in_=iotap_t[:, 0:1], scalar=float(32 * q4), op=ALU.is_ge), reads=[IOTAP], writes=[MASKQ])
            op("vector", lambda e, q4=q4: e.tensor_single_scalar(out=tmpm_t[:, q4:q4 + 1], in_=iotap_t[:, 0:1], scalar=float(32 * q4 + 32), op=ALU.is_lt), reads=[IOTAP], writes=[TMPM])
        op("vector", lambda e: e.tensor_tensor(out=maskq_t[:], in0=maskq_t[:], in1=tmpm_t[:], op=ALU.mult), reads=[MASKQ, TMPM], writes=[MASKQ])
        for q4 in range(4):
            op("vector", lambda e, q4=q4: e.tensor_copy(out=maskb_t[:, :, 32 * q4:32 * q4 + 32], in_=maskq_t[:, q4:q4 + 1].unsqueeze(2).to_broadcast([128, 4, 32])),
               reads=[MASKQ], writes=[MASKB])
        for g in range(4):
            op("vector", lambda e, g=g: e.tensor_single_scalar(out=invc_t[:, g, :], in_=iota1_t[:, 0:16], scalar=float(2 ** (g + 1)), op=ALU.min), reads=[IOTA1], writes=[INVC])
        op("vector", lambda e: e.reciprocal(out=invc_t[:], in_=invc_t[:]), reads=[INVC], writes=[INVC])

        for c in range(8):
            P.dma("sync", lambda e, c=c: e.dma_start(out=xres_t[:, c, :], in_=d_x[:, c, :]), P.grp(f"x{c}"), writes=XR[c])
        P.dma("sync", lambda e: e.dma_start(out=gains_t[:], in_=d_gains), P.grp("gains"), writes=[GAINS])
        P.dma("sync", lambda e: e.dma_start(out=vec_t[:], in_=d_vec), P.grp("vec"), writes=[VEC])
        P.dma("sync", lambda e: e.dma_start(out=router_t[:], in_=d_router), P.grp("router"), writes=[ROUTER])

        def load_wbuf(src):
            P.dma("gpsimd", lambda e: e.dma_start(out=wbuf_t[:, :].rearrange("p (a b) -> p a b", a=8), in_=src), P.grp("wbuf"), writes=[WBUF])

        def load_mixer_small(l):
            P.dma("gpsimd", lambda e: e.dma_start(out=wglu_t[:], in_=d_wglu[l]), P.grp("wglu"), writes=[WGLU])
            P.dma("gpsimd", lambda e: e.dma_start(out=poolw_t[:], in_=d_poolw[l]), P.grp("poolw"), writes=[POOLW])
            P.dma("sync", lambda e: e.dma_start(out=ssma_t[:], in_=d_ssma[l]), P.grp("ssma"), writes=[SSMA])
            P.dma("sync", lambda e: e.dma_start(out=ssmbc_t[:], in_=d_ssmbc[l]), P.grp("ssmbc"), writes=[SSMBC])

        def rmsnorm(gi, sqb, rst, tmpl, sink, rstd_keep=None):
            for tc in range(NTC):
                ps = next_ps()
                for c in range(8):
                    sq = sqb[(tc * 8 + c) % len(sqb)]
                    op("scalar", lambda e, sq=sq, c=c, tc=tc: e.activation(out=sq[:], in_=XR[c][tc][:], func=AF.Square), reads=[XR[c][tc]], writes=[sq])
                    op("tensor", lambda e, sq=sq, ps=ps, c=c: e.matmul(ps[:], lhsT=onesb_t[:], rhs=sq[:], start=(c == 0), stop=(c == 7)),
                       reads=[sq, ONESB], writes=[ps], signal=True)
                r = rst[tc % len(rst)] if rstd_keep is None else rstd_keep[tc]
                op("scalar", lambda e, ps=ps: e.activation(out=tmpl[:], in_=ps[:], func=AF.Ln, scale=1.0 / 1024.0, bias=1e-6), reads=[ps], writes=[tmpl])
                op("scalar", lambda e, r=r: e.activation(out=r[:], in_=tmpl[:], func=AF.Exp, scale=-0.5), reads=[tmpl], writes=[r])
                for c in range(8):
                    sink(c, tc, r)

        def norm_to_h(gi):
            def sink(c, tc, r):
                op("vector", lambda e: e.scalar_tensor_tensor(out=H[c][tc][:], in0=XR[c][tc][:], scalar=gains_t[:, gi, c:c + 1], in1=r[:], op0=ALU.mult, op1=ALU.mult),
                   reads=[XR[c][tc], GAINS, r], writes=[H[c][tc]])
            return sink

        def mixer(l):
            cv = Carver()
            USSM = [[cv.take([8, 64], BF16, f"u{mc}_{tc}") for tc in range(NTC)] for mc in range(4)]
            SBF = [[cv.take([2, 260], BF16, f"sbf{s}_{q4}") for q4 in range(4)] for s in range(2)]
            NTMP = 12
            off_tmps = cv.off
            TMPS = [[cv.take([256], F32, f"st{s}_{i}") for i in range(NTMP)] for s in range(1)]
            off_pb = cv.off
            PBRE = cv.take([8, 4, 32], F32, "pbre")
            PBIM = cv.take([8, 4, 32], F32, "pbim")
            off_wx = cv.off
            WX = cv.take([16, 4, 128], BF16, "wx")
            KD = cv.take([8, 128], BF16, "kd")
            POOLED = [cv.take([512], BF16, f"pooled{i}") for i in range(4)]
            TAIL = [cv.take([16], F32, f"tail{g}") for g in range(4)]
            cva = Carver(); cva.off = off_tmps
            XH = [cva.take([528], F32, f"xh{i}") for i in range(2)]
            SA = cva.take([528], F32, "sa")
            SB_ = cva.take([528], F32, "sb")
            cvb = Carver(); cvb.off = off_wx
            SQB = [cvb.take([512], BF16, f"sqb{i}") for i in range(4)]
            RST = [cvb.take([512], F32, f"rst{i}") for i in range(2)]
            TMPL = cvb.take([512], F32, "tmpl")
            assert cvb.off <= off_wx + 8192
            cvc = Carver(); cvc.off = off_pb
            SGT = [cvc.take([512], F32, f"sgt{i}") for i in range(2)]
            def p16(name):
                return cv.take([16], F32, name)
            DT, AR, ZR, ZI, MAG, Q1, QF, PH, SN, PA, CS, LR, LI, DEN, RDEN, NR, CR, CI, TA, TB, RHO8, F8 = [p16(f"pp{i}") for i in range(22)]
            QI = cv.take([16], I32, "qi")
            PWR = cv.take([9, 16], F32, "pwr")
            PWI = cv.take([9, 16], F32, "pwi")
            BBRE = cv.take([16, 16], F32, "bbre")
            BBIM = cv.take([16, 16], F32, "bbim")
            BT1 = cv.take([16, 16], F32, "bt1")
            CRET = cv.take([16, 32], F32, "cret")
            NCIMT = cv.take([16, 32], F32, "ncimt")
            PBT = cv.take([8, 4, 16], F32, "pbt")
            WCT1 = cv.take([8, 16], F32, "wct1")
            WCT2 = cv.take([8, 16], F32, "wct2")
            DIAGD = cv.take([128], F32, "diagd")
            KTMP = cv.take([128], F32, "ktmp")
            TI = [cv.take([256], I32, f"ti{s}") for s in range(1)]

            V = lambda e: e

            def tt(eng, out, in0, in1, o, reads, writes):
                return op(eng, lambda e: e.tensor_tensor(out=out, in0=in0, in1=in1, op=o), reads=reads, writes=writes)

            load_wbuf(d_win[l])
            load_mixer_small(l)

            rmsnorm(2 * l, SQB, RST, TMPL, norm_to_h(2 * l))

            A_ldt = ssma_t[:, 0, :]
            A_are = ssma_t[:, 1, :]
            A_aim = ssma_t[:, 2, :]
            op("scalar", lambda e: e.activation(out=DT[:], in_=A_ldt, func=AF.Exp), reads=[SSMA], writes=[DT])
            op("vector", lambda e: e.tensor_single_scalar(out=AR[:], in_=A_are, scalar=-1e-4, op=ALU.min), reads=[SSMA], writes=[AR])
            tt("vector", ZR[:], AR[:], DT[:], ALU.mult, [AR, DT], [ZR])
            tt("vector", ZI[:], A_aim, DT[:], ALU.mult, [SSMA, DT], [ZI])
            op("scalar", lambda e: e.activation(out=MAG[:], in_=ZR[:], func=AF.Exp), reads=[ZR], writes=[MAG])

            def sincos(src, scale, SNo, CSo):
                op("vector", lambda e: e.tensor_single_scalar(out=Q1[:], in_=src[:], scalar=scale / TWO_PI, op=ALU.mult), reads=[src], writes=[Q1])
                op("vector", lambda e: e.tensor_copy(out=QI[:], in_=Q1[:]), reads=[Q1], writes=[QI])
                op("vector", lambda e: e.tensor_copy(out=QF[:], in_=QI[:]), reads=[QI], writes=[QF])
                tt("vector", PH[:], Q1[:], QF[:], ALU.subtract, [Q1, QF], [PH])
                if SNo is not None:
                    op("scalar", lambda e: e.activation(out=SNo[:], in_=PH[:], func=AF.Sin, scale=TWO_PI), reads=[PH], writes=[SNo])
                if CSo is not None:
                    op("vector", lambda e: e.scalar_tensor_tensor(out=PA[:], in0=PH[:], scalar=-1.0, in1=PH[:], op0=ALU.mult, op1=ALU.max), reads=[PH], writes=[PA])
                    op("scalar", lambda e: e.activation(out=CSo[:], in_=PA[:], func=AF.Sin, scale=-TWO_PI, bias=HALFPI[:, 0:1]), reads=[PA, HALFPI_T], writes=[CSo])

            sincos(ZI, 1.0, SN, CS)
            tt("vector", LR[:], MAG[:], CS[:], ALU.mult, [MAG, CS], [LR])
            tt("vector", LI[:], MAG[:], SN[:], ALU.mult, [MAG, SN], [LI])
            tt("vector", TA[:], AR[:], AR[:], ALU.mult, [AR], [TA])
            tt("vector", TB[:], A_aim, A_aim, ALU.mult, [SSMA], [TB])
            tt("vector", DEN[:], TA[:], TB[:], ALU.add, [TA, TB], [DEN])
            op("vector", lambda e: e.reciprocal(out=RDEN[:], in_=DEN[:]), reads=[DEN], writes=[RDEN])
            op("vector", lambda e: e.tensor_single_scalar(out=NR[:], in_=LR[:], scalar=-1.0, op=ALU.add), reads=[LR], writes=[NR])
            tt("vector", TA[:], NR[:], AR[:], ALU.mult, [NR, AR], [TA])
            tt("vector", TB[:], LI[:], A_aim, ALU.mult, [LI, SSMA], [TB])
            tt("vector", TA[:], TA[:], TB[:], ALU.add, [TA, TB], [TA])
            tt("vector", CR[:], TA[:], RDEN[:], ALU.mult, [TA, RDEN], [CR])
            tt("vector", TA[:], LI[:], AR[:], ALU.mult, [LI, AR], [TA])
            tt("vector", TB[:], NR[:], A_aim, ALU.mult, [NR, SSMA], [TB])
            tt("vector", TA[:], TA[:], TB[:], ALU.subtract, [TA, TB], [TA])
            tt("vector", CI[:], TA[:], RDEN[:], ALU.mult, [TA, RDEN], [CI])
            b_re = ssmbc_t[:, 0]
            b_im = ssmbc_t[:, 1]
            c_re = ssmbc_t[:, 2]
            c_im = ssmbc_t[:, 3]
            crb = CR[:].unsqueeze(2).to_broadcast([128, 16, 16])
            cib = CI[:].unsqueeze(2).to_broadcast([128, 16, 16])
            tt("vector", BBRE[:], b_re, crb, ALU.mult, [SSMBC, CR], [BBRE])
            tt("vector", BT1[:], b_im, cib, ALU.mult, [SSMBC, CI], [BT1])
            tt("vector", BBRE[:], BBRE[:], BT1[:], ALU.subtract, [BBRE, BT1], [BBRE])
            tt("vector", BBIM[:], b_im, crb, ALU.mult, [SSMBC, CR], [BBIM])
            tt("vector", BT1[:], b_re, cib, ALU.mult, [SSMBC, CI], [BT1])
            tt("vector", BBIM[:], BBIM[:], BT1[:], ALU.add, [BBIM, BT1], [BBIM])
            op("vector", lambda e: e.memset(PWR[:, 0, :], 1.0), writes=[PWR])
            op("vector", lambda e: e.memset(PWI[:, 0, :], 0.0), writes=[PWI])
            for k in range(8):
                tt("vector", TA[:], PWR[:, k, :], LR[:], ALU.mult, [PWR, LR], [TA])
                tt("vector", TB[:], PWI[:, k, :], LI[:], ALU.mult, [PWI, LI], [TB])
                tt("vector", PWR[:, k + 1, :], TA[:], TB[:], ALU.subtract, [TA, TB], [PWR])
                tt("vector", TA[:], PWR[:, k, :], LI[:], ALU.mult, [PWR, LI], [TA])
                tt("vector", TB[:], PWI[:, k, :], LR[:], ALU.mult, [PWI, LR], [TB])
                tt("vector", PWI[:, k + 1, :], TA[:], TB[:], ALU.add, [TA, TB], [PWI])
            op("scalar", lambda e: e.activation(out=RHO8[:], in_=ZR[:], func=AF.Exp, scale=float(J)), reads=[ZR], writes=[RHO8])
            op("vector", lambda e: e.tensor_single_scalar(out=Q1[:], in_=ZI[:], scalar=float(J) / TWO_PI, op=ALU.mult), reads=[ZI], writes=[Q1])
            op("vector", lambda e: e.tensor_copy(out=QI[:], in_=Q1[:]), reads=[Q1], writes=[QI])
            op("vector", lambda e: e.tensor_copy(out=QF[:], in_=QI[:]), reads=[QI], writes=[QF])
            tt("vector", F8[:], Q1[:], QF[:], ALU.subtract, [Q1, QF], [F8])
            op("vector", lambda e: e.memset(CRET[:], 0.0), writes=[CRET])
            op("vector", lambda e: e.memset(NCIMT[:], 0.0), writes=[NCIMT])
            for h2 in range(2):
                ps_ = slice(64 * h2, 64 * h2 + 64)
                cs_ = slice(16 * h2, 16 * h2 + 16)
                op("vector", lambda e, ps_=ps_, cs_=cs_: e.tensor_copy(out=CRET[ps_, :, cs_], in_=c_re[ps_]), reads=[SSMBC], writes=[CRET])
                op("vector", lambda e, ps_=ps_, cs_=cs_: e.tensor_single_scalar(out=NCIMT[ps_, :, cs_], in_=c_im[ps_], scalar=-1.0, op=ALU.mult), reads=[SSMBC], writes=[NCIMT])
            op("vector", lambda e: e.memset(PBRE[:], 0.0), writes=[PBRE])
            op("vector", lambda e: e.memset(PBIM[:], 0.0), writes=[PBIM])
            for s in range(2):
                for q4 in range(4):
                    op("vector", lambda e, s=s, q4=q4: e.memset(SBF[s][q4][:], 0.0), writes=[SBF[s][q4]])

            for tc in range(NTC):
                for mc in [4, 5, 6, 7, 0, 1, 2, 3]:
                    ps = next_ps()
                    for k in range(8):
                        op("tensor", lambda e, ps=ps, k=k, mc=mc, tc=tc: e.matmul(ps[:], lhsT=wbuf_t[:, k * 1024 + mc * 128:k * 1024 + mc * 128 + 128], rhs=H[k][tc][:],
                                                                                   start=(k == 0), stop=(k == 7)),
                           reads=[WBUF, H[k][tc]], writes=[ps], signal=(k == 7))
                    if mc < 4:
                        u = USSM[mc][tc]
                        op("scalar", lambda e, ps=ps, u=u: e.activation(out=u[:], in_=ps[:].rearrange("p (m i) -> p i m", i=8), func=AF.Copy), reads=[ps], writes=[u])
                    else:
                        g = mc - 4
                        w = 2 ** (g + 1)
                        xh = XH[(tc * 4 + g) % 2]
                        op("scalar", lambda e, ps=ps, xh=xh: e.activation(out=xh[:, 16:528], in_=ps[:], func=AF.Copy), reads=[ps], writes=[xh])
                        if tc == 0:
                            op("vector", lambda e, xh=xh: e.memset(xh[:, 0:16], 0.0), writes=[xh])
                        else:
                            op("vector", lambda e, xh=xh, g=g: e.tensor_copy(out=xh[:, 0:16], in_=TAIL[g][:]), reads=[TAIL[g]], writes=[xh])
                        op("vector", lambda e, xh=xh, g=g: e.tensor_copy(out=TAIL[g][:], in_=xh[:, 512:528]), reads=[xh], writes=[TAIL[g]])
                        src = xh
                        bufs = [SA, SB_]
                        sh = 1
                        lo = 1
                        for lev in range(g + 1):
                            dst = bufs[lev % 2]
                            op("vector", lambda e, src=src, dst=dst, sh=sh, lo=lo: e.tensor_tensor(out=dst[:, lo:528], in0=src[:, lo:528], in1=src[:, lo - sh:528 - sh], op=ALU.add),
                               reads=[src], writes=[dst])
                            src = dst
                            sh *= 2
                            lo += sh
                        pl = POOLED[g]
                        op("vector", lambda e, src=src, xh=xh, pl=pl, w=w: e.scalar_tensor_tensor(out=pl[:], in0=src[:, 16:528], scalar=1.0 / w, in1=xh[:, 16:528], op0=ALU.mult, op1=ALU.subtract),
                           reads=[src, xh], writes=[pl])
                        if tc == 0:
                            tmpc = TMPL
                            op("vector", lambda e, src=src, g=g: e.tensor_tensor(out=TMPL[:, 0:16], in0=src[:, 16:32], in1=invc_t[:, g, :], op=ALU.mult), reads=[src, INVC], writes=[TMPL])
                            op("vector", lambda e, xh=xh, pl=pl: e.tensor_tensor(out=pl[:, 0:16], in0=TMPL[:, 0:16], in1=xh[:, 16:32], op=ALU.subtract), reads=[TMPL, xh], writes=[pl])
                for g in range(4):
                    ps = next_ps()
                    op("tensor", lambda e, ps=ps, g=g: e.matmul(ps[:], lhsT=poolw_t[:, g, :], rhs=POOLED[g][:], start=True, stop=True), reads=[POOLW, POOLED[g]], writes=[ps])
                    op("scalar", lambda e, ps=ps, g=g, tc=tc: e.activation(out=H[4 + g][tc][:], in_=ps[:], func=AF.Identity, scale=vec_t[:, l, 1, g:g + 1]), reads=[ps, VEC], writes=[H[4 + g][tc]])

            op("gpsimd", lambda e: e.memset(wbuf_t[:, :], 0.0), writes=[WBUF])
            WC = wbuf_t[:, :].rearrange("p (j q r n) -> p j q r n", j=8, q=4, r=2)

            first_tab_extra = [Tok(P.sem[e_], P.cnt[e_]) for e_ in ("vector", "scalar", "tensor") if P.cnt[e_] > 0]
            for qt in range(4):
                for h2 in range(2):
                    pp = slice(64 * h2, 64 * h2 + 64)
                    cc = slice(16 * h2, 16 * h2 + 16)
                    pr = PWR[pp, 0:8, 4 * qt:4 * qt + 4].unsqueeze(3).to_broadcast([64, 8, 4, 16])
                    pi = PWI[pp, 0:8, 4 * qt:4 * qt + 4].unsqueeze(3).to_broadcast([64, 8, 4, 16])
                    br = BBRE[pp, 4 * qt:4 * qt + 4, :].unsqueeze(1).to_broadcast([64, 8, 4, 16])
                    bi = BBIM[pp, 4 * qt:4 * qt + 4, :].unsqueeze(1).to_broadcast([64, 8, 4, 16])
                    tt("vector", PBRE[pp, :, :, cc], pr, br, ALU.mult, [PWR, BBRE], [PBRE])
                    tt("vector", PBT[pp], pi, bi, ALU.mult, [PWI, BBIM], [PBT])
                    tt("vector", PBRE[pp, :, :, cc], PBRE[pp, :, :, cc], PBT[pp], ALU.subtract, [PBRE, PBT], [PBRE])
                    tt("vector", PBIM[pp, :, :, cc], pr, bi, ALU.mult, [PWR, BBIM], [PBIM])
                    tt("vector", PBT[pp], pi, br, ALU.mult, [PWI, BBRE], [PBT])
                    tt("vector", PBIM[pp, :, :, cc], PBIM[pp, :, :, cc], PBT[pp], ALU.add, [PBIM, PBT], [PBIM])
                for b in range(4):
                    ps = next_ps()
                    for i4 in range(4):
                        idx = 4 * b + i4
                        j = idx // 2
                        reim = idx % 2
                        src = PBRE if reim == 0 else PBIM
                        op("tensor", lambda e, ps=ps, i4=i4, src=src, j=j: e.transpose(ps[:, i4 * 128:(i4 + 1) * 128], src[:, 7 - j, :, :].rearrange("p a b -> p (a b)"), ident_t[:]),
                           reads=[src, IDENT], writes=[ps], signal=(i4 == 3))
                    for q4 in range(4):
                        op("scalar", lambda e, ps=ps, b=b, q4=q4: e.activation(out=WX[:, 4 * b:4 * b + 4, q4, :], in_=ps[:].rearrange("p (a n) -> p a n", a=4), func=AF.Identity,
                                                                                scale=maskq_t[:, q4:q4 + 1]), reads=[ps, MASKQ], writes=[WX],
                           extra=(first_tab_extra if (qt == 0 and b == 0 and q4 == 0) else ()))
                psk = [next_ps(), next_ps()]
                for d in range(8):
                    pk = psk[d // 4]
                    oc = slice((d % 4) * 128, (d % 4) * 128 + 128)
                    op("tensor", lambda e, pk=pk, oc=oc, d=d, qt=qt: e.matmul(pk[:, oc], lhsT=PBRE[:, d, :, :].rearrange("p a b -> p (a b)"),
                                                                       rhs=CRET[:, 4 * qt:4 * qt + 4, :].rearrange("p a b -> p (a b)"), start=True, stop=False),
                       reads=[PBRE, CRET], writes=[pk], signal=False)
                    op("tensor", lambda e, pk=pk, oc=oc, d=d, qt=qt: e.matmul(pk[:, oc], lhsT=PBIM[:, d, :, :].rearrange("p a b -> p (a b)"),
                                                                       rhs=NCIMT[:, 4 * qt:4 * qt + 4, :].rearrange("p a b -> p (a b)"), start=False, stop=True),
                       reads=[PBIM, NCIMT], writes=[pk], signal=(d % 4 == 3))
                op("vector", lambda e, qt=qt: e.tensor_scalar(out=DIAGD[:], in0=ident_t[:], scalar1=vec_t[:, l, 2, qt:qt + 1], scalar2=None, op0=ALU.mult), reads=[IDENT, VEC], writes=[DIAGD])
                tt("vector", KTMP[:], psk[0][:, 0:128], maskb_t[:, 0, :], ALU.mult, [psk[0], MASKB], [KTMP])
                tt("vector", KD[:, 0, :], KTMP[:], DIAGD[:], ALU.add, [KTMP, DIAGD], [KD])
                tt("vector", KD[:, 1:4, :], psk[0][:, 128:512].rearrange("p (a n) -> p a n", a=3), maskb_t[:, 0:3, :], ALU.mult, [psk[0], MASKB], [KD])
                tt("vector", KD[:, 4:8, :], psk[1][:].rearrange("p (a n) -> p a n", a=4), maskb_t[:], ALU.mult, [psk[1], MASKB], [KD])
                for q4 in range(4):
                    q = 4 * qt + q4
                    for h2 in range(2):
                        pp = slice(64 * h2, 64 * h2 + 64)
                        cc = slice(32 * q4 + 16 * h2, 32 * q4 + 16 * h2 + 16)
                        crb_ = c_re[pp, q, :].unsqueeze(1).to_broadcast([64, 8, 16])
                        cib_ = c_im[pp, q, :].unsqueeze(1).to_broadcast([64, 8, 16])
                        prb = PWR[pp, 1:9, q].unsqueeze(2).to_broadcast([64, 8, 16])
                        pib = PWI[pp, 1:9, q].unsqueeze(2).to_broadcast([64, 8, 16])
                        tt("vector", WCT1[pp], crb_, prb, ALU.mult, [SSMBC, PWR], [WCT1])
                        tt("vector", WCT2[pp], cib_, pib, ALU.mult, [SSMBC, PWI], [WCT2])
                        tt("vector", WC[pp, :, q4, 0, cc], WCT1[pp], WCT2[pp], ALU.subtract, [WCT1, WCT2], [WBUF])
                        tt("vector", WCT1[pp], crb_, pib, ALU.mult, [SSMBC, PWI], [WCT1])
                        tt("vector", WCT2[pp], cib_, prb, ALU.mult, [SSMBC, PWR], [WCT2])
                        op("vector", lambda e, pp=pp, q4=q4, cc=cc: e.scalar_tensor_tensor(out=WC[pp, :, q4, 1, cc], in0=WCT1[pp], scalar=-1.0, in1=WCT2[pp], op0=ALU.mult, op1=ALU.subtract),
                           reads=[WCT1, WCT2], writes=[WBUF])
                sset = qt % 2
                for q4 in range(4):
                    q = 4 * qt + q4
                    psx = next_ps()
                    for reim in range(2):
                        for j in range(8):
                            op("tensor", lambda e, psx=psx, reim=reim, j=j, q4=q4, qt=qt: e.matmul(
                                psx[:, reim * 256:(reim + 1) * 256].rearrange("p (a m) -> p a m", a=4),
                                lhsT=WX[:, 2 * j + reim, q4, :],
                                rhs=scr_u[qt][:, :, j, :], start=(j == 0), stop=(j == 7)),
                               reads=[WX] + USSM[qt], writes=[psx], signal=(reim == 1 and j == 7))
                    tm = TMPS[0]
                    PHr, TFp, SN0, SN1, CS0, CS1, T1, T2, VRE, VIM, RRE, RIM = tm
                    SNt = SN0 if q % 2 == 0 else SN1
                    CSt = CS0 if q % 2 == 0 else CS1
                    tix = TI[0]
                    ex_ = first_tab_extra if (qt == 0 and q4 == 0) else ()
                    op("vector", lambda e, q=q: e.tensor_scalar(out=PHr[:], in0=iota1_t[:], scalar1=F8[:, q:q + 1], scalar2=None, op0=ALU.mult), reads=[IOTA1, F8], writes=[PHr], extra=ex_)
                    op("vector", lambda e: e.tensor_copy(out=tix[:], in_=PHr[:]), reads=[PHr], writes=[tix])
                    op("vector", lambda e: e.tensor_copy(out=TFp[:], in_=tix[:]), reads=[tix], writes=[TFp])
                    op("vector", lambda e: e.tensor_tensor(out=PHr[:], in0=PHr[:], in1=TFp[:], op=ALU.subtract), reads=[PHr, TFp], writes=[PHr])
                    op("vector", lambda e: e.scalar_tensor_tensor(out=TFp[:], in0=PHr[:], scalar=-1.0, in1=PHr[:], op0=ALU.mult, op1=ALU.max), reads=[PHr], writes=[TFp])
                    op("scalar", lambda e, SNt=SNt: e.activation(out=SNt[:], in_=PHr[:], func=AF.Sin, scale=TWO_PI), reads=[PHr], writes=[SNt])
                    op("scalar", lambda e, CSt=CSt: e.activation(out=CSt[:], in_=TFp[:], func=AF.Sin, scale=-TWO_PI, bias=HALFPI[:, 0:1]), reads=[TFp, HALFPI_T], writes=[CSt])
                    xre = psx[:, 0:256]
                    xim = psx[:, 256:512]
                    tt("vector", T1[:], xre, CSt[:], ALU.mult, [psx, CSt], [T1])
                    tt("vector", T2[:], xim, SNt[:], ALU.mult, [psx, SNt], [T2])
                    tt("vector", VRE[:], T1[:], T2[:], ALU.add, [T1, T2], [VRE])
                    tt("vector", T1[:], xim, CSt[:], ALU.mult, [psx, CSt], [T1])
                    tt("vector", T2[:], xre, SNt[:], ALU.mult, [psx, SNt], [T2])
                    tt("vector", VIM[:], T1[:], T2[:], ALU.subtract, [T1, T2], [VIM])
                    rho = RHO8[:, q:q + 1].to_broadcast([128, 256])
                    op("vector", lambda e, VRE=VRE, RRE=RRE, rho=rho: e.tensor_tensor_scan(out=RRE[:], data0=rho, data1=VRE[:], initial=0.0, op0=ALU.mult, op1=ALU.add), reads=[VRE, RHO8], writes=[RRE])
                    op("vector", lambda e, VIM=VIM, RIM=RIM, rho=rho: e.tensor_tensor_scan(out=RIM[:], data0=rho, data1=VIM[:], initial=0.0, op0=ALU.mult, op1=ALU.add), reads=[VIM, RHO8], writes=[RIM])
                    sb = SBF[sset][q4]
                    tt("vector", T1[:], RRE[:], CSt[:], ALU.mult, [RRE, CSt], [T1])
                    tt("vector", T2[:], RIM[:], SNt[:], ALU.mult, [RIM, SNt], [T2])
                    tt("vector", sb[:, 0, 1:257], T1[:], T2[:], ALU.subtract, [T1, T2], [sb])
                    tt("vector", T1[:], RRE[:], SNt[:], ALU.mult, [RRE, SNt], [T1])
                    tt("vector", T2[:], RIM[:], CSt[:], ALU.mult, [RIM, CSt], [T2])
                    tt("vector", sb[:, 1, 1:257], T1[:], T2[:], ALU.add, [T1, T2], [sb])
                for tc in range(NTC):
                    psy = next_ps()
                    for j in range(8):
                        oc = slice(j * 64, j * 64 + 64)
                        first = True
                        for q4 in range(4):
                            for reim in range(2):
                                op("tensor", lambda e, psy=psy, oc=oc, j=j, q4=q4, reim=reim, tc=tc, first=first, sset=sset: e.matmul(
                                    psy[:, oc], lhsT=WC[:, j, q4, reim, :], rhs=SBF[sset][q4][:, reim, 64 * tc:64 * tc + 64], start=first, stop=False),
                                   reads=[WBUF, SBF[sset][q4]], writes=[psy], signal=False)
                                first = False
                        for i in range(j + 1):
                            op("tensor", lambda e, psy=psy, oc=oc, j=j, i=i, tc=tc, qt=qt: e.matmul(psy[:, oc], lhsT=KD[:, j - i, :], rhs=USSM[qt][tc][:, i, :], start=False, stop=(i == j)),
                               reads=[KD, USSM[qt][tc]], writes=[psy], signal=(j == 7 and i == j))
                    op("scalar", lambda e, psy=psy, tc=tc, qt=qt: e.activation(out=H[qt][tc][:].rearrange("p (m j) -> p m j", j=8), in_=psy[:].rearrange("p (j m) -> p m j", j=8),
                                                                        func=AF.Gelu_apprx_tanh), reads=[psy], writes=[H[qt][tc]])

            if dbg and l == 0:
                P.barrier()
                P.dma("sync", lambda e: e.dma_start(out=d_dbgw, in_=wbuf_t[:, :]), g_out)
                for c in range(8):
                    P.dma("sync", lambda e, c=c: e.dma_start(out=d_dbgh[1, :, c, :], in_=h_t[:, c, :]), g_out)
                P.barrier()
            for tc in range(NTC):
                pss = [next_ps() for _ in range(4)]
                for mo in range(4):
                    for k in range(4):
                        op("tensor", lambda e, mo=mo, k=k, tc=tc, pss=pss: e.matmul(pss[mo][:], lhsT=wglu_t[:, k, mo * 128:(mo + 1) * 128], rhs=H[k][tc][:], start=(k == 0), stop=(k == 3)),
                           reads=[WGLU, H[k][tc]], writes=[pss[mo]], signal=(k == 3))
                for mo in range(4):
                    sg = SGT[mo % 2]
                    op("scalar", lambda e, mo=mo, sg=sg, pss=pss: e.activation(out=sg[:], in_=pss[mo][:], func=AF.Sigmoid, bias=vec_t[:, l, 0, mo:mo + 1]), reads=[pss[mo], VEC], writes=[sg])
                    tt("vector", H[mo][tc][:], H[mo][tc][:], sg[:], ALU.mult, [H[mo][tc], sg], [H[mo][tc]])

            dumph(l)
            if dbg and l == 0:
                P.barrier()
                P.dma("sync", lambda e: e.dma_start(out=d_dbgs, in_=scr_t[:, :]), g_out)
                P.barrier()
            load_wbuf(d_wout[l])
            for tc in range(NTC):
                for mc in range(8):
                    ps = next_ps()
                    for k in range(8):
                        op("tensor", lambda e, ps=ps, k=k, mc=mc, tc=tc: e.matmul(ps[:], lhsT=wbuf_t[:, k * 1024 + mc * 128:k * 1024 + mc * 128 + 128], rhs=H[k][tc][:],
                                                                                   start=(k == 0), stop=(k == 7)),
                           reads=[WBUF, H[k][tc]], writes=[ps], signal=(k == 7))
                    if dbg and False:
                        pass
                    tt("vector", XR[mc][tc][:], ps[:], XR[mc][tc][:], ALU.add, [ps, XR[mc][tc]], [XR[mc][tc]])

        scr_u = {}

        halfpi_t = P.sbuf("halfpi", [128, 1], F32)
        HALFPI_T = T(halfpi_t[:], "halfpi")
        HALFPI = halfpi_t
        op("vector", lambda e: e.memset(halfpi_t[:], math.pi / 2.0), writes=[HALFPI_T])
        for mc in range(4):
            base = mc * (4 * 8 * 64)
            scr_u[mc] = scr_t[:, base:base + 4 * 8 * 64].rearrange("p (t i m) -> p t i m", t=4, i=8)

        class FFNState:
            pass

        def ffn_setup():
            cv = Carver()
            st = FFNState()
            st.GU = [cv.take([2, 8, 128], BF16, f"gu{s}") for s in range(NS)]
            st.DN = [cv.take([1024], BF16, f"dn{s}") for s in range(NS)]
            st.A = [[cv.take([512], BF16, f"a{p_}_{i}") for i in range(4)] for p_ in range(2)]
            st.SG = [cv.take([512], BF16, f"sg{i}") for i in range(2)]
            off_a = cv.off - 4096 - 1024
            off_g = cv.off
            st.GATEBC = [[cv.take([512], F32, f"gbc{p_}_{tc}") for tc in range(NTC)] for p_ in range(2)]
            cvg = Carver(); cvg.off = off_g
            st.RSTD = [cvg.take([512], F32, f"rstd{tc}") for tc in range(NTC)]
            st.SQB = [cvg.take([512], BF16, f"fsqb{i}") for i in range(4)]
            st.TMPL = cvg.take([512], F32, "ftmpl")
            assert cvg.off <= cv.off
            cva_ = Carver(); cva_.off = off_a
            st.HN32 = cva_.take([L], F32, "hn32")
            st.LG = cv.take([16, 8], F32, "lg")
            st.LG2 = cv.take([16, 8], F32, "lg2")
            st.EQ = cv.take([16, 8], F32, "eq")
            st.G = cv.take([16, 8], F32, "gates")
            st.M1 = cv.take([16], F32, "m1")
            st.M2 = cv.take([16], F32, "m2")
            st.DG = [cv.take([128], F32, f"dg{i}") for i in range(4)]
            st.issued = 0
            st.chunks = []
            return st

        def ffn_issue_loads(st, upto):
            while st.issued < min(upto, len(st.chunks)):
                ci = st.issued
                gu_src, dn_src = st.chunks[ci]
                s = ci % NS
                P.dma("gpsimd", lambda e, s=s, gu_src=gu_src: e.dma_start(out=st.GU[s][:], in_=gu_src), P.grp(f"gu{s}"), writes=[st.GU[s]])
                P.dma("gpsimd", lambda e, s=s, dn_src=dn_src: e.dma_start(out=st.DN[s][:], in_=dn_src), P.grp(f"dn{s}"), writes=[st.DN[s]])
                st.issued += 1

        def ffn_run(st, base_ci, gate_set=None, mid_hook=None):
            f0 = 0
            for gi_, gsz in enumerate(GROUPS):
                if gi_ == 3 and mid_hook is not None:
                    mid_hook()
                ffn_issue_loads(st, base_ci + f0 + NS)
                for tc in range(NTC):
                    par = tc % 2
                    for fl in range(gsz):
                        ci = base_ci + f0 + fl
                        s = ci % NS
                        psg = next_ps()
                        psu = next_ps()
                        for k in range(8):
                            op("tensor", lambda e, psg=psg, s=s, k=k, tc=tc: e.matmul(psg[:], lhsT=st.GU[s][:, 0, k, :], rhs=H[k][tc][:], start=(k == 0), stop=(k == 7)),
                               reads=[st.GU[s], H[k][tc]], writes=[psg], signal=(k == 7))
                        for k in range(8):
                            op("tensor", lambda e, psu=psu, s=s, k=k, tc=tc: e.matmul(psu[:], lhsT=st.GU[s][:, 1, k, :], rhs=H[k][tc][:], start=(k == 0), stop=(k == 7)),
                               reads=[st.GU[s], H[k][tc]], writes=[psu], signal=(k == 7))
                        sg = st.SG[(tc * 4 + fl) % 2]
                        a = st.A[par][fl]
                        op("scalar", lambda e, psg=psg, sg=sg: e.activation(out=sg[:], in_=psg[:], func=AF.Silu), reads=[psg], writes=[sg])
                        if gate_set is not None:
                            gb = st.GATEBC[gate_set][tc]
                            op("gpsimd", lambda e, sg=sg, gb=gb: e.tensor_tensor(out=sg[:], in0=sg[:], in1=gb[:], op=ALU.mult), reads=[sg, gb], writes=[sg])
                        op("vector", lambda e, psu=psu, sg=sg, a=a: e.tensor_tensor(out=a[:], in0=psu[:], in1=sg[:], op=ALU.mult), reads=[psu, sg], writes=[a])
                    for mo in range(8):
                        ps = next_ps()
                        for fl in range(gsz):
                            ci = base_ci + f0 + fl
                            s = ci % NS
                            op("tensor", lambda e, ps=ps, s=s, mo=mo, fl=fl, par=par, gsz=gsz: e.matmul(ps[:], lhsT=st.DN[s][:, mo * 128:(mo + 1) * 128], rhs=st.A[par][fl][:],
                                                                                                   start=(fl == 0), stop=(fl == gsz - 1)),
                               reads=[st.DN[s], st.A[par][fl]], writes=[ps], signal=(fl == gsz - 1))
                        xr = XR[mo][tc]
                        op("vector", lambda e, ps=ps, xr=xr: e.tensor_tensor(out=xr[:], in0=ps[:], in1=xr[:], op=ALU.add), reads=[ps, xr], writes=[xr])
                f0 += gsz

        def dump(idx):
            if not dbg:
                return
            for c in range(8):
                P.dma("sync", lambda e, c=c: e.dma_start(out=d_dbg[idx, :, c, :], in_=xres_t[:, c, :]), g_out, reads=XR[c])

        def dumph(idx):
            if not dbg:
                return
            for c in range(8):
                P.dma("sync", lambda e, c=c: e.dma_start(out=d_dbgh[idx, :, c, :], in_=h_t[:, c, :]), g_out, reads=H[c])

        if "mix0" in stages:
            mixer(0)
            dump(0)
        P.barrier()
        if "ffn0" in stages:
            st = ffn_setup()
            st.chunks = [(d_fgu[f], d_fdn[f]) for f in range(NF)]
            ffn_issue_loads(st, NS)
            rmsnorm(1, st.SQB, None, st.TMPL, norm_to_h(1), rstd_keep=st.RSTD)
            ffn_run(st, 0, None)
            dump(1)
        P.barrier()
        if "mix1" in stages:
            mixer(1)
            dump(2)
        P.barrier()
        if "ffn1" in stages:
            NSM = 4
            GROUPS_S = [2] * 11
            cv = Carver()
            st = FFNState()
            st.GU = [cv.take([2, 8, 128], BF16, f"mgu{s_}") for s_ in range(NSM)]
            st.DN = [cv.take([1024], BF16, f"mdn{s_}") for s_ in range(NSM)]
            off_a = cv.off
            SW = 640
            NOB = False
            CAP0 = 512 if NOB else SW
            st.A = [[cv.take([SW], BF16, f"ma{p_}_{i}") for i in range(2)] for p_ in range(2)]
            st.SG = [T(wglu_t[:, :, :].rearrange("p a b -> p (a b)")[:, i * SW:(i + 1) * SW], f"msg{i}") for i in range(2)]
            off_hc = cv.off
            HC = cv.take([8, SW], BF16, "hc")
            YSM = cv.take([5, 1024], BF16, "ysm")
            off_sel = cv.off
            SEL = [cv.take([SW], BF16, f"sel{i}") for i in range(4)]
            off_selt = cv.off
            SELT = [cv.take([512], BF16, f"selt{i}") for i in range(5)]
            cvs = Carver(); cvs.off = off_sel
            st.STG = [cvs.take([512], BF16, f"stg{i}") for i in range(2)]
            POSBC = [cv.take([512], F32, f"posbc{tc}") for tc in range(NTC)]
            st.GATEBC = [[cv.take([512], F32, f"mgbc{tc}") for tc in range(NTC)]]
            IOTA512 = cv.take([SW], F32, "iota512")
            st.LG = cv.take([16, 8], F32, "lg")
            st.LG2 = cv.take([16, 8], F32, "lg2")
            st.EQ = cv.take([16, 8], F32, "eq")
            st.G = cv.take([16, 8], F32, "gates")
            POSM = T(st.LG2.ap, "posm")
            TOT = T(st.LG.ap, "tot")
            OFFS = cv.take([16, 8], F32, "offs")
            st.M1 = cv.take([16], F32, "m1")
            st.M2 = cv.take([16], F32, "m2")
            NE = cv.take([8], F32, "ne")
            FL = cv.take([8, 4], F32, "fl")
            FLI = cv.take([8, 4], I32, "fli")
            FLANY = cv.take([2], I32, "flany")
            cvd = Carver(); cvd.off = off_selt
            st.DG = [cvd.take([128], F32, f"dg{i}") for i in range(2)]
            UT = T(poolw_t[:, :, :].rearrange("p a b -> p (a b)")[:, 0:256].bitcast(F32), "ut")
            IDENTB = cv.take([128], BF16, "identb")
            cvt = Carver(); cvt.off = off_a
            TMPS_ = cvt.take([512], F32, "tmps")
            cvn = Carver(); cvn.off = off_hc
            st.RSTD = [cvn.take([512], F32, f"mrstd{tc}") for tc in range(NTC)]
            st.SQB = [cvn.take([512], BF16, f"msqb{i}") for i in range(4)]
            st.TMPL = cvn.take([512], F32, "mtmpl")
            assert cvn.off <= off_hc + 8 * SW + 5120
            cvh = Carver(); cvh.off = 0
            st.HN32 = cvh.take([L], F32, "mhn32")
            YACC = [T(wbuf_t[:, :].bitcast(F32).rearrange("p (m n) -> p m n", m=8)[:, m, :], f"yacc{m}") for m in range(8)]
            YACC2 = T(ssmbc_t[:, :, :, :].rearrange("p a b c -> p (a b c)").rearrange("p (m n) -> p m n", m=8), "yacc2")
            hT_view = h_t[:, :, :].rearrange("p a b -> p (a b)").rearrange("p (k c) -> p k c", k=16)
            HTK = [T(hT_view[:, k, :], f"ht{k}") for k in range(16)]

            op("gpsimd", lambda e: e.iota(IOTA512[:], [[1, SW]], base=0, channel_multiplier=0, allow_small_or_imprecise_dtypes=True), writes=[IOTA512])
            op("vector", lambda e: e.tensor_scalar(out=UT[:], in0=iotaf_t[:], scalar1=iotap_t[:, 0:1], scalar2=None, op0=ALU.is_gt), reads=[IOTAF, IOTAP], writes=[UT])
            op("vector", lambda e: e.tensor_copy(out=IDENTB[:], in_=ident_t[:]), reads=[IDENT], writes=[IDENTB])

            cnt_ = [0]

            def sink_t(c, tc, r):
                stg = st.STG[cnt_[0] % 2]
                cnt_[0] += 1
                op("vector", lambda e: e.scalar_tensor_tensor(out=stg[:], in0=XR[c][tc][:], scalar=gains_t[:, 3, c:c + 1], in1=r[:], op0=ALU.mult, op1=ALU.mult),
                   reads=[XR[c][tc], GAINS, r], writes=[stg])
                ps = next_ps()
                psb = ps[:].bitcast(BF16)
                for j in range(4):
                    op("tensor", lambda e, j=j: e.transpose(psb[:, j * 128:(j + 1) * 128], stg[:, j * 128:(j + 1) * 128], IDENTB[:]), reads=[stg, IDENTB], writes=[ps], signal=(j == 3))
                op("scalar", lambda e: e.activation(out=hT_view[:, 4 * tc:4 * tc + 4, c * 128:(c + 1) * 128], in_=psb[:, 0:512].rearrange("p (a n) -> p a n", a=4), func=AF.Copy),
                   reads=[ps], writes=[HTK[4 * tc + j] for j in range(4)])

            rmsnorm(3, st.SQB, None, st.TMPL, sink_t, rstd_keep=st.RSTD)

            psl = next_ps()
            hn_v = st.HN32[:].rearrange("p (b c n) -> p b c n", b=2, c=8)
            g3b = gains_t[:, 3, :].unsqueeze(2).to_broadcast([128, 8, 128])
            for tl in range(16):
                tc_, o_ = tl // 4, (tl % 4) * 128
                hv = hn_v[:, tl % 2]
                op("vector", lambda e, hv=hv, tl=tl: e.tensor_tensor(out=hv, in0=xres_t[:, :, tl * 128:(tl + 1) * 128], in1=g3b, op=ALU.mult),
                   reads=[XR[c][tc_] for c in range(8)] + [GAINS], writes=[st.HN32])
                op("vector", lambda e, hv=hv, tc_=tc_, o_=o_: e.tensor_tensor(out=hv, in0=hv, in1=st.RSTD[tc_][:, o_:o_ + 128].unsqueeze(1).to_broadcast([128, 8, 128]), op=ALU.mult),
                   reads=[st.RSTD[tc_], st.HN32], writes=[st.HN32])
                for c in range(8):
                    op("tensor", lambda e, hv=hv, c=c, tl=tl: e.matmul(psl[:, tl * 8:(tl + 1) * 8], lhsT=hv[:, c, :], rhs=router_t[:, c, :], start=(c == 0), stop=(c == 7)),
                       reads=[st.HN32, ROUTER], writes=[psl], signal=(c == 7))
            lg3 = psl[:, 0:128].rearrange("p (t e) -> p t e", e=8)
            op("vector", lambda e: e.tensor_copy(out=st.LG[:], in_=lg3), reads=[psl], writes=[st.LG])
            op("vector", lambda e: e.tensor_reduce(out=st.M1[:], in_=st.LG[:], axis=AX.X, op=ALU.max), reads=[st.LG], writes=[st.M1])
            m1b = st.M1[:].unsqueeze(2).to_broadcast([128, 16, 8])
            op("vector", lambda e: e.tensor_tensor(out=st.EQ[:], in0=st.LG[:], in1=m1b, op=ALU.is_equal), reads=[st.LG, st.M1], writes=[st.EQ])
            op("vector", lambda e: e.scalar_tensor_tensor(out=st.LG2[:], in0=st.EQ[:], scalar=-1e30, in1=st.LG[:], op0=ALU.mult, op1=ALU.add), reads=[st.EQ, st.LG], writes=[st.LG2])
            op("vector", lambda e: e.tensor_reduce(out=st.M2[:], in_=st.LG2[:], axis=AX.X, op=ALU.max), reads=[st.LG2], writes=[st.M2])
            m2b = st.M2[:].unsqueeze(2).to_broadcast([128, 16, 8])
            op("vector", lambda e: e.tensor_tensor(out=st.EQ[:], in0=st.LG[:], in1=m2b, op=ALU.is_ge), reads=[st.LG, st.M2], writes=[st.EQ])
            op("vector", lambda e: e.tensor_tensor(out=st.LG2[:], in0=st.LG[:], in1=m1b, op=ALU.subtract), reads=[st.LG, st.M1], writes=[st.LG2])
            op("scalar", lambda e: e.activation(out=st.LG2[:], in_=st.LG2[:], func=AF.Exp), reads=[st.LG2], writes=[st.LG2])
            op("vector", lambda e: e.tensor_tensor(out=st.LG2[:], in0=st.LG2[:], in1=st.EQ[:], op=ALU.mult), reads=[st.LG2, st.EQ], writes=[st.LG2])
            op("vector", lambda e: e.tensor_reduce(out=st.M1[:], in_=st.LG2[:], axis=AX.X, op=ALU.add), reads=[st.LG2], writes=[st.M1])
            op("vector", lambda e: e.reciprocal(out=st.M1[:], in_=st.M1[:]), reads=[st.M1], writes=[st.M1])
            op("vector", lambda e: e.tensor_tensor(out=st.G[:], in0=st.LG2[:], in1=m1b, op=ALU.mult), reads=[st.LG2, st.M1], writes=[st.G])

            psw = next_ps()
            eq2 = st.EQ[:].rearrange("p t e -> p (t e)")
            op("tensor", lambda e: e.matmul(psw[:, 0:128], lhsT=UT[:], rhs=eq2, start=True, stop=True), reads=[UT, st.EQ], writes=[psw])
            op("tensor", lambda e: e.matmul(psw[:, 128:256], lhsT=onesf_t[:], rhs=eq2, start=True, stop=True), reads=[ONESF, st.EQ], writes=[psw])
            op("vector", lambda e: e.tensor_copy(out=TOT[:], in_=psw[:, 128:256].rearrange("p (t e) -> p t e", e=8)), reads=[psw], writes=[TOT])
            op("vector", lambda e: e.memset(OFFS[:, 0, :], 0.0), writes=[OFFS])
            for k in range(15):
                op("vector", lambda e, k=k: e.tensor_tensor(out=OFFS[:, k + 1, :], in0=OFFS[:, k, :], in1=TOT[:, k, :], op=ALU.add), reads=[OFFS, TOT], writes=[OFFS])
            op("vector", lambda e: e.tensor_tensor(out=NE[:], in0=OFFS[:, 15, :], in1=TOT[:, 15, :], op=ALU.add), reads=[OFFS, TOT], writes=[NE])
            op("vector", lambda e: e.tensor_tensor(out=POSM[:], in0=psw[:, 0:128].rearrange("p (t e) -> p t e", e=8), in1=OFFS[:], op=ALU.add), reads=[psw, OFFS], writes=[POSM])
            op("vector", lambda e: e.tensor_single_scalar(out=POSM[:], in_=POSM[:], scalar=1.0e6, op=ALU.add), reads=[POSM], writes=[POSM])
            op("vector", lambda e: e.tensor_tensor(out=POSM[:], in0=POSM[:], in1=st.EQ[:], op=ALU.mult), reads=[POSM, st.EQ], writes=[POSM])
            op("vector", lambda e: e.tensor_single_scalar(out=POSM[:], in_=POSM[:], scalar=-1.0e6, op=ALU.add), reads=[POSM], writes=[POSM])
            for cch in range(4):
                op("vector", lambda e, cch=cch: e.tensor_single_scalar(out=FL[:, :, cch], in_=NE[:], scalar=float(0 if cch == 0 else CAP0 + 512 * (cch - 1)) + 0.5, op=ALU.is_gt), reads=[NE], writes=[FL])
            op("vector", lambda e: e.tensor_reduce(out=st.M2[:, 0:1], in_=NE[:], axis=AX.X, op=ALU.max), reads=[NE], writes=[st.M2])
            op("vector", lambda e: e.tensor_single_scalar(out=st.M2[:, 1:2], in_=st.M2[:, 0:1], scalar=float(CAP0) + 0.5, op=ALU.is_gt), reads=[st.M2], writes=[st.M2])
            op("vector", lambda e: e.tensor_copy(out=FLANY[:, 0:1], in_=st.M2[:, 1:2]), reads=[st.M2], writes=[FLANY])
            op("vector", lambda e: e.tensor_copy(out=FLI[:], in_=FL[:]), reads=[FL], writes=[FLI])

            def build_bc(src, dst, ex):
                for tc in range(NTC):
                    ps = next_ps()
                    for t4 in range(4):
                        tl = tc * 4 + t4
                        dg = st.DG[t4 % 2]
                        op("vector", lambda e, dg=dg, tl=tl: e.tensor_scalar(out=dg[:], in0=ident_t[:], scalar1=src[:, tl, ex:ex + 1], scalar2=None, op0=ALU.mult), reads=[IDENT, src], writes=[dg])
                        op("tensor", lambda e, ps=ps, dg=dg, t4=t4: e.matmul(ps[:, t4 * 128:(t4 + 1) * 128], lhsT=onesf_t[:], rhs=dg[:], start=True, stop=True),
                           reads=[ONESF, dg], writes=[ps], signal=True)
                    gb = dst[tc]
                    op("scalar", lambda e, ps=ps, gb=gb: e.activation(out=gb[:], in_=ps[:], func=AF.Copy), reads=[ps], writes=[gb])

            gci = [0]

            pre_state = {}

            def prefetch_first(ex):
                slot_of = [(gci[0] + f) % NSM for f in range(NF)]
                pre_state[ex] = (gci[0], slot_of)
                gci[0] += NF
                ex_dep = [FLI.last_w]
                for f in range(NSM):
                    s_ = slot_of[f]
                    gu_src, dn_src = d_mgu[ex * NF + f], d_mdn[ex * NF + f]
                    P.dma("gpsimd", lambda e, s_=s_, gu_src=gu_src: e.dma_start(out=st.GU[s_][:], in_=gu_src), P.grp(f"mgu{s_}"), writes=[st.GU[s_]], extra=ex_dep)
                    P.dma("gpsimd", lambda e, s_=s_, dn_src=dn_src: e.dma_start(out=st.DN[s_][:], in_=dn_src), P.grp(f"mdn{s_}"), writes=[st.DN[s_]], extra=ex_dep)

            def chunk_pass(ex, cch, flag_ap, mid_hook=None):
                base = 0 if cch == 0 else CAP0 + 512 * (cch - 1)
                has_b = (cch == 0) and not NOB
                nsl = SW if has_b else 512
                nst = 5 if has_b else 4
                loads = [(d_mgu[ex * NF + f], d_mdn[ex * NF + f]) for f in range(NF)]
                if cch == 0 and ex in pre_state:
                    slot_of = pre_state[ex][1]
                    issued = [NSM]
                else:
                    slot_of = [(gci[0] + f) % NSM for f in range(NF)]
                    gci[0] += NF
                    issued = [0]
                P.cond_begin(flag_ap, FLI)

                def issue(upto):
                    while issued[0] < min(upto, NF):
                        f = issued[0]
                        s_ = slot_of[f]
                        gu_src, dn_src = loads[f]
                        P.dma("gpsimd", lambda e, s_=s_, gu_src=gu_src: e.dma_start(out=st.GU[s_][:], in_=gu_src), P.grp(f"mgu{s_}"), writes=[st.GU[s_]])
                        P.dma("gpsimd", lambda e, s_=s_, dn_src=dn_src: e.dma_start(out=st.DN[s_][:], in_=dn_src), P.grp(f"mdn{s_}"), writes=[st.DN[s_]])
                        issued[0] += 1

                issue(NSM)
                for half in range(2):
                    pss = [next_ps() for _ in range(4)]
                    psbl = [next_ps() for _ in range(4)] if has_b else None
                    for k in range(16):
                        sel = SEL[k % 4]
                        op("vector", lambda e, sel=sel, k=k: e.tensor_scalar(out=sel[:, 0:nsl], in0=IOTA512[:, 0:nsl], scalar1=float(base), scalar2=POSM[:, k, ex:ex + 1], op0=ALU.add, op1=ALU.is_equal),
                           reads=[IOTA512, POSM], writes=[sel])
                        for mi in range(4):
                            m = half * 4 + mi
                            op("tensor", lambda e, sel=sel, k=k, mi=mi, m=m, pss=pss: e.matmul(pss[mi][:], lhsT=HTK[k][:, m * 128:(m + 1) * 128], rhs=sel[:, 0:512], start=(k == 0), stop=(k == 15)),
                               reads=[HTK[k], sel], writes=[pss[mi]], signal=(mi == 3 and not has_b))
                        if has_b:
                            for mi in range(4):
                                m = half * 4 + mi
                                op("tensor", lambda e, sel=sel, k=k, mi=mi, m=m, psbl=psbl: e.matmul(psbl[mi][:, 0:128], lhsT=HTK[k][:, m * 128:(m + 1) * 128], rhs=sel[:, 512:SW],
                                                                                                      start=(k == 0), stop=(k == 15)),
                                   reads=[HTK[k], sel], writes=[psbl[mi]], signal=(mi == 3))
                    for mi in range(4):
                        m = half * 4 + mi
                        op("scalar", lambda e, mi=mi, m=m, pss=pss: e.activation(out=HC[:, m, 0:512], in_=pss[mi][:], func=AF.Copy), reads=[pss[mi]], writes=[HC])
                    if has_b:
                        for mi in range(4):
                            m = half * 4 + mi
                            op("scalar", lambda e, mi=mi, m=m, psbl=psbl: e.activation(out=HC[:, m, 512:SW], in_=psbl[mi][:, 0:128], func=AF.Copy), reads=[psbl[mi]], writes=[HC])
                f0 = 0
                for gi_, gsz in enumerate(GROUPS_S):
                    issue(f0 + NSM)
                    par = gi_ % 2
                    for fl in range(gsz):
                        s_ = slot_of[f0 + fl]
                        psg = next_ps()
                        psu = next_ps()
                        psb = next_ps() if has_b else None
                        for k in range(8):
                            op("tensor", lambda e, psg=psg, s_=s_, k=k: e.matmul(psg[:], lhsT=st.GU[s_][:, 0, k, :], rhs=HC[:, k, 0:512], start=(k == 0), stop=(k == 7)),
                               reads=[st.GU[s_], HC], writes=[psg], signal=(k == 7))
                        for k in range(8):
                            op("tensor", lambda e, psu=psu, s_=s_, k=k: e.matmul(psu[:], lhsT=st.GU[s_][:, 1, k, :], rhs=HC[:, k, 0:512], start=(k == 0), stop=(k == 7)),
                               reads=[st.GU[s_], HC], writes=[psu], signal=(k == 7))
                        if has_b:
                            for r_ in range(2):
                                for k in range(8):
                                    op("tensor", lambda e, psb=psb, s_=s_, k=k, r_=r_: e.matmul(psb[:, r_ * 128:(r_ + 1) * 128], lhsT=st.GU[s_][:, r_, k, :], rhs=HC[:, k, 512:SW], start=(k == 0), stop=(k == 7)),
                                       reads=[st.GU[s_], HC], writes=[psb], signal=(k == 7))
                        sg = st.SG[fl % 2]
                        a = st.A[par][fl]
                        op("scalar", lambda e, psg=psg, sg=sg: e.activation(out=sg[:, 0:512], in_=psg[:], func=AF.Silu), reads=[psg], writes=[sg])
                        if has_b:
                            op("scalar", lambda e, psb=psb, sg=sg: e.activation(out=sg[:, 512:SW], in_=psb[:, 0:128], func=AF.Silu), reads=[psb], writes=[sg])
                        op("vector", lambda e, psu=psu, sg=sg, a=a: e.tensor_tensor(out=a[:, 0:512], in0=psu[:], in1=sg[:, 0:512], op=ALU.mult), reads=[psu, sg], writes=[a])
                        if has_b:
                            op("vector", lambda e, psb=psb, sg=sg, a=a: e.tensor_tensor(out=a[:, 512:SW], in0=psb[:, 128:256], in1=sg[:, 512:SW], op=ALU.mult), reads=[psb, sg], writes=[a])
                    psd = [next_ps(), next_ps()] if has_b else None
                    for m in range(8):
                        ps = next_ps()
                        while psd is not None and (ps is psd[0] or ps is psd[1]):
                            ps = next_ps()
                        for fl in range(gsz):
                            s_ = slot_of[f0 + fl]
                            op("tensor", lambda e, ps=ps, s_=s_, m=m, fl=fl, par=par, gsz=gsz: e.matmul(ps[:], lhsT=st.DN[s_][:, m * 128:(m + 1) * 128], rhs=st.A[par][fl][:, 0:512],
                                                                                                     start=(fl == 0), stop=(fl == gsz - 1)),
                               reads=[st.DN[s_], st.A[par][fl]], writes=[ps], signal=(fl == gsz - 1))
                        if has_b:
                            for fl in range(gsz):
                                s_ = slot_of[f0 + fl]
                                op("tensor", lambda e, psd=psd, s_=s_, m=m, fl=fl, par=par, gsz=gsz: e.matmul(psd[m // 4][:, (m % 4) * 128:(m % 4 + 1) * 128], lhsT=st.DN[s_][:, m * 128:(m + 1) * 128],
                                                                                                           rhs=st.A[par][fl][:, 512:SW], start=(fl == 0), stop=(fl == gsz - 1)),
                                   reads=[st.DN[s_], st.A[par][fl]], writes=[psd[m // 4]], signal=(fl == gsz - 1))
                        ya = YACC[m]
                        if gi_ == 0:
                            op("scalar", lambda e, ps=ps, ya=ya: e.activation(out=ya[:], in_=ps[:], func=AF.Copy), reads=[ps], writes=[ya])
                        else:
                            op("vector", lambda e, ps=ps, ya=ya: e.tensor_tensor(out=ya[:], in0=ps[:], in1=ya[:], op=ALU.add), reads=[ps, ya], writes=[ya])
                    if has_b:
                        for hf in range(2):
                            y2 = YACC2[:, hf * 4:hf * 4 + 4, :]
                            pv = psd[hf][:].rearrange("p (a n) -> p a n", a=4)
                            if gi_ == 0:
                                op("scalar", lambda e, pv=pv, y2=y2: e.activation(out=y2, in_=pv, func=AF.Copy), reads=[psd[hf]], writes=[YACC2])
                            else:
                                op("vector", lambda e, pv=pv, y2=y2: e.tensor_tensor(out=y2, in0=pv, in1=y2, op=ALU.add), reads=[psd[hf], YACC2], writes=[YACC2])
                    f0 += gsz
                P.cond_end()
                if mid_hook is not None:
                    mid_hook()
                P.cond_begin(flag_ap, FLI)
                for st4 in range(nst):
                    for hf in range(2):
                        ps = next_ps()
                        for mi in range(4):
                            m = hf * 4 + mi
                            if st4 < 4:
                                op("tensor", lambda e, ps=ps, mi=mi, m=m, st4=st4: e.transpose(ps[:, mi * 128:(mi + 1) * 128], YACC[m][:, st4 * 128:(st4 + 1) * 128], ident_t[:]),
                                   reads=[YACC[m], IDENT], writes=[ps], signal=(mi == 3))
                            else:
                                op("tensor", lambda e, ps=ps, mi=mi, m=m: e.transpose(ps[:, mi * 128:(mi + 1) * 128], YACC2[:, m, :], ident_t[:]),
                                   reads=[YACC2, IDENT], writes=[ps], signal=(mi == 3))
                        op("scalar", lambda e, ps=ps, st4=st4, hf=hf: e.activation(out=YSM[:, st4, hf * 512:(hf + 1) * 512], in_=ps[:], func=AF.Copy), reads=[ps], writes=[YSM])
                for tcx in range(NTC):
                    for st4 in range(nst):
                        op("vector", lambda e, st4=st4, tcx=tcx: e.tensor_scalar(out=SELT[st4][:], in0=POSBC[tcx][:], scalar1=float(base + 128 * st4), scalar2=iotap_t[:, 0:1],
                                                                                  op0=ALU.subtract, op1=ALU.is_equal), reads=[POSBC[tcx], IOTAP], writes=[SELT[st4]])
                    for m in range(8):
                        ps = next_ps()
                        for st4 in range(nst):
                            op("tensor", lambda e, ps=ps, st4=st4, m=m: e.matmul(ps[:], lhsT=YSM[:, st4, m * 128:(m + 1) * 128], rhs=SELT[st4][:], start=(st4 == 0), stop=(st4 == nst - 1)),
                               reads=[YSM, SELT[st4]], writes=[ps], signal=(st4 == nst - 1))
                        gb = st.GATEBC[0][tcx]
                        xr = XR[m][tcx]
                        op("vector", lambda e, ps=ps, gb=gb: e.tensor_tensor(out=TMPS_[:], in0=ps[:], in1=gb[:], op=ALU.mult), reads=[ps, gb], writes=[TMPS_])
                        op("vector", lambda e, xr=xr: e.tensor_tensor(out=xr[:], in0=TMPS_[:], in1=xr[:], op=ALU.add), reads=[TMPS_, xr], writes=[xr])
                P.cond_end()

            prefetch_first(0)
            for ex in range(8):
                build_bc(st.G, st.GATEBC[0], ex)
                build_bc(POSM, POSBC, ex)
                chunk_pass(ex, 0, FLI[0:1, ex, 0:1], mid_hook=(lambda ex=ex: prefetch_first(ex + 1)) if ex < 7 else None)
            P.cond_begin(FLANY[0:1, 0:1], FLANY)
            for ex in range(8):
                P.cond_begin(FLI[0:1, ex, 1:2], FLI)
                build_bc(st.G, st.GATEBC[0], ex)
                build_bc(POSM, POSBC, ex)
                for cch in range(1, 4):
                    chunk_pass(ex, cch, FLI[0:1, ex, cch:cch + 1])
                P.cond_end()
            P.cond_end()
            dump(3)
        if "final" in stages:
            cv = Carver()
            cv.off = SCR_ELEMS - 16 * 1024
            P.barrier()
            OST = [cv.take([512], F32, f"ost{i}") for i in range(4)]
            SQB = [cv.take([512], BF16, f"osq{i}") for i in range(4)]
            RST = [cv.take([512], F32, f"orst{i}") for i in range(2)]
            TMPL2 = cv.take([512], F32, "otmpl")
            cnt = [0]
            last = [None]

            def sink(c, tc, r):
                oi = cnt[0] % 4
                o = OST[oi]
                cnt[0] += 1
                op("vector", lambda e: e.scalar_tensor_tensor(out=o[:], in0=XR[c][tc][:], scalar=gains_t[:, 4, c:c + 1], in1=r[:], op0=ALU.mult, op1=ALU.mult),
                   reads=[XR[c][tc], GAINS, r], writes=[o])
                last[0] = P.dma("sync", lambda e: e.dma_start(out=d_out[:, c, tc * TC:(tc + 1) * TC], in_=o[:]), P.grp(f"ost{oi}"), reads=[o])

            rmsnorm(4, SQB, RST, TMPL2, sink)
        else:
            last = [None]
            for c in range(8):
                last[0] = P.dma("sync", lambda e, c=c: e.dma_start(out=d_out[:, c, :], in_=xres_t[:, c, :]), P.grp(f"ost{c % 4}"), reads=XR[c])
        for g_ in P.groups:
            if g_.count > 0:
                P.wait_tok("sync", Tok(g_.sem, 16 * g_.count))
        P.barrier()
        P.finish()
    return nc


def _prep_shared(inp):
    f = np.float32

    def cmaj(v):
        return np.ascontiguousarray(np.asarray(v, f).reshape(8, 128).T)

    def c4(v):
        return np.ascontiguousarray(np.asarray(v, f).reshape(4, 128).T)

    gains = np.stack([cmaj(inp["norm_mix_g"][0]), cmaj(inp["norm_ffn_g"][0]), cmaj(inp["norm_mix_g"][1]), cmaj(inp["norm_ffn_g"][1]),
                      cmaj(inp["final_norm_g"])], axis=1)
    w_in = np.ascontiguousarray(np.asarray(inp["w_in"], f).reshape(2, 8, 128, 1024).transpose(0, 2, 1, 3))
    w_out = np.ascontiguousarray(np.asarray(inp["w_out"], f).reshape(2, 8, 128, 1024).transpose(0, 2, 1, 3))
    w_glu = np.ascontiguousarray(np.asarray(inp["ssm_w_glu"], f).reshape(2, 4, 128, 512).transpose(0, 2, 1, 3))
    vec = np.stack([np.stack([c4(inp["ssm_b_glu"][l]), c4(inp["pool_scale"][l]), c4(inp["ssm_d"][l])], axis=1) for l in range(2)], axis=1)
    pool_w = np.ascontiguousarray(np.asarray(inp["pool_w"], f).transpose(0, 2, 1, 3))

    def hn(v):
        return np.asarray(v, f).reshape(16, 2, 64).transpose(1, 2, 0).reshape(128, 16)

    ssm_a = np.zeros((2, 128, 3, 16), f)
    ssm_bc = np.zeros((2, 128, 4, 16, 16), f)
    for l in range(2):
        ssm_a[l, :, 0] = hn(np.repeat(np.asarray(inp["ssm_log_dt"][l], f)[:, None], 64, axis=1))
        ssm_a[l, :, 1] = hn(inp["ssm_a_re"][l])
        ssm_a[l, :, 2] = hn(inp["ssm_a_im"][l])
        for i, key in enumerate(["ssm_b_re", "ssm_b_im"]):
            b = np.asarray(inp[key][l], f).reshape(16, 2, 64, 16)
            ssm_bc[l, :, i] = b.transpose(1, 2, 0, 3).reshape(128, 16, 16)
        for i, key in enumerate(["ssm_c_re", "ssm_c_im"]):
            c = np.asarray(inp[key][l], f).reshape(16, 2, 16, 64)
            ssm_bc[l, :, 2 + i] = c.transpose(1, 3, 0, 2).reshape(128, 16, 16)

    def gu(wg, wu):
        a = np.stack([np.asarray(wg, f), np.asarray(wu, f)], axis=0)
        a = a.reshape(2, 8, 128, NF, 128)
        return np.ascontiguousarray(a.transpose(3, 2, 0, 1, 4))

    ffn_gu = gu(inp["ffn_w_gate"][0], inp["ffn_w_up"][0])
    ffn_dn = np.ascontiguousarray(np.asarray(inp["ffn_w_down"][0], f).reshape(NF, 128, 1024))
    moe_gu = np.concatenate([gu(inp["moe_w_gate"][0][e], inp["moe_w_up"][0][e]) for e in range(8)], axis=0)
    moe_dn = np.ascontiguousarray(np.asarray(inp["moe_w_down"][0], f).reshape(8 * NF, 128, 1024))
    router = np.ascontiguousarray(np.asarray(inp["router_w"][0], f).reshape(8, 128, 8).transpose(1, 0, 2))
    return dict(gains=np.ascontiguousarray(gains), w_in=w_in, w_out=w_out, w_glu=w_glu, vec512=np.ascontiguousarray(vec), pool_w=pool_w,
                ssm_a=ssm_a, ssm_bc=ssm_bc, ffn_gu=ffn_gu, ffn_dn=ffn_dn, moe_gu=moe_gu, moe_dn=moe_dn, router=router)


def _x_layout(xb):
    return np.ascontiguousarray(np.asarray(xb, np.float32).T.reshape(8, 128, L).transpose(1, 0, 2))


def _out_layout(y):
    return np.ascontiguousarray(y.transpose(1, 0, 2).reshape(1024, L).T)


def kernel(**inputs):
    shared = _prep_shared(inputs)
    x = np.asarray(inputs["x"], np.float32)
    nb = x.shape[0]
    nc = build_program()
    in_maps = []
    for b in range(nb):
        m = dict(shared)
        m["xT"] = _x_layout(x[b])
        in_maps.append(m)
    res = run_bass_kernel_spmd(nc, in_maps, core_ids=list(range(nb)))
    out = np.stack([_out_layout(np.asarray(r["yT"])) for r in res.results], axis=0)
    return out.astype(np.float32)
```
